# Optimizing a Trainium2 kernel written in Bass

```python
import jax, jax.numpy as jnp
from jax import lax
import numpy as np

D_MODEL = 1024
BATCH = 32
SEQ = 2048
DEPTH = 1

D_MIX = D_MODEL
D_RET = D_MIX // 2
D_GLA = D_MIX - D_RET
RET_HEADS = 4
RET_DK = D_RET // RET_HEADS
RET_DV = D_RET // RET_HEADS
RET_CHUNK = 128
GLA_HEADS = 4
GLA_DK_TOTAL = D_GLA // 2
GLA_DK = GLA_DK_TOTAL // GLA_HEADS
GLA_DV = D_GLA // GLA_HEADS
GLA_CHUNK = 64
GLA_GATE_RANK = 16
GLA_GATE_TAU = 16.0
ROPE_THETA = 10000.0
EPS = 1e-6
D_IN = 4 * D_RET + 2 * GLA_DK_TOTAL + 2 * D_GLA + GLA_GATE_RANK

kernel_name = "hybrid_retention_gla_adaln_layer"


def _rms(x):
    xf = x.astype(jnp.float32)
    return (xf * lax.rsqrt(jnp.mean(xf * xf, axis=-1, keepdims=True) + EPS)).astype(x.dtype)


def _rotary(t):
    S, d = t.shape[1], t.shape[-1]
    inv_freq = ROPE_THETA ** (-jnp.arange(0, d, 2, dtype=jnp.float32) / d)
    ang = jnp.arange(S, dtype=jnp.float32)[:, None] * inv_freq[None, :]
    cos = jnp.cos(ang)[None, :, None, :]
    sin = jnp.sin(ang)[None, :, None, :]
    t1, t2 = jnp.split(t.astype(jnp.float32), 2, axis=-1)
    return jnp.concatenate([t1 * cos - t2 * sin, t1 * sin + t2 * cos], axis=-1).astype(t.dtype)


def _retention(q, k, v):
    B, S, H, dk = q.shape
    dv = v.shape[-1]
    C = RET_CHUNK
    nc = S // C
    log_g = jnp.log(1.0 - 2.0 ** (-5.0 - jnp.arange(H, dtype=jnp.float32)))
    q = q.reshape(B, nc, C, H, dk)
    k = k.reshape(B, nc, C, H, dk)
    v = v.reshape(B, nc, C, H, dv)
    idx = jnp.arange(C, dtype=jnp.float32)
    diff = idx[:, None] - idx[None, :]
    decay = jnp.where(diff[None] >= 0,
                      jnp.exp(jnp.maximum(diff, 0.0)[None] * log_g[:, None, None]), 0.0)
    scores = jnp.einsum('bnihd,bnjhd->bnhij', q, k) * decay
    inner = jnp.einsum('bnhij,bnjhe->bnihe', scores, v)
    zeta = jnp.exp((C - 1.0 - idx)[None, :] * log_g[:, None])
    kv = jnp.einsum('bnjhd,hj,bnjhe->nbhde', k, zeta, v)
    chunk_decay = jnp.exp(C * log_g)[None, :, None, None]

    def step(R, kv_n):
        return R * chunk_decay + kv_n, R

    _, R_prev = lax.scan(step, jnp.zeros((B, H, dk, dv), kv.dtype), kv)
    xi = jnp.exp((idx + 1.0)[None, :] * log_g[:, None])
    cross = jnp.einsum('bnihd,nbhde,hi->bnihe', q, R_prev, xi)
    return (inner + cross).reshape(B, S, H, dv)


def _gla(q, k, v, log_a):
    B, S, H, dk = q.shape
    dv = v.shape[-1]
    C = GLA_CHUNK
    nc = S // C
    q = q.reshape(B, nc, C, H, dk).astype(jnp.float32)
    k = k.reshape(B, nc, C, H, dk).astype(jnp.float32)
    v = v.reshape(B, nc, C, H, dv)
    b = jnp.cumsum(log_a.reshape(B, nc, C, H, dk).astype(jnp.float32), axis=2)
    q_e = q * jnp.exp(b)
    k_e = k * jnp.exp(-b)
    mask = jnp.tril(jnp.ones((C, C), dtype=bool))
    scores = jnp.where(mask, jnp.einsum('bnihd,bnjhd->bnhij', q_e, k_e), 0.0)
    inner = jnp.einsum('bnhij,bnjhe->bnihe', scores, v)
    b_last = b[:, :, -1:]
    kv = jnp.einsum('bnjhd,bnjhe->nbhde', k * jnp.exp(b_last - b), v)
    a_chunk = jnp.transpose(jnp.exp(b_last[:, :, 0]), (1, 0, 2, 3))

    def step(St, inp):
        kv_n, a_n = inp
        return St * a_n[..., None] + kv_n, St

    _, S_prev = lax.scan(step, jnp.zeros((B, H, dk, dv), kv.dtype), (kv, a_chunk))
    cross = jnp.einsum('bnihd,nbhde->bnihe', q_e, S_prev)
    return (inner + cross).reshape(B, S, H, dv)


def setup_inputs(seed: int = 0) -> dict:
    key = jax.random.key(seed)
    ks = jax.random.split(key, 12)
    f32 = jnp.float32
    x = jax.random.normal(ks[0], (BATCH, SEQ, D_MODEL), f32)
    c = jax.random.normal(ks[1], (BATCH, D_MODEL), f32)
    norm_gain = 1.0 + 0.02 * jax.random.normal(ks[2], (DEPTH, D_MODEL), f32)
    w_ada = jax.random.normal(ks[3], (DEPTH, D_MODEL, 3 * D_MODEL), f32) * D_MODEL ** -0.5
    b_ada = 0.02 * jax.random.normal(ks[4], (DEPTH, 3 * D_MODEL), f32)
    w_in = jax.random.normal(ks[5], (DEPTH, D_MODEL, D_IN), f32) * D_MODEL ** -0.5
    w_gate_up = jax.random.normal(ks[6], (DEPTH, GLA_GATE_RANK, GLA_DK_TOTAL), f32) * GLA_GATE_RANK ** -0.5
    b_gate_up = 0.02 * jax.random.normal(ks[7], (DEPTH, GLA_DK_TOTAL), f32)
    gla_norm_gain = 1.0 + 0.02 * jax.random.normal(ks[8], (DEPTH, GLA_DV), f32)
    w_out = jax.random.normal(ks[9], (DEPTH, D_MIX, D_MODEL), f32) * D_MIX ** -0.5
    final_gain = 1.0 + 0.02 * jax.random.normal(ks[10], (D_MODEL,), f32)
    return {"x": x, "c": c, "norm_gain": norm_gain, "w_ada": w_ada, "b_ada": b_ada,
            "w_in": w_in, "w_gate_up": w_gate_up, "b_gate_up": b_gate_up,
            "gla_norm_gain": gla_norm_gain, "w_out": w_out, "final_gain": final_gain}


def reference(x, c, norm_gain, w_ada, b_ada, w_in, w_gate_up, b_gate_up, gla_norm_gain, w_out, final_gain):
    B, S, D = x.shape
    cs = jax.nn.silu(c)
    for l in range(DEPTH):
        ada = cs @ w_ada[l] + b_ada[l]
        shift, scale, gate = jnp.split(ada, 3, axis=-1)
        h = _rms(x) * norm_gain[l] * (1.0 + scale[:, None, :]) + shift[:, None, :]
        p = h @ w_in[l]
        o = np.cumsum([0, D_RET, D_RET, D_RET, D_RET, GLA_DK_TOTAL, GLA_DK_TOTAL, D_GLA, D_GLA, GLA_GATE_RANK])
        rq, rk, rv, rz, gq, gk, gv, gz, glr = [p[..., o[i]:o[i + 1]] for i in range(9)]
        rq = _rotary(rq.reshape(B, S, RET_HEADS, RET_DK))
        rk = _rotary(rk.reshape(B, S, RET_HEADS, RET_DK)) * (RET_DK ** -0.5)
        y_ret = _retention(rq, rk, rv.reshape(B, S, RET_HEADS, RET_DV))
        y_ret = _rms(y_ret) * jax.nn.silu(rz.reshape(B, S, RET_HEADS, RET_DV).astype(jnp.float32))
        log_a = jax.nn.log_sigmoid((glr @ w_gate_up[l] + b_gate_up[l]).astype(jnp.float32)) / GLA_GATE_TAU
        y_gla = _gla(gq.reshape(B, S, GLA_HEADS, GLA_DK) * (GLA_DK ** -0.5),
                     gk.reshape(B, S, GLA_HEADS, GLA_DK),
                     gv.reshape(B, S, GLA_HEADS, GLA_DV),
                     log_a.reshape(B, S, GLA_HEADS, GLA_DK))
        y_gla = _rms(y_gla) * gla_norm_gain[l] * jax.nn.silu(gz.reshape(B, S, GLA_HEADS, GLA_DV).astype(jnp.float32))
        mix = jnp.concatenate([y_ret.reshape(B, S, D_RET), y_gla.reshape(B, S, D_GLA)], axis=-1).astype(x.dtype)
        x = x + gate[:, None, :] * (mix @ w_out[l])
    return _rms(x) * final_gain
```

```python
import math
from contextlib import ExitStack

import numpy as np
import concourse.bass as bass
import concourse.mybir as mybir
from concourse.bass_utils import run_bass_kernel_spmd

F32 = mybir.dt.float32
BF16 = mybir.dt.bfloat16
AF = mybir.ActivationFunctionType
ALU = mybir.AluOpType

D = 1024
SEQ = 2048
DIN = 3600
NCORES = 8
EPS = 1e-6
G = [1.0 - 2.0 ** (-5.0 - h) for h in range(4)]


class Buf:
    __slots__ = ("name", "w", "r")

    def __init__(self, name):
        self.name = name
        self.w = None
        self.r = []


class KB:
    def __init__(self, nc, stack):
        self.nc = nc
        self.engs = {"pe": nc.tensor, "act": nc.scalar, "dve": nc.vector, "pool": nc.gpsimd, "sp": nc.sync}
        self.sems = {}
        self.cnt = {}
        self.stack = stack
        for e in self.engs:
            self.sems[e] = stack.enter_context(nc.semaphore("s_" + e))
            self.cnt[e] = 0
        self.seen = {e: {} for e in self.engs}
        self.nwaits = 0
        self.dma_toks = {}

    def new_sem(self, name):
        self.sems[name] = self.stack.enter_context(self.nc.semaphore("s_" + name))
        self.cnt[name] = 0
        return name

    @staticmethod
    def _deps(reads, writes):
        deps = []
        for b in reads:
            if b.w is not None:
                deps.append(b.w)
        for b in writes:
            if b.w is not None:
                deps.append(b.w)
            deps.extend(b.r)
        return deps

    def _wait(self, e, deps):
        best = {}
        for (s, v) in deps:
            if v > best.get(s, 0):
                best[s] = v
        for s, v in best.items():
            if self.seen[e].get(s, 0) < v:
                self.engs[e].wait_ge(self.sems[s], v)
                self.seen[e][s] = v
                self.nwaits += 1

    @staticmethod
    def _commit(tok, reads, writes):
        for b in writes:
            b.w = tok
            b.r = []
        for b in reads:
            if b not in writes:
                b.r.append(tok)

    def op(self, e, fn, reads=(), writes=()):
        self._wait(e, self._deps(reads, writes))
        inst = fn()
        self.cnt[e] += 1
        inst.then_inc(self.sems[e], 1)
        tok = (e, self.cnt[e])
        self._commit(tok, reads, writes)
        return tok

    def dma(self, e, sem, pairs, reads=(), writes=(), **kw):
        if sem is None:
            sem = self.new_sem(f"d{len(self.sems)}")
        self._wait(e, self._deps(reads, writes))
        for (o, i) in pairs:
            inst = self.engs[e].dma_start(out=o, in_=i, **kw)
            self.cnt[sem] += 16
            inst.then_inc(self.sems[sem], 16)
        tok = (sem, self.cnt[sem])
        self._commit(tok, reads, writes)
        self.dma_toks[sem] = tok
        return tok

    def finish(self):
        for e in ("sp", "pool"):
            self._wait(e, list(self.dma_toks.values()))

    def wait_all(self, e, bufs):
        deps = []
        for b in bufs:
            if b.w is not None:
                deps.append(b.w)
            deps.extend(b.r)
        self._wait(e, deps)


def make_consts():
    C = 128
    pos = np.arange(SEQ, dtype=np.float64)
    invf = 10000.0 ** (-np.arange(0, 128, 2, dtype=np.float64) / 128.0)
    ang = pos[:, None] * invf[None, :]
    cos = np.cos(ang).reshape(16, C, 1, 64)
    sin = np.sin(ang).reshape(16, C, 1, 64)
    p = np.arange(C, dtype=np.float64)
    g = np.array(G, dtype=np.float64)
    sq = g[None, :] ** (p[:, None] + 1.0)
    sk = g[None, :] ** (C - 1.0 - p[:, None]) * (128.0 ** -0.5)
    tab = np.concatenate(
        [cos * sq[None, :, :, None], sin * sq[None, :, :, None], cos * sk[None, :, :, None], sin * sk[None, :, :, None]],
        axis=2,
    )
    tab = tab.reshape(16, C, 1024).astype(np.float32)
    j = np.arange(C)[:, None]
    i = np.arange(C)[None, :]
    caus = (i >= j).astype(np.float64)
    mask_r = np.stack([caus * g[h] ** (-float(C)) for h in range(4)], axis=1).reshape(C, 512)
    Lp = (j <= i).astype(np.float64) * (-1.0 / 16.0)
    Up = (j > i).astype(np.float64) * (-1.0 / 16.0)
    ident = np.eye(C)
    cst = np.concatenate([mask_r, caus, Lp, Up, ident], axis=1).astype(np.float32)
    return tab, cst


def build_nc(NB=4, NT=16, stop=99):
    NTOK = NB * NT * 128
    nc = bass.Bass("TRN2", target_bir_lowering=False)
    dr = lambda name, shape, kind="ExternalInput": nc.dram_tensor(name, shape, F32, kind=kind).ap()
    x_d = dr("x", [NTOK, D])
    c_d = dr("c", [NB, D])
    ng_d = dr("norm_gain", [1, D])
    wada_d = dr("w_ada", [D, 3 * D])
    bada_d = dr("b_ada", [1, 3 * D])
    win_d = dr("w_in", [D, DIN])
    wg_d = dr("w_gate_up", [16, 256])
    bg_d = dr("b_gate_up", [1, 256])
    gng_d = dr("gla_norm_gain", [1, 128])
    wout_d = dr("w_out", [D, D])
    fg_d = dr("final_gain", [1, D])
    tab_d = dr("tab", [16, 128, 1024])
    cst_d = dr("cst", [128, 1024])
    out_d = dr("out", [NTOK, D], kind="ExternalOutput")

    with ExitStack() as st:
        kb = KB(nc, st)
        _n = [0]

        def sb(shape, dt, name=None):
            _n[0] += 1
            t = st.enter_context(nc.sbuf_tensor("S_" + (name or f"sb{_n[0]}"), shape, dt))
            return t, Buf(name or f"sb{_n[0]}")

        def ps(shape, dt, name):
            t = st.enter_context(nc.psum_tensor(name, shape, dt))
            return t, Buf(name)

        V, S_, P_, T_ = nc.vector, nc.scalar, nc.gpsimd, nc.tensor

        w_in_bf, b_win = sb([128, 8, DIN], BF16, "w_in_bf")
        w_out_bf, b_wout = sb([128, 8, D], BF16, "w_out_bf")
        cst, b_cst = sb([128, 1024], F32, "cst")
        ident_bf, b_idbf = sb([128, 128], BF16, "ident_bf")
        mask_r = cst[:, 0:512]
        mask_g = cst[:, 512:640]
        Lp = cst[:, 640:768]
        Up = cst[:, 768:896]
        identf = cst[:, 896:1024]
        xs = [sb([128, D], F32, f"xs{i}") for i in range(2)]
        tabs = [sb([128, 1024], F32, f"tabs{i}") for i in range(2)]
        stage = [sb([128, 8, 128], F32, f"stage{i}") for i in range(2)]
        xn, b_xn = sb([128, D], BF16, "xn")
        junk, b_junk = sb([128, D], BF16, "junk")
        hT = [sb([128, 8, 128], BF16, f"hT{i}") for i in range(2)]
        rA, b_rA = sb([128, 512], F32, "rA")
        rB, b_rB = sb([128, 512], F32, "rB")
        q_r, b_qr = sb([128, 512], BF16, "q_r")
        k_r, b_kr = sb([128, 512], BF16, "k_r")
        v_bf, b_v = sb([128, 512], BF16, "v_bf")
        gv_bf, b_gv = sb([128, 512], BF16, "gv_bf")
        szr, b_szr = sb([128, 512], F32, "szr")
        szg, b_szg = sb([128, 512], F32, "szg")
        qkT, b_qkT = sb([128, 8, 128], BF16, "qkT")
        geT, b_geT = sb([128, 6, 128], BF16, "geT")
        q_e, b_qe = sb([128, 256], BF16, "q_e")
        k_e, b_ke = sb([128, 256], BF16, "k_e")
        k_l, b_kl = sb([128, 256], BF16, "k_l")
        e_sb, b_e = sb([128, 256], F32, "e_sb")
        l_sb, b_l = sb([128, 256], F32, "l_sb")
        E1, b_E1 = sb([128, 256], F32, "E1")
        E2, b_E2 = sb([128, 256], F32, "E2")
        E3, b_E3 = sb([128, 256], F32, "E3")
        a_fm, b_afm = sb([128, 2], F32, "a_fm")
        glrT, b_glrT = sb([32, 128], BF16, "glrT")
        wg_aug, b_wg = sb([32, 256], BF16, "wg_aug")
        ST_r, b_STr = sb([128, 512], BF16, "ST_r")
        ST_g, b_STg = sb([128, 512], BF16, "ST_g")
        R, b_R = sb([128, 4, 128], F32, "R")
        R_bf, b_Rbf = sb([128, 4, 128], BF16, "R_bf")
        Sg, b_Sg = sb([128, 2, 128], F32, "Sg")
        Sg_bf, b_Sgbf = sb([128, 2, 128], BF16, "Sg_bf")
        mix, b_mix = sb([128, D], BF16, "mix")
        mixT, b_mixT = sb([128, 8, 128], BF16, "mixT")
        res, b_res = sb([128, D], F32, "res")
        outs = [sb([128, D], F32, f"outs{i}") for i in range(2)]
        gate_bc, b_gbc = sb([128, D], F32, "gate_bc")
        fg_bc, b_fgbc = sb([128, D], F32, "fg_bc")
        grep_, b_grep = sb([128, 8, 128], F32, "grep")
        stats, b_stats = sb([128, 16], F32, "stats")
        msq, b_msq = sb([128, 16], F32, "msq")
        rstd, b_rstd = sb([128, 16], F32, "rstd")
        nhalf, b_nhalf = sb([128, 16], F32, "nhalf")
        m16, b_m16 = sb([128, 2], F32, "m16")
        cT, b_cT = sb([128, 8, NB], F32, "cT")
        ng_fm, b_ngfm = sb([128, 8], F32, "ng_fm")
        bada_fm, b_badafm = sb([128, 24], F32, "bada_fm")
        gng_fm, b_gngfm = sb([128, 1], F32, "gng_fm")
        ada_fm, b_ada = sb([128, 24, NB], F32, "ada_fm")
        A_fm, b_Afm = sb([128, 8, NB], F32, "A_fm")

        big = [ps([128, 512], F32, f"pbig{i}") for i in range(6)]
        pT, b_pT = ps([128, 8, 128], BF16, "pT")
        pM, b_pM = ps([128, 512], F32, "pM")
        _rr = [0]

        def bank():
            t = big[_rr[0] % len(big)]
            _rr[0] += 1
            return t

        for s in ["ld_x0", "ld_x1", "ld_t0", "ld_t1", "ld_s0", "ld_s1", "st0", "st1"]:
            kb.new_sem(s)

        kb.dma("sp", None, [(cst[:], cst_d[:, :])], writes=[b_cst])
        kb.dma("sp", None, [(cT[:, :, b], c_d[b:b + 1, :].rearrange("o (k p) -> p (o k)", p=128)) for b in range(NB)],
               writes=[b_cT], allow_slow_non_contiguous=True)
        kb.dma("sp", None, [(ng_fm[:], ng_d.rearrange("o (k p) -> p (o k)", p=128))], writes=[b_ngfm],
               allow_slow_non_contiguous=True)
        kb.dma("sp", None, [(bada_fm[:, j * 8:(j + 1) * 8], bada_d[:, j * 1024:(j + 1) * 1024].rearrange("o (k p) -> p (o k)", p=128))
                              for j in range(3)], writes=[b_badafm], allow_slow_non_contiguous=True)
        kb.dma("sp", None, [(gng_fm[:], gng_d.rearrange("o (p one) -> (o p) one", one=1))], writes=[b_gngfm],
               allow_slow_non_contiguous=True)
        kb.dma("sp", None, [(fg_bc[:], fg_d.partition_broadcast(128))] if False else
               [(fg_bc[:], fg_d[0:1, :].to_broadcast([128, D]))], writes=[b_fgbc])
        kb.dma("pool", None, [(w_in_bf[:, kc, :], win_d[kc * 128:(kc + 1) * 128, :]) for kc in range(8)],
               writes=[b_win], max_dma_last_dim=4096)
        kb.dma("pool", None, [(w_out_bf[:, kc, :], wout_d[kc * 128:(kc + 1) * 128, :]) for kc in range(4)],
               writes=[b_wout], max_dma_last_dim=4096)
        kb.dma("pool", None, [(wg_aug[0:16, :], wg_d[:, :]), (wg_aug[16:17, :], bg_d[:, :])], writes=[b_wg])

        if stop == 0:
            kb.finish()
            return nc
        kb.op("dve", lambda: V.memset(nhalf[:], -0.5), writes=[b_nhalf])
        kb.op("dve", lambda: V.memset(m16[:], -1.0 / 16.0), writes=[b_m16])
        kb.op("dve", lambda: V.memset(glrT[:], 1.0), writes=[b_glrT])
        kb.op("dve", lambda: V.memset(geT[:], 0.0), writes=[b_geT])
        kb.op("dve", lambda: V.tensor_copy(out=ident_bf[:], in_=identf), reads=[b_cst], writes=[b_idbf])

        for kc in range(4, 8):
            stg, b_stg = stage[kc % 2]
            kb.dma("sp", f"ld_s{kc % 2}", [(stg[:].rearrange("p a b -> p (a b)"), wout_d[kc * 128:(kc + 1) * 128, :])], writes=[b_stg])
            kb.op("dve", lambda: V.tensor_scalar(out=w_out_bf[:, kc, :], in0=stg[:].rearrange("p a b -> p (a b)"),
                                                  scalar1=gng_fm[:, 0:1], scalar2=None, op0=ALU.mult),
                  reads=[b_stg, b_gngfm], writes=[b_wout])

        if stop == 1:
            kb.finish()
            return nc
        kb.op("act", lambda: S_.activation(out=cT[:], in_=cT[:], func=AF.Silu), reads=[b_cT], writes=[b_cT])
        for j in range(24):
            stg, b_stg = stage[j % 2]
            kb.dma("sp", f"ld_s{j % 2}", [(stg[:], wada_d[:, j * 128:(j + 1) * 128].rearrange("(k p) c -> p k c", p=128))], writes=[b_stg])

            def mm_ada():
                for kc in range(8):
                    i = T_.matmul(pM[:, 0:NB], lhsT=stg[:, kc, :], rhs=cT[:, kc, :], start=(kc == 0), stop=(kc == 7))
                return i
            kb.op("pe", mm_ada, reads=[b_stg, b_cT], writes=[b_pM])
            kb.op("dve", lambda: V.tensor_scalar(out=ada_fm[:, j, :], in0=pM[:, 0:NB], scalar1=bada_fm[:, j:j + 1], scalar2=None, op0=ALU.add),
                  reads=[b_pM, b_badafm], writes=[b_ada])
        kb.op("dve", lambda: V.tensor_scalar(out=A_fm[:], in0=ada_fm[:, 8:16, :], scalar1=1.0, scalar2=None, op0=ALU.add),
              reads=[b_ada], writes=[b_Afm])
        kb.op("dve", lambda: V.tensor_tensor(out=A_fm[:], in0=A_fm[:], in1=ng_fm[:].unsqueeze(2).to_broadcast([128, 8, NB]), op=ALU.mult),
              reads=[b_Afm, b_ngfm], writes=[b_Afm])

        if stop == 2:
            kb.finish()
            return nc
        cd = [G[h] ** 128.0 for h in range(4)]
        LN_EIGHTH = math.log(0.125)

        def rsqrt_cols(c0, c1, inv_n):
            kb.op("dve", lambda: V.tensor_scalar(out=msq[:, c0:c1], in0=stats[:, c0:c1], scalar1=inv_n, scalar2=EPS, op0=ALU.mult, op1=ALU.add),
                  reads=[b_stats], writes=[b_msq])
            kb.op("pool", lambda: P_.tensor_tensor(out=rstd[:, c0:c1], in0=msq[:, c0:c1], in1=nhalf[:, c0:c1], op=ALU.pow),
                  reads=[b_msq, b_nhalf], writes=[b_rstd])

        def load_tile(g):
            t = g % NT
            xt, b_x = xs[g % 2]
            tt, b_t = tabs[g % 2]
            kb.dma("sp", f"ld_x{g % 2}", [(xt[:], x_d[g * 128:(g + 1) * 128, :])], writes=[b_x])
            kb.dma("sp", f"ld_t{g % 2}", [(tt[:], tab_d[t, :, :])], writes=[b_t])

        def seq_start(b):
            kb.op("dve", lambda: V.memset(R[:], 0.0), writes=[b_R])
            kb.op("dve", lambda: V.memset(R_bf[:], 0.0), writes=[b_Rbf])
            kb.op("dve", lambda: V.memset(Sg[:], 0.0), writes=[b_Sg])
            kb.op("dve", lambda: V.memset(Sg_bf[:], 0.0), writes=[b_Sgbf])
            kb.op("dve", lambda: V.tensor_copy(out=grep_[:], in_=ada_fm[:, 16:24, b:b + 1].to_broadcast([128, 8, 128])),
                  reads=[b_ada], writes=[b_grep])
            for n in range(2):
                pb, b_pb = bank()

                def mm_g():
                    for q in range(4):
                        kc = n * 4 + q
                        i = T_.matmul(pb[:, q * 128:(q + 1) * 128], lhsT=grep_[:, kc, :], rhs=identf, start=True, stop=True)
                    return i
                kb.op("pe", mm_g, reads=[b_grep, b_cst], writes=[b_pb])
                kb.op("act", lambda: S_.copy(out=gate_bc[:, n * 512:(n + 1) * 512], in_=pb[:]), reads=[b_pb], writes=[b_gbc])

        def rotary(pb, b_pb, tt, b_t, toff, dst, b_dst):
            src4 = pb[:].rearrange("p (h two f) -> p h two f", h=4, two=2)
            cs4 = tt[:, toff:toff + 256].rearrange("p (h f) -> p h f", h=4).unsqueeze(2).to_broadcast([128, 4, 2, 64])
            sn4 = tt[:, toff + 256:toff + 512].rearrange("p (h f) -> p h f", h=4).unsqueeze(2).to_broadcast([128, 4, 2, 64])
            A4 = rA[:].rearrange("p (h two f) -> p h two f", h=4, two=2)
            B4 = rB[:].rearrange("p (h two f) -> p h two f", h=4, two=2)
            d4 = dst[:].rearrange("p (h two f) -> p h two f", h=4, two=2)
            kb.op("dve", lambda: V.tensor_tensor(out=A4, in0=src4, in1=cs4, op=ALU.mult), reads=[b_pb, b_t], writes=[b_rA])
            kb.op("dve", lambda: V.tensor_tensor(out=B4, in0=src4, in1=sn4, op=ALU.mult), reads=[b_pb, b_t], writes=[b_rB])
            kb.op("dve", lambda: V.tensor_tensor(out=d4[:, :, 0, :], in0=A4[:, :, 0, :], in1=B4[:, :, 1, :], op=ALU.subtract),
                  reads=[b_rA, b_rB], writes=[b_dst])
            kb.op("dve", lambda: V.tensor_tensor(out=d4[:, :, 1, :], in0=B4[:, :, 0, :], in1=A4[:, :, 1, :], op=ALU.add),
                  reads=[b_rA, b_rB], writes=[b_dst])

        def inproj_group(hTt, b_hT, c0, ncols):
            pb, b_pb = bank()

            def mm():
                for kc in range(8):
                    i = T_.matmul(pb[:, 0:ncols], lhsT=hTt[:, kc, :], rhs=w_in_bf[:, kc, c0:c0 + ncols], start=(kc == 0), stop=(kc == 7))
                return i
            kb.op("pe", mm, reads=[b_hT, b_win], writes=[b_pb])
            return pb, b_pb

        def tile_body(g):
            b = g // NT
            t = g % NT
            xt, b_x = xs[g % 2]
            tt, b_t = tabs[g % 2]
            hTt, b_hT = hT[g % 2]
            ot, b_o = outs[g % 2]
            if t == 0:
                seq_start(b)
            kb.op("act", lambda: S_.activation(out=junk[:], in_=xt[:], func=AF.Square, accum_out=stats[:, 0:1]),
                  reads=[b_x], writes=[b_junk, b_stats])
            rsqrt_cols(0, 1, 1.0 / D)
            kb.op("act", lambda: S_.activation(out=xn[:], in_=xt[:], func=AF.Copy, scale=rstd[:, 0:1]), reads=[b_x, b_rstd], writes=[b_xn])
            def tr_x():
                for kc in range(8):
                    i = T_.transpose(out=pT[:, kc, :], in_=xn[:, kc * 128:(kc + 1) * 128], identity=ident_bf[:])
                return i
            kb.op("pe", tr_x, reads=[b_xn, b_idbf], writes=[b_pT])
            for kc in range(8):
                kb.op("act", lambda: S_.activation(out=hTt[:, kc, :], in_=pT[:, kc, :], func=AF.Identity,
                                                    scale=A_fm[:, kc, b:b + 1], bias=ada_fm[:, kc, b:b + 1]),
                      reads=[b_pT, b_Afm, b_ada], writes=[b_hT])
            if stop == 3:
                return True
            def mm_glr():
                for kc in range(8):
                    i = T_.matmul(pM[0:16, 256:384], lhsT=w_in_bf[:, kc, 3584:3600], rhs=hTt[:, kc, :], start=(kc == 0), stop=(kc == 7))
                return i
            kb.op("pe", mm_glr, reads=[b_hT, b_win], writes=[b_pM])
            kb.op("act", lambda: S_.copy(out=glrT[0:16, :], in_=pM[0:16, 256:384]), reads=[b_pM], writes=[b_glrT])
            kb.op("pe", lambda: T_.matmul(pM[:, 0:256], lhsT=glrT[0:17, :], rhs=wg_aug[0:17, :], start=True, stop=True),
                  reads=[b_glrT, b_wg], writes=[b_pM])
            kb.op("act", lambda: S_.activation(out=e_sb[:], in_=pM[:, 0:256], func=AF.Exp, scale=-1.0), reads=[b_pM], writes=[b_e])
            kb.op("act", lambda: S_.activation(out=l_sb[:], in_=e_sb[:], func=AF.Ln, bias=1.0), reads=[b_e], writes=[b_l])
            if stop == 4:
                return True
            pq, b_pq = inproj_group(hTt, b_hT, 0, 512)
            rotary(pq, b_pq, tt, b_t, 0, q_r, b_qr)
            pk, b_pk = inproj_group(hTt, b_hT, 512, 512)
            rotary(pk, b_pk, tt, b_t, 512, k_r, b_kr)
            if stop == 5:
                return True
            pb_b, b_pbb = bank()

            def mm_b():
                T_.matmul(pb_b[:, 0:256], lhsT=Lp, rhs=l_sb[:], start=True, stop=True)
                i = T_.matmul(pb_b[:, 256:512], lhsT=Up, rhs=l_sb[:], start=True, stop=True)
                return i
            kb.op("pe", mm_b, reads=[b_l, b_cst], writes=[b_pbb])

            def mm_bl():
                T_.matmul(pM[:, 384:385], lhsT=l_sb[:, 0:128], rhs=m16[:, 0:1], start=True, stop=True)
                i = T_.matmul(pM[:, 385:386], lhsT=l_sb[:, 128:256], rhs=m16[:, 0:1], start=True, stop=True)
                return i
            kb.op("pe", mm_bl, reads=[b_l, b_m16], writes=[b_pM])
            kb.op("act", lambda: S_.activation(out=E1[:], in_=pb_b[:, 0:256], func=AF.Exp, bias=LN_EIGHTH), reads=[b_pbb], writes=[b_E1])
            kb.op("act", lambda: S_.activation(out=E2[:], in_=pb_b[:, 0:256], func=AF.Exp, scale=-1.0), reads=[b_pbb], writes=[b_E2])
            kb.op("act", lambda: S_.activation(out=E3[:], in_=pb_b[:, 256:512], func=AF.Exp), reads=[b_pbb], writes=[b_E3])
            kb.op("act", lambda: S_.activation(out=a_fm[:], in_=pM[:, 384:386], func=AF.Exp), reads=[b_pM], writes=[b_afm])
            if stop == 6:
                return True
            pg, b_pg = inproj_group(hTt, b_hT, 2048, 512)
            kb.op("dve", lambda: V.tensor_tensor(out=q_e[:], in0=pg[:, 0:256], in1=E1[:], op=ALU.mult), reads=[b_pg, b_E1], writes=[b_qe])
            kb.op("dve", lambda: V.tensor_tensor(out=k_e[:], in0=pg[:, 256:512], in1=E2[:], op=ALU.mult), reads=[b_pg, b_E2], writes=[b_ke])
            kb.op("dve", lambda: V.tensor_tensor(out=k_l[:], in0=pg[:, 256:512], in1=E3[:], op=ALU.mult), reads=[b_pg, b_E3], writes=[b_kl])
            if stop == 7:
                return True
            pv, b_pv = inproj_group(hTt, b_hT, 1024, 512)
            kb.op("act", lambda: S_.copy(out=v_bf[:], in_=pv[:]), reads=[b_pv], writes=[b_v])
            pgv, b_pgv = inproj_group(hTt, b_hT, 2560, 512)
            kb.op("act", lambda: S_.copy(out=gv_bf[:], in_=pgv[:]), reads=[b_pgv], writes=[b_gv])
            pz, b_pz = inproj_group(hTt, b_hT, 1536, 512)
            kb.op("act", lambda: S_.activation(out=szr[:], in_=pz[:], func=AF.Silu), reads=[b_pz], writes=[b_szr])
            pgz, b_pgz = inproj_group(hTt, b_hT, 3072, 512)
            kb.op("act", lambda: S_.activation(out=szg[:], in_=pgz[:], func=AF.Silu), reads=[b_pgz], writes=[b_szg])
            if stop == 8:
                return True
            def tr_qk():
                for h in range(4):
                    T_.transpose(out=pT[:, h, :], in_=q_r[:, h * 128:(h + 1) * 128], identity=ident_bf[:])
                for h in range(4):
                    i = T_.transpose(out=pT[:, 4 + h, :], in_=k_r[:, h * 128:(h + 1) * 128], identity=ident_bf[:])
                return i
            kb.op("pe", tr_qk, reads=[b_qr, b_kr, b_idbf], writes=[b_pT])
            kb.op("dve", lambda: V.tensor_copy(out=qkT[:], in_=pT[:]), reads=[b_pT], writes=[b_qkT])
            def tr_ge():
                for u in range(2):
                    T_.transpose(out=pT[:, u, :], in_=q_e[:, u * 128:(u + 1) * 128], identity=ident_bf[:])
                for u in range(2):
                    i = T_.transpose(out=pT[:, 2 + u, :], in_=k_e[:, u * 128:(u + 1) * 128], identity=ident_bf[:])
                return i
            kb.op("pe", tr_ge, reads=[b_qe, b_ke, b_idbf], writes=[b_pT])
            def ev_ge():
                for h in range(4):
                    r0 = (h % 2) * 64
                    V.tensor_copy(out=geT[r0:r0 + 64, h, :], in_=pT[r0:r0 + 64, h // 2, :])
                return V.tensor_copy(out=geT[:, 4:6, :], in_=pT[:, 2:4, :])
            kb.op("dve", ev_ge, reads=[b_pT], writes=[b_geT])
            if stop == 9:
                return True
            psr, b_psr = bank()

            def mm_sr():
                for h in range(4):
                    i = T_.matmul(psr[:, h * 128:(h + 1) * 128], lhsT=qkT[:, 4 + h, :], rhs=qkT[:, h, :], start=True, stop=True)
                return i
            kb.op("pe", mm_sr, reads=[b_qkT], writes=[b_psr])
            kb.op("dve", lambda: V.tensor_tensor(out=ST_r[:], in0=psr[:], in1=mask_r, op=ALU.mult), reads=[b_psr, b_cst], writes=[b_STr])
            psg, b_psg = bank()

            def mm_sg():
                for h in range(4):
                    r0 = (h % 2) * 64
                    i = T_.matmul(psg[:, h * 128:(h + 1) * 128], lhsT=geT[:, 4 + h // 2, :], rhs=geT[:, h, :], start=True, stop=True)
                return i
            kb.op("pe", mm_sg, reads=[b_geT], writes=[b_psg])
            kb.op("dve", lambda: V.tensor_tensor(out=ST_g[:].rearrange("p (h i) -> p h i", h=4), in0=psg[:].rearrange("p (h i) -> p h i", h=4),
                                                 in1=mask_g.unsqueeze(1).to_broadcast([128, 4, 128]), op=ALU.mult),
                  reads=[b_psg, b_cst], writes=[b_STg])
            if stop == 10:
                return True
            pyr, b_pyr = bank()

            def mm_yr():
                for h in range(4):
                    T_.matmul(pyr[:, h * 128:(h + 1) * 128], lhsT=ST_r[:, h * 128:(h + 1) * 128], rhs=v_bf[:, h * 128:(h + 1) * 128],
                              start=True, stop=False)
                    i = T_.matmul(pyr[:, h * 128:(h + 1) * 128], lhsT=qkT[:, h, :], rhs=R_bf[:, h, :], start=False, stop=True)
                return i
            kb.op("pe", mm_yr, reads=[b_STr, b_v, b_qkT, b_Rbf], writes=[b_pyr])
            pkr, b_pkr = bank()

            def mm_kr():
                for h in range(4):
                    i = T_.matmul(pkr[:, h * 128:(h + 1) * 128], lhsT=k_r[:, h * 128:(h + 1) * 128], rhs=v_bf[:, h * 128:(h + 1) * 128],
                                  start=True, stop=True)
                return i
            kb.op("pe", mm_kr, reads=[b_kr, b_v], writes=[b_pkr])
            pyg, b_pyg = bank()

            def mm_yg():
                for h in range(4):
                    r0 = (h % 2) * 64
                    T_.matmul(pyg[:, h * 128:(h + 1) * 128], lhsT=ST_g[:, h * 128:(h + 1) * 128], rhs=gv_bf[:, h * 128:(h + 1) * 128],
                              start=True, stop=False)
                    i = T_.matmul(pyg[:, h * 128:(h + 1) * 128], lhsT=geT[:, h, :], rhs=Sg_bf[:, h // 2, :], start=False, stop=True)
                return i
            kb.op("pe", mm_yg, reads=[b_STg, b_gv, b_geT, b_Sgbf], writes=[b_pyg])
            pkg, b_pkg = bank()

            def mm_kg():
                for h in range(4):
                    r0 = (h % 2) * 64
                    u = h // 2
                    i = T_.matmul(pkg[r0:r0 + 64, u * 128:(u + 1) * 128], lhsT=k_l[:, h * 64:(h + 1) * 64], rhs=gv_bf[:, h * 128:(h + 1) * 128],
                                  start=True, stop=True)
                return i
            kb.op("pe", mm_kg, reads=[b_kl, b_gv], writes=[b_pkg])
            if stop == 11:
                return True
            for h in range(4):
                kb.op("act", lambda: S_.activation(out=junk[:, 0:128], in_=pyr[:, h * 128:(h + 1) * 128], func=AF.Square, accum_out=stats[:, 1 + h:2 + h]),
                      reads=[b_pyr], writes=[b_junk, b_stats])
            for h in range(4):
                kb.op("act", lambda: S_.activation(out=junk[:, 0:128], in_=pyg[:, h * 128:(h + 1) * 128], func=AF.Square, accum_out=stats[:, 5 + h:6 + h]),
                      reads=[b_pyg], writes=[b_junk, b_stats])
            rsqrt_cols(1, 9, 1.0 / 128.0)
            for h in range(4):
                kb.op("dve", lambda: V.scalar_tensor_tensor(out=mix[:, h * 128:(h + 1) * 128], in0=pyr[:, h * 128:(h + 1) * 128], scalar=rstd[:, 1 + h:2 + h],
                                                             in1=szr[:, h * 128:(h + 1) * 128], op0=ALU.mult, op1=ALU.mult),
                      reads=[b_pyr, b_rstd, b_szr], writes=[b_mix])
            for h in range(4):
                kb.op("dve", lambda: V.scalar_tensor_tensor(out=mix[:, 512 + h * 128:512 + (h + 1) * 128], in0=pyg[:, h * 128:(h + 1) * 128],
                                                             scalar=rstd[:, 5 + h:6 + h], in1=szg[:, h * 128:(h + 1) * 128], op0=ALU.mult, op1=ALU.mult),
                      reads=[b_pyg, b_rstd, b_szg], writes=[b_mix])
            if stop == 12:
                return True
            for h in range(4):
                kb.op("dve", lambda: V.scalar_tensor_tensor(out=R[:, h, :], in0=R[:, h, :], scalar=cd[h], in1=pkr[:, h * 128:(h + 1) * 128],
                                                             op0=ALU.mult, op1=ALU.add), reads=[b_R, b_pkr], writes=[b_R])
            kb.op("act", lambda: S_.copy(out=R_bf[:], in_=R[:]), reads=[b_R], writes=[b_Rbf])
            for u in range(2):
                kb.op("dve", lambda: V.scalar_tensor_tensor(out=Sg[:, u, :], in0=Sg[:, u, :], scalar=a_fm[:, u:u + 1], in1=pkg[:, u * 128:(u + 1) * 128],
                                                             op0=ALU.mult, op1=ALU.add), reads=[b_Sg, b_pkg, b_afm], writes=[b_Sg])
            kb.op("act", lambda: S_.copy(out=Sg_bf[:], in_=Sg[:]), reads=[b_Sg], writes=[b_Sgbf])
            if stop == 13:
                return True
            def tr_m():
                for kc in range(8):
                    i = T_.transpose(out=pT[:, kc, :], in_=mix[:, kc * 128:(kc + 1) * 128], identity=ident_bf[:])
                return i
            kb.op("pe", tr_m, reads=[b_mix, b_idbf], writes=[b_pT])
            kb.op("dve", lambda: V.tensor_copy(out=mixT[:], in_=pT[:]), reads=[b_pT], writes=[b_mixT])
            for n in range(2):
                po, b_po = bank()

                def mm_o():
                    for kc in range(8):
                        i = T_.matmul(po[:], lhsT=mixT[:, kc, :], rhs=w_out_bf[:, kc, n * 512:(n + 1) * 512], start=(kc == 0), stop=(kc == 7))
                    return i
                kb.op("pe", mm_o, reads=[b_mixT, b_wout], writes=[b_po])
                sl = slice(n * 512, (n + 1) * 512)
                kb.op("dve", lambda: V.tensor_tensor(out=res[:, sl], in0=po[:], in1=gate_bc[:, sl], op=ALU.mult), reads=[b_po, b_gbc], writes=[b_res])
            kb.op("dve", lambda: V.tensor_tensor(out=res[:], in0=res[:], in1=xt[:], op=ALU.add), reads=[b_res, b_x], writes=[b_res])
            kb.op("act", lambda: S_.activation(out=junk[:], in_=res[:], func=AF.Square, accum_out=stats[:, 9:10]),
                  reads=[b_res], writes=[b_junk, b_stats])
            rsqrt_cols(9, 10, 1.0 / D)
            kb.op("dve", lambda: V.scalar_tensor_tensor(out=ot[:], in0=res[:], scalar=rstd[:, 9:10], in1=fg_bc[:], op0=ALU.mult, op1=ALU.mult),
                  reads=[b_res, b_rstd, b_fgbc], writes=[b_o])
            kb.dma("pool", f"st{g % 2}", [(out_d[g * 128:(g + 1) * 128, :], ot[:])], reads=[b_o])

        NG = NB * NT
        load_tile(0)
        for g in range(NG):
            if g + 1 < NG:
                load_tile(g + 1)
            if tile_body(g):
                break
        kb.finish()
    return nc


_CACHE = {}


def kernel(x, c, norm_gain, w_ada, b_ada, w_in, w_gate_up, b_gate_up, gla_norm_gain, w_out, final_gain):
    NB, NT = 4, 16
    f = lambda a: np.ascontiguousarray(np.asarray(a, dtype=np.float32))
    x = f(x)
    c = f(c)
    if "nc" not in _CACHE:
        _CACHE["nc"] = build_nc(NB, NT)
        _CACHE["consts"] = make_consts()
    nc = _CACHE["nc"]
    tab, cst = _CACHE["consts"]
    shared = {
        "norm_gain": f(norm_gain).reshape(1, D), "w_ada": f(w_ada).reshape(D, 3 * D), "b_ada": f(b_ada).reshape(1, 3 * D),
        "w_in": f(w_in).reshape(D, DIN), "w_gate_up": f(w_gate_up).reshape(16, 256), "b_gate_up": f(b_gate_up).reshape(1, 256),
        "gla_norm_gain": f(gla_norm_gain).reshape(1, 128), "w_out": f(w_out).reshape(D, D), "final_gain": f(final_gain).reshape(1, D),
        "tab": tab, "cst": cst,
    }
    in_maps = []
    for i in range(NCORES):
        m = dict(shared)
        m["x"] = x[i * NB:(i + 1) * NB].reshape(NB * SEQ, D)
        m["c"] = c[i * NB:(i + 1) * NB]
        in_maps.append(m)
    res = run_bass_kernel_spmd(nc, in_maps, core_ids=list(range(NCORES)))
    out = np.concatenate([r["out"].reshape(NB, SEQ, D) for r in res.results], axis=0)
    return out.astype(np.float32)
```

```python
import math
from contextlib import ExitStack

import numpy as np
import concourse.bass as bass
import concourse.mybir as mybir
from concourse.bass_utils import run_bass_kernel_spmd

F32 = mybir.dt.float32
BF16 = mybir.dt.bfloat16
AF = mybir.ActivationFunctionType
ALU = mybir.AluOpType

D = 1024
SEQ = 2048
DIN = 3600
NCORES = 8
EPS = 1e-6
G = [1.0 - 2.0 ** (-5.0 - h) for h in range(4)]
import os
SKIP_PE_SELF = os.environ.get('SKIP_PE_SELF', '0') == '1'
SEQ_ORDER = os.environ.get('SEQ_ORDER', '0') == '1'


class Buf:
    __slots__ = ("name",)

    def __init__(self, name):
        self.name = name


class Rec:
    __slots__ = ("kind", "e", "fn", "reads", "writes", "cost", "sem", "pairs", "kw", "deps", "raw", "start", "tag")


class KB:
    ENG = ("pe", "act", "dve", "pool", "sp")

    def __init__(self, nc, stack):
        self.nc = nc
        self.engs = {"pe": nc.tensor, "act": nc.scalar, "dve": nc.vector, "pool": nc.gpsimd, "sp": nc.sync}
        self.sems = {}
        self.cnt = {}
        self.stack = stack
        for e in self.engs:
            self.sems[e] = stack.enter_context(nc.semaphore("s_" + e))
            self.cnt[e] = 0
        self.seen = {e: {} for e in self.engs}
        self.prog = []
        self.tag = -1
        self.nwaits = 0

    def new_sem(self, name):
        self.sems[name] = self.stack.enter_context(self.nc.semaphore("s_" + name))
        self.cnt[name] = 0
        return name

    def op(self, e, fn, reads=(), writes=(), cost=0.3):
        r = Rec()
        r.kind, r.e, r.fn, r.reads, r.writes, r.cost = "op", e, fn, tuple(reads), tuple(writes), cost
        r.tag = self.tag
        self.prog.append(r)

    def dma(self, e, sem, pairs, reads=(), writes=(), cost=6.0, **kw):
        if sem is None:
            sem = self.new_sem(f"d{len(self.sems)}")
        r = Rec()
        r.kind, r.e, r.reads, r.writes, r.cost = "dma", e, tuple(reads), tuple(writes), cost
        r.sem, r.pairs, r.kw = sem, list(pairs), kw
        r.tag = self.tag
        self.prog.append(r)

    def _wait(self, e, toks):
        best = {}
        for (s, v) in toks:
            if v > best.get(s, 0):
                best[s] = v
        for s, v in best.items():
            if self.seen[e].get(s, 0) < v:
                self.engs[e].wait_ge(self.sems[s], v)
                self.seen[e][s] = v
                self.nwaits += 1

    def run(self):
        prog = self.prog
        N = len(prog)
        lastw, readers = {}, {}
        succ = [[] for _ in range(N)]
        indeg = [0] * N
        for i, r in enumerate(prog):
            raw, other = set(), set()
            for b in r.reads:
                if b in lastw:
                    raw.add(lastw[b])
            for b in r.writes:
                if b in lastw:
                    other.add(lastw[b])
                other.update(readers.get(b, ()))
            deps = (raw | other)
            deps.discard(i)
            r.deps, r.raw = deps, raw
            for d in deps:
                succ[d].append(i)
            indeg[i] = len(deps)
            for b in r.writes:
                lastw[b] = i
                readers[b] = []
            for b in r.reads:
                if b not in r.writes:
                    readers.setdefault(b, []).append(i)
        finish = [0.0] * N
        ready_t = [0.0] * N
        avail = {e: [] for e in self.ENG}
        free = {e: 0.0 for e in self.ENG}
        for i in range(N):
            if indeg[i] == 0:
                avail[prog[i].e].append(i)
        order = []
        while len(order) < N:
            best = None
            for e in self.ENG:
                fe = free[e]
                for i in avail[e]:
                    key = (max(ready_t[i], fe), i)
                    if best is None or key < best[0]:
                        best = (key, e, i)
            (st, _), e, i = best
            avail[e].remove(i)
            r = prog[i]
            r.start = st
            if r.kind == "dma":
                free[e] = st + 0.15 * len(r.pairs)
            else:
                free[e] = st + r.cost
            finish[i] = st + r.cost
            order.append(i)
            for s_ in succ[i]:
                lat = 0.06 if prog[s_].e == e and r.kind == "op" else 0.2
                if finish[i] + lat > ready_t[s_]:
                    ready_t[s_] = finish[i] + lat
                indeg[s_] -= 1
                if indeg[s_] == 0:
                    avail[prog[s_].e].append(s_)
        self.est_us = max(finish) if N else 0.0
        self.finish_t = finish
        self.order = order
        if SEQ_ORDER:
            order = list(range(N))
        tok = [None] * N
        dma_last = {}
        for i in order:
            r = prog[i]
            waits = []
            for d in r.deps:
                pd = prog[d]
                if pd.kind == "op" and pd.e == r.e == "pe" and SKIP_PE_SELF:
                    continue
                waits.append(tok[d])
            self._wait(r.e, waits)
            if r.kind == "op":
                inst = r.fn()
                self.cnt[r.e] += 1
                inst.then_inc(self.sems[r.e], 1)
                tok[i] = (r.e, self.cnt[r.e])
            else:
                for (o, a) in r.pairs:
                    inst = self.engs[r.e].dma_start(out=o, in_=a, **r.kw)
                    self.cnt[r.sem] += 16
                    inst.then_inc(self.sems[r.sem], 16)
                tok[i] = (r.sem, self.cnt[r.sem])
                dma_last[r.sem] = tok[i]
        for e in ("sp", "pool"):
            self._wait(e, list(dma_last.values()))


def make_consts():
    C = 128
    pos = np.arange(SEQ, dtype=np.float64)
    invf = 10000.0 ** (-np.arange(0, 128, 2, dtype=np.float64) / 128.0)
    ang = pos[:, None] * invf[None, :]
    cos = np.cos(ang).reshape(16, C, 1, 64)
    sin = np.sin(ang).reshape(16, C, 1, 64)
    p = np.arange(C, dtype=np.float64)
    g = np.array(G, dtype=np.float64)
    sq = g[None, :] ** (p[:, None] + 1.0)
    sk = g[None, :] ** (C - 1.0 - p[:, None]) * (128.0 ** -0.5)
    tab = np.concatenate(
        [cos * sq[None, :, :, None], sin * sq[None, :, :, None], cos * sk[None, :, :, None], sin * sk[None, :, :, None]],
        axis=2,
    )
    tab = tab.reshape(16, C, 1024).astype(np.float32)
    j = np.arange(C)[:, None]
    i = np.arange(C)[None, :]
    caus = (i >= j).astype(np.float64)
    mask_r = np.stack([caus * g[h] ** (-float(C)) for h in range(4)], axis=1).reshape(C, 512)
    Lp = (j <= i).astype(np.float64) * (-1.0 / 16.0)
    Up = (j > i).astype(np.float64) * (-1.0 / 16.0)
    ident = np.eye(C)
    cst = np.concatenate([mask_r, caus, Lp, Up, ident], axis=1).astype(np.float32)
    return tab, cst


def build_nc(NB=4, NT=16):
    NTOK = NB * NT * 128
    nc = bass.Bass("TRN2", target_bir_lowering=False)
    dr = lambda name, shape, kind="ExternalInput": nc.dram_tensor(name, shape, F32, kind=kind).ap()
    x_d = dr("x", [NTOK, D])
    c_d = dr("c", [NB, D])
    ng_d = dr("norm_gain", [1, D])
    wada_d = dr("w_ada", [D, 3 * D])
    bada_d = dr("b_ada", [1, 3 * D])
    win_d = dr("w_in", [D, DIN])
    wg_d = dr("w_gate_up", [16, 256])
    bg_d = dr("b_gate_up", [1, 256])
    gng_d = dr("gla_norm_gain", [1, 128])
    wout_d = dr("w_out", [D, D])
    fg_d = dr("final_gain", [1, D])
    tab_d = dr("tab", [16, 128, 1024])
    cst_d = dr("cst", [128, 1024])
    out_d = dr("out", [NTOK, D], kind="ExternalOutput")

    with ExitStack() as st:
        kb = KB(nc, st)
        _n = [0]

        def sb(shape, dt, name):
            t = st.enter_context(nc.sbuf_tensor("S_" + name, shape, dt))
            return t, Buf(name)

        def sbn(n, shape, dt, name):
            return [sb(shape, dt, f"{name}{i}") for i in range(n)]

        def ps(shape, dt, name):
            t = st.enter_context(nc.psum_tensor(name, shape, dt))
            return t, Buf(name)

        V, S_, P_, T_ = nc.vector, nc.scalar, nc.gpsimd, nc.tensor

        def dve(fn, n, reads, writes, mode=1.0):
            kb.op("dve", fn, reads, writes, cost=(n / mode + 150.0) / 960.0)

        def act(fn, n, reads, writes):
            kb.op("act", fn, reads, writes, cost=(n + 260.0) / 1200.0)

        def pe(fn, cols, reads, writes):
            kb.op("pe", fn, reads, writes, cost=cols / 2000.0 + 0.06)

        def pool(fn, cost, reads, writes):
            kb.op("pool", fn, reads, writes, cost=cost)

        w_in_bf, b_win = sb([128, 8, DIN], BF16, "w_in_bf")
        w_out_bf, b_wout = sb([128, 8, D], BF16, "w_out_bf")
        cst, b_cst = sb([128, 1024], F32, "cst")
        ident_bf, b_idbf = sb([128, 128], BF16, "ident_bf")
        mask_r = cst[:, 0:512]
        mask_g = cst[:, 512:640]
        Lp = cst[:, 640:768]
        Up = cst[:, 768:896]
        identf = cst[:, 896:1024]
        xs = sbn(3, [128, D], F32, "xs")
        tabs = sbn(2, [128, 1024], F32, "tabs")
        outs = sbn(2, [128, D], F32, "outs")
        xn_ = sbn(2, [128, D], BF16, "xn")
        hT = sbn(2, [128, 8, 128], BF16, "hT")
        rA, b_rA = sb([128, 512], F32, "rA")
        rB, b_rB = sb([128, 512], F32, "rB")
        q_r_ = sbn(2, [128, 512], BF16, "q_r")
        k_r_ = sbn(2, [128, 512], BF16, "k_r")
        v_bf_ = sbn(2, [128, 512], BF16, "v_bf")
        gv_bf_ = sbn(2, [128, 512], BF16, "gv_bf")
        szr_ = sbn(2, [128, 512], F32, "szr")
        szg_ = sbn(2, [128, 512], F32, "szg")
        qkT_ = sbn(2, [128, 8, 128], BF16, "qkT")
        geT_ = sbn(2, [128, 6, 128], BF16, "geT")
        q_e_ = sbn(2, [128, 256], BF16, "q_e")
        k_e_ = sbn(2, [128, 256], BF16, "k_e")
        k_l_ = sbn(2, [128, 256], BF16, "k_l")
        e_sb, b_e = sb([128, 256], F32, "e_sb")
        l_sb_ = sbn(2, [128, 256], F32, "l_sb")
        E1_ = sbn(2, [128, 256], F32, "E1")
        E2_ = sbn(2, [128, 256], F32, "E2")
        E3_ = sbn(2, [128, 256], F32, "E3")
        a_fm_ = sbn(2, [128, 2], F32, "a_fm")
        glrT_ = sbn(2, [32, 128], BF16, "glrT")
        wg_aug, b_wg = sb([32, 256], BF16, "wg_aug")
        ST_r_ = sbn(2, [128, 512], BF16, "ST_r")
        ST_g_ = sbn(2, [128, 512], BF16, "ST_g")
        R, b_R = sb([128, 4, 128], F32, "R")
        R_bf, b_Rbf = sb([128, 4, 128], BF16, "R_bf")
        Sg, b_Sg = sb([128, 2, 128], F32, "Sg")
        Sg_bf, b_Sgbf = sb([128, 2, 128], BF16, "Sg_bf")
        mix_ = sbn(2, [128, D], BF16, "mix")
        mixT_ = sbn(2, [128, 8, 128], BF16, "mixT")
        res_ = sbn(2, [128, D], F32, "res")
        gate_bc, b_gbc = sb([128, D], F32, "gate_bc")
        fg_bc, b_fgbc = sb([128, D], F32, "fg_bc")
        grep_, b_grep = sb([128, 8, 128], F32, "grep")
        stats_ = [st.enter_context(nc.sbuf_tensor(f"S_stats{i}", [128, 16], F32)) for i in range(2)]
        msq_ = [st.enter_context(nc.sbuf_tensor(f"S_msq{i}", [128, 16], F32)) for i in range(2)]
        rstd_ = [st.enter_context(nc.sbuf_tensor(f"S_rstd{i}", [128, 16], F32)) for i in range(2)]
        b_stats = [{k: Buf(f"stats{i}{k}") for k in "xyf"} for i in range(2)]
        b_msq = [{k: Buf(f"msq{i}{k}") for k in "xyf"} for i in range(2)]
        b_rstd = [{k: Buf(f"rstd{i}{k}") for k in "xyf"} for i in range(2)]
        nhalf, b_nhalf = sb([128, 16], F32, "nhalf")
        m16, b_m16 = sb([128, 2], F32, "m16")
        cT, b_cT = sb([128, 8, NB], F32, "cT")
        ng_fm, b_ngfm = sb([128, 8], F32, "ng_fm")
        bada_fm, b_badafm = sb([128, 24], F32, "bada_fm")
        gng_fm, b_gngfm = sb([128, 1], F32, "gng_fm")
        ada_fm, b_ada = sb([128, 24, NB], F32, "ada_fm")
        A_fm, b_Afm = sb([128, 8, NB], F32, "A_fm")

        headb = [ps([128, 512], F32, f"phead{i}") for i in range(2)]
        tailb = [ps([128, 512], F32, f"ptail{i}") for i in range(3)]
        pT_ = [ps([128, 8, 128], BF16, f"pT{i}") for i in range(2)]
        pM, b_pM = ps([128, 512], F32, "pM")
        _rr = {"h": 0, "t": 0}

        def bank(kind="t"):
            pool_ = headb if kind == "h" else tailb
            t = pool_[_rr[kind] % len(pool_)]
            _rr[kind] += 1
            return t

        for s in ["ld_x0", "ld_x1", "ld_x2", "ld_t0", "ld_t1", "ld_s0", "ld_s1", "st0", "st1"]:
            kb.new_sem(s)

        kb.dma("sp", None, [(cst[:], cst_d[:, :])], writes=[b_cst])
        kb.dma("sp", None, [(cT[:, :, b], c_d[b:b + 1, :].rearrange("o (k p) -> p (o k)", p=128)) for b in range(NB)],
               writes=[b_cT], allow_slow_non_contiguous=True)
        kb.dma("sp", None, [(ng_fm[:], ng_d.rearrange("o (k p) -> p (o k)", p=128))], writes=[b_ngfm],
               allow_slow_non_contiguous=True)
        kb.dma("sp", None, [(bada_fm[:, j * 8:(j + 1) * 8], bada_d[:, j * 1024:(j + 1) * 1024].rearrange("o (k p) -> p (o k)", p=128))
                            for j in range(3)], writes=[b_badafm], allow_slow_non_contiguous=True)
        kb.dma("sp", None, [(gng_fm[:], gng_d.rearrange("o (p one) -> (o p) one", one=1))], writes=[b_gngfm],
               allow_slow_non_contiguous=True)
        kb.dma("sp", None, [(fg_bc[:], fg_d[0:1, :].to_broadcast([128, D]))], writes=[b_fgbc])
        kb.dma("pool", None, [(w_in_bf[:, kc, :], win_d[kc * 128:(kc + 1) * 128, :]) for kc in range(8)],
               writes=[b_win], cost=90.0, max_dma_last_dim=4096)
        kb.dma("pool", None, [(w_out_bf[:, kc, :], wout_d[kc * 128:(kc + 1) * 128, :]) for kc in range(4)],
               writes=[b_wout], cost=20.0, max_dma_last_dim=4096)
        kb.dma("pool", None, [(wg_aug[0:16, :], wg_d[:, :]), (wg_aug[16:17, :], bg_d[:, :])], writes=[b_wg])

        dve(lambda: V.memset(nhalf[:], -0.5), 16, [], [b_nhalf])
        dve(lambda: V.memset(m16[:], -1.0 / 16.0), 2, [], [b_m16])
        for i in range(2):
            dve(lambda i=i: V.memset(glrT_[i][0][:], 1.0), 128, [], [glrT_[i][1]])
            dve(lambda i=i: V.memset(geT_[i][0][:], 0.0), 768, [], [geT_[i][1]])
        dve(lambda: V.tensor_copy(out=ident_bf[:], in_=identf), 128, [b_cst], [b_idbf])

        for kc in range(4, 8):
            stg, b_stg = outs[kc % 2]
            kb.dma("sp", f"ld_s{kc % 2}", [(stg[:], wout_d[kc * 128:(kc + 1) * 128, :])], writes=[b_stg])
            dve(lambda kc=kc, stg=stg: V.tensor_scalar(out=w_out_bf[:, kc, :], in0=stg[:], scalar1=gng_fm[:, 0:1], scalar2=None, op0=ALU.mult),
                1024, [b_stg, b_gngfm], [b_wout])

        act(lambda: S_.activation(out=cT[:], in_=cT[:], func=AF.Silu), 8 * NB, [b_cT], [b_cT])
        for j in range(24):
            stg, b_stg = outs[j % 2]
            stg3 = stg[:].rearrange("p (k c) -> p k c", k=8)
            kb.dma("sp", f"ld_s{j % 2}", [(stg3, wada_d[:, j * 128:(j + 1) * 128].rearrange("(k p) c -> p k c", p=128))], writes=[b_stg])

            def mm_ada(stg3=stg3):
                for kc in range(8):
                    i = T_.matmul(pM[:, 0:NB], lhsT=stg3[:, kc, :], rhs=cT[:, kc, :], start=(kc == 0), stop=(kc == 7))
                return i
            pe(mm_ada, 8 * 4 * 128, [b_stg, b_cT], [b_pM])
            dve(lambda j=j: V.tensor_scalar(out=ada_fm[:, j, :], in0=pM[:, 0:NB], scalar1=bada_fm[:, j:j + 1], scalar2=None, op0=ALU.add),
                NB, [b_pM, b_badafm], [b_ada])
        dve(lambda: V.tensor_scalar(out=A_fm[:], in0=ada_fm[:, 8:16, :], scalar1=1.0, scalar2=None, op0=ALU.add), 8 * NB, [b_ada], [b_Afm])
        dve(lambda: V.tensor_tensor(out=A_fm[:], in0=A_fm[:], in1=ng_fm[:].unsqueeze(2).to_broadcast([128, 8, NB]), op=ALU.mult),
            8 * NB, [b_Afm, b_ngfm], [b_Afm])

        cd = [G[h] ** 128.0 for h in range(4)]
        LN_EIGHTH = math.log(0.125)

        def rsqrt_cols(par, grp, c0, c1, inv_n):
            stt, msq, rstd = stats_[par], msq_[par], rstd_[par]
            dve(lambda: V.tensor_scalar(out=msq[:, c0:c1], in0=stt[:, c0:c1], scalar1=inv_n, scalar2=EPS, op0=ALU.mult, op1=ALU.add),
                c1 - c0, [b_stats[par][grp]], [b_msq[par][grp]])
            pool(lambda: P_.tensor_tensor(out=rstd[:, c0:c1], in0=msq[:, c0:c1], in1=nhalf[:, c0:c1], op=ALU.pow),
                 0.9, [b_msq[par][grp], b_nhalf], [b_rstd[par][grp]])

        def load_tile(g):
            t = g % NT
            xt, b_x = xs[g % 3]
            tt, b_t = tabs[g % 2]
            kb.dma("sp", f"ld_x{g % 3}", [(xt[:], x_d[g * 128:(g + 1) * 128, :])], writes=[b_x])
            kb.dma("sp", f"ld_t{g % 2}", [(tt[:], tab_d[t, :, :])], writes=[b_t])

        def seq_start(b):
            dve(lambda: V.memset(R[:], 0.0), 512, [], [b_R])
            dve(lambda: V.memset(R_bf[:], 0.0), 512, [], [b_Rbf], mode=2.0)
            dve(lambda: V.memset(Sg[:], 0.0), 256, [], [b_Sg])
            dve(lambda: V.memset(Sg_bf[:], 0.0), 256, [], [b_Sgbf], mode=2.0)
            dve(lambda: V.tensor_copy(out=grep_[:], in_=ada_fm[:, 16:24, b:b + 1].to_broadcast([128, 8, 128])), 1024, [b_ada], [b_grep])
            for n in range(2):
                pb, b_pb = tailb[1 + n]

                def mm_g(n=n, pb=pb):
                    for q in range(4):
                        kc = n * 4 + q
                        i = T_.matmul(pb[:, q * 128:(q + 1) * 128], lhsT=grep_[:, kc, :], rhs=identf, start=True, stop=True)
                    return i
                pe(mm_g, 4 * 4 * 128, [b_grep, b_cst], [b_pb])
                act(lambda n=n, pb=pb: S_.copy(out=gate_bc[:, n * 512:(n + 1) * 512], in_=pb[:]), 512, [b_pb], [b_gbc])

        def rotary(pb, b_pb, tt, b_t, toff, dst, b_dst):
            src4 = pb[:].rearrange("p (h two f) -> p h two f", h=4, two=2)
            cs4 = tt[:, toff:toff + 256].rearrange("p (h f) -> p h f", h=4).unsqueeze(2).to_broadcast([128, 4, 2, 64])
            sn4 = tt[:, toff + 256:toff + 512].rearrange("p (h f) -> p h f", h=4).unsqueeze(2).to_broadcast([128, 4, 2, 64])
            A4 = rA[:].rearrange("p (h two f) -> p h two f", h=4, two=2)
            B4 = rB[:].rearrange("p (h two f) -> p h two f", h=4, two=2)
            d4 = dst[:].rearrange("p (h two f) -> p h two f", h=4, two=2)
            dve(lambda: V.tensor_tensor(out=A4, in0=src4, in1=cs4, op=ALU.mult), 512, [b_pb, b_t], [b_rA])
            dve(lambda: V.tensor_tensor(out=B4, in0=src4, in1=sn4, op=ALU.mult), 512, [b_pb, b_t], [b_rB])
            dve(lambda: V.tensor_tensor(out=d4[:, :, 0, :], in0=A4[:, :, 0, :], in1=B4[:, :, 1, :], op=ALU.subtract), 256, [b_rA, b_rB], [b_dst])
            dve(lambda: V.tensor_tensor(out=d4[:, :, 1, :], in0=B4[:, :, 0, :], in1=A4[:, :, 1, :], op=ALU.add), 256, [b_rA, b_rB], [b_dst])

        def inproj_group(hTt, b_hT, c0, ncols):
            pb, b_pb = bank("h")

            def mm():
                for kc in range(8):
                    i = T_.matmul(pb[:, 0:ncols], lhsT=hTt[:, kc, :], rhs=w_in_bf[:, kc, c0:c0 + ncols], start=(kc == 0), stop=(kc == 7))
                return i
            pe(mm, 8 * ncols, [b_hT, b_win], [b_pb])
            return pb, b_pb

        def tile_body(g):
            b = g // NT
            t = g % NT
            par = g % 2
            xt, b_x = xs[g % 3]
            tt, b_t = tabs[par]
            hTt, b_hT = hT[par]
            ot, b_o = outs[par]
            xn, b_xn = xn_[par]
            q_r, b_qr = q_r_[par]
            k_r, b_kr = k_r_[par]
            v_bf, b_v = v_bf_[par]
            gv_bf, b_gv = gv_bf_[par]
            szr, b_szr = szr_[par]
            szg, b_szg = szg_[par]
            qkT, b_qkT = qkT_[par]
            geT, b_geT = geT_[par]
            q_e, b_qe = q_e_[par]
            k_e, b_ke = k_e_[par]
            k_l, b_kl = k_l_[par]
            l_sb, b_l = l_sb_[par]
            E1, b_E1 = E1_[par]
            E2, b_E2 = E2_[par]
            E3, b_E3 = E3_[par]
            a_fm, b_afm = a_fm_[par]
            glrT, b_glrT = glrT_[par]
            ST_r, b_STr = ST_r_[par]
            ST_g, b_STg = ST_g_[par]
            mix, b_mix = mix_[par]
            mixT, b_mixT = mixT_[par]
            res, b_res = res_[par]
            stats, rstd = stats_[par], rstd_[par]
            bs, br = b_stats[par], b_rstd[par]
            pT, b_pT = pT_[par]
            if t == 0:
                seq_start(b)
            act(lambda: S_.activation(out=xn[:], in_=xt[:], func=AF.Square, accum_out=stats[:, 0:1]), 1024, [b_x], [bs["x"], b_xn])
            rsqrt_cols(par, "x", 0, 1, 1.0 / D)
            act(lambda: S_.activation(out=xn[:], in_=xt[:], func=AF.Copy, scale=rstd[:, 0:1]), 1024, [b_x, br["x"]], [b_xn])
            def tr_x():
                for kc in range(8):
                    i = T_.transpose(out=pT[:, kc, :], in_=xn[:, kc * 128:(kc + 1) * 128], identity=ident_bf[:])
                return i
            pe(tr_x, 1024, [b_xn, b_idbf], [b_pT])

            def ev_h():
                for kc in range(8):
                    i = S_.activation(out=hTt[:, kc, :], in_=pT[:, kc, :], func=AF.Identity,
                                      scale=A_fm[:, kc, b:b + 1], bias=ada_fm[:, kc, b:b + 1])
                return i
            kb.op("act", ev_h, [b_pT, b_Afm, b_ada], [b_hT], cost=8 * 0.46)
            def mm_glr():
                for kc in range(8):
                    i = T_.matmul(pM[0:16, 256:384], lhsT=w_in_bf[:, kc, 3584:3600], rhs=hTt[:, kc, :], start=(kc == 0), stop=(kc == 7))
                return i
            pe(mm_glr, 1024, [b_hT, b_win], [b_pM])
            act(lambda: S_.copy(out=glrT[0:16, :], in_=pM[0:16, 256:384]), 128, [b_pM], [b_glrT])
            pe(lambda: T_.matmul(pM[:, 0:256], lhsT=glrT[0:17, :], rhs=wg_aug[0:17, :], start=True, stop=True), 256, [b_glrT, b_wg], [b_pM])
            act(lambda: S_.activation(out=e_sb[:], in_=pM[:, 0:256], func=AF.Exp, scale=-1.0), 256, [b_pM], [b_e])
            act(lambda: S_.activation(out=l_sb[:], in_=e_sb[:], func=AF.Ln, bias=1.0), 256, [b_e], [b_l])
            pq, b_pq = inproj_group(hTt, b_hT, 0, 512)
            rotary(pq, b_pq, tt, b_t, 0, q_r, b_qr)
            pk, b_pk = inproj_group(hTt, b_hT, 512, 512)
            rotary(pk, b_pk, tt, b_t, 512, k_r, b_kr)
            pb_b, b_pbb = bank("h")

            pe(lambda: T_.matmul(pb_b[:, 0:256], lhsT=Lp, rhs=l_sb[:], start=True, stop=True), 1024, [b_l, b_cst], [b_pbb])

            def mm_bl():
                T_.matmul(pM[:, 384:385], lhsT=l_sb[:, 0:128], rhs=m16[:, 0:1], start=True, stop=True)
                i = T_.matmul(pM[:, 385:386], lhsT=l_sb[:, 128:256], rhs=m16[:, 0:1], start=True, stop=True)
                return i
            pe(mm_bl, 512, [b_l, b_m16], [b_pM])
            act(lambda: S_.activation(out=E1[:], in_=pb_b[:, 0:256], func=AF.Exp, bias=LN_EIGHTH), 256, [b_pbb], [b_E1])
            act(lambda: S_.activation(out=E2[:], in_=pb_b[:, 0:256], func=AF.Exp, scale=-1.0), 256, [b_pbb], [b_E2])
            act(lambda: S_.activation(out=a_fm[:], in_=pM[:, 384:386], func=AF.Exp), 2, [b_pM], [b_afm])
            pg, b_pg = inproj_group(hTt, b_hT, 2048, 512)
            dve(lambda: V.tensor_tensor(out=q_e[:], in0=pg[:, 0:256], in1=E1[:], op=ALU.mult), 256, [b_pg, b_E1], [b_qe])
            dve(lambda: V.tensor_tensor(out=k_e[:], in0=pg[:, 256:512], in1=E2[:], op=ALU.mult), 256, [b_pg, b_E2], [b_ke])
            pv, b_pv = inproj_group(hTt, b_hT, 1024, 512)
            act(lambda: S_.copy(out=v_bf[:], in_=pv[:]), 512, [b_pv], [b_v])
            pgv, b_pgv = inproj_group(hTt, b_hT, 2560, 512)
            act(lambda: S_.copy(out=gv_bf[:], in_=pgv[:]), 512, [b_pgv], [b_gv])
            pz, b_pz = inproj_group(hTt, b_hT, 1536, 512)
            act(lambda: S_.activation(out=szr[:], in_=pz[:], func=AF.Silu), 512, [b_pz], [b_szr])
            pgz, b_pgz = inproj_group(hTt, b_hT, 3072, 512)
            act(lambda: S_.activation(out=szg[:], in_=pgz[:], func=AF.Silu), 512, [b_pgz], [b_szg])
            def tr_qk():
                for h in range(4):
                    T_.transpose(out=pT[:, h, :], in_=q_r[:, h * 128:(h + 1) * 128], identity=ident_bf[:])
                for h in range(4):
                    i = T_.transpose(out=pT[:, 4 + h, :], in_=k_r[:, h * 128:(h + 1) * 128], identity=ident_bf[:])
                return i
            pe(tr_qk, 1024, [b_qr, b_kr, b_idbf], [b_pT])
            dve(lambda: V.tensor_copy(out=qkT[:], in_=pT[:]), 1024, [b_pT], [b_qkT], mode=2.0)
            def tr_ge():
                for u in range(2):
                    T_.transpose(out=pT[:, u, :], in_=q_e[:, u * 128:(u + 1) * 128], identity=ident_bf[:])
                for u in range(2):
                    i = T_.transpose(out=pT[:, 2 + u, :], in_=k_e[:, u * 128:(u + 1) * 128], identity=ident_bf[:])
                return i
            pe(tr_ge, 512, [b_qe, b_ke, b_idbf], [b_pT])

            def ev_ge():
                for h in range(4):
                    r0 = (h % 2) * 64
                    V.tensor_copy(out=geT[r0:r0 + 64, h, :], in_=pT[r0:r0 + 64, h // 2, :])
                return V.tensor_copy(out=geT[:, 4:6, :], in_=pT[:, 2:4, :])
            kb.op("dve", ev_ge, [b_pT], [b_geT], cost=5 * 0.25)
            (T0, bT0), (T1, bT1), (T2, bT2) = tailb
            psr, b_psr = T0, bT0
            psg, b_psg = T1, bT1
            pyr, b_pyr = T2, bT2
            pkr, b_pkr = T0, bT0
            pyg, b_pyg = T1, bT1
            pkg, b_pkg = T0, bT0

            def mm_sr():
                for h in range(4):
                    i = T_.matmul(psr[:, h * 128:(h + 1) * 128], lhsT=qkT[:, 4 + h, :], rhs=qkT[:, h, :], start=True, stop=True)
                return i
            pe(mm_sr, 512, [b_qkT], [b_psr])
            dve(lambda: V.tensor_tensor(out=ST_r[:], in0=psr[:], in1=mask_r, op=ALU.mult), 512, [b_psr, b_cst], [b_STr])

            def mm_sg():
                for h in range(4):
                    i = T_.matmul(psg[:, h * 128:(h + 1) * 128], lhsT=geT[:, 4 + h // 2, :], rhs=geT[:, h, :], start=True, stop=True)
                return i
            pe(mm_sg, 512, [b_geT], [b_psg])
            dve(lambda: V.tensor_tensor(out=ST_g[:].rearrange("p (h i) -> p h i", h=4), in0=psg[:].rearrange("p (h i) -> p h i", h=4),
                                        in1=mask_g.unsqueeze(1).to_broadcast([128, 4, 128]), op=ALU.mult), 512, [b_psg, b_cst], [b_STg])

            def mm_yr():
                for h in range(4):
                    T_.matmul(pyr[:, h * 128:(h + 1) * 128], lhsT=ST_r[:, h * 128:(h + 1) * 128], rhs=v_bf[:, h * 128:(h + 1) * 128],
                              start=True, stop=False)
                    i = T_.matmul(pyr[:, h * 128:(h + 1) * 128], lhsT=qkT[:, h, :], rhs=R_bf[:, h, :], start=False, stop=True)
                return i
            pe(mm_yr, 1024, [b_STr, b_v, b_qkT, b_Rbf], [b_pyr])

            def mm_kr():
                for h in range(4):
                    i = T_.matmul(pkr[:, h * 128:(h + 1) * 128], lhsT=k_r[:, h * 128:(h + 1) * 128], rhs=v_bf[:, h * 128:(h + 1) * 128],
                                  start=True, stop=True)
                return i
            pe(mm_kr, 512, [b_kr, b_v], [b_pkr])

            def up_R():
                for h in range(4):
                    i = V.scalar_tensor_tensor(out=R[:, h, :], in0=R[:, h, :], scalar=cd[h], in1=pkr[:, h * 128:(h + 1) * 128],
                                               op0=ALU.mult, op1=ALU.add)
                return i
            kb.op("dve", up_R, [b_pkr], [b_R], cost=4 * 0.3)
            act(lambda: S_.copy(out=R_bf[:], in_=R[:]), 512, [b_R], [b_Rbf])

            def mm_yg():
                for h in range(4):
                    T_.matmul(pyg[:, h * 128:(h + 1) * 128], lhsT=ST_g[:, h * 128:(h + 1) * 128], rhs=gv_bf[:, h * 128:(h + 1) * 128],
                              start=True, stop=False)
                    i = T_.matmul(pyg[:, h * 128:(h + 1) * 128], lhsT=geT[:, h, :], rhs=Sg_bf[:, h // 2, :], start=False, stop=True)
                return i
            pe(mm_yg, 1024, [b_STg, b_gv, b_geT, b_Sgbf], [b_pyg])

            def mm_kg():
                for h in range(4):
                    r0 = (h % 2) * 64
                    u = h // 2
                    i = T_.matmul(pkg[r0:r0 + 64, u * 128:(u + 1) * 128], lhsT=k_e[:, h * 64:(h + 1) * 64], rhs=gv_bf[:, h * 128:(h + 1) * 128],
                                  start=True, stop=True)
                return i
            pe(mm_kg, 512, [b_ke, b_gv], [b_pkg])
            Sg2 = Sg[:].rearrange("p u e -> p (u e)")
            dve(lambda: V.tensor_tensor(out=Sg2, in0=Sg2, in1=pkg[:, 0:256], op=ALU.add), 256, [b_pkg], [b_Sg])
            dve(lambda: V.tensor_tensor(out=Sg[:], in0=Sg[:], in1=a_fm[:].unsqueeze(2).to_broadcast([128, 2, 128]), op=ALU.mult), 256, [b_afm], [b_Sg])
            act(lambda: S_.copy(out=Sg_bf[:], in_=Sg[:]), 256, [b_Sg], [b_Sgbf])
            def sq_y():
                for h in range(4):
                    S_.activation(out=mix[:, h * 128:(h + 1) * 128], in_=pyr[:, h * 128:(h + 1) * 128], func=AF.Square, accum_out=stats[:, 1 + h:2 + h])
                for h in range(4):
                    i = S_.activation(out=mix[:, 512 + h * 128:512 + (h + 1) * 128], in_=pyg[:, h * 128:(h + 1) * 128], func=AF.Square, accum_out=stats[:, 5 + h:6 + h])
                return i
            kb.op("act", sq_y, [b_pyr, b_pyg], [bs["y"], b_mix], cost=8 * 0.42)
            rsqrt_cols(par, "y", 1, 9, 1.0 / 128.0)

            def mk_mix():
                for h in range(4):
                    V.scalar_tensor_tensor(out=mix[:, h * 128:(h + 1) * 128], in0=pyr[:, h * 128:(h + 1) * 128], scalar=rstd[:, 1 + h:2 + h],
                                           in1=szr[:, h * 128:(h + 1) * 128], op0=ALU.mult, op1=ALU.mult)
                for h in range(4):
                    i = V.scalar_tensor_tensor(out=mix[:, 512 + h * 128:512 + (h + 1) * 128], in0=pyg[:, h * 128:(h + 1) * 128],
                                               scalar=rstd[:, 5 + h:6 + h], in1=szg[:, h * 128:(h + 1) * 128], op0=ALU.mult, op1=ALU.mult)
                return i
            kb.op("dve", mk_mix, [b_pyr, b_pyg, br["y"], b_szr, b_szg], [b_mix], cost=8 * 0.3)
            def tr_m():
                for kc in range(8):
                    i = T_.transpose(out=pT[:, kc, :], in_=mix[:, kc * 128:(kc + 1) * 128], identity=ident_bf[:])
                return i
            pe(tr_m, 1024, [b_mix, b_idbf], [b_pT])
            dve(lambda: V.tensor_copy(out=mixT[:], in_=pT[:]), 1024, [b_pT], [b_mixT], mode=2.0)
            for n in range(2):
                po, b_po = tailb[0] if n == 0 else tailb[2]

                def mm_o(n=n, po=po):
                    for kc in range(8):
                        i = T_.matmul(po[:], lhsT=mixT[:, kc, :], rhs=w_out_bf[:, kc, n * 512:(n + 1) * 512], start=(kc == 0), stop=(kc == 7))
                    return i
                pe(mm_o, 8 * 512, [b_mixT, b_wout], [b_po])
                sl = slice(n * 512, (n + 1) * 512)
                dve(lambda po=po, sl=sl: V.tensor_tensor(out=res[:, sl], in0=po[:], in1=gate_bc[:, sl], op=ALU.mult), 512, [b_po, b_gbc], [b_res])
            dve(lambda: V.tensor_tensor(out=res[:], in0=res[:], in1=xt[:], op=ALU.add), 1024, [b_res, b_x], [b_res])
            act(lambda: S_.activation(out=ot[:], in_=res[:], func=AF.Square, accum_out=stats[:, 9:10]), 1024, [b_res], [bs["f"], b_o])
            rsqrt_cols(par, "f", 9, 10, 1.0 / D)
            dve(lambda: V.scalar_tensor_tensor(out=ot[:], in0=res[:], scalar=rstd[:, 9:10], in1=fg_bc[:], op0=ALU.mult, op1=ALU.mult),
                1024, [b_res, br["f"], b_fgbc], [b_o])
            kb.dma("pool", f"st{par}", [(out_d[g * 128:(g + 1) * 128, :], ot[:])], reads=[b_o])

        NG = NB * NT
        for g in range(NG):
            kb.tag = g
            load_tile(g)
            tile_body(g)
        kb.run()
        build_nc.last_est_us = kb.est_us
        build_nc.last_kb = kb
    return nc


_CACHE = {}


def kernel(x, c, norm_gain, w_ada, b_ada, w_in, w_gate_up, b_gate_up, gla_norm_gain, w_out, final_gain):
    NB, NT = 4, 16
    f = lambda a: np.ascontiguousarray(np.asarray(a, dtype=np.float32))
    x = f(x)
    c = f(c)
    if "nc" not in _CACHE:
        _CACHE["nc"] = build_nc(NB, NT)
        _CACHE["consts"] = make_consts()
    nc = _CACHE["nc"]
    tab, cst = _CACHE["consts"]
    shared = {
        "norm_gain": f(norm_gain).reshape(1, D), "w_ada": f(w_ada).reshape(D, 3 * D), "b_ada": f(b_ada).reshape(1, 3 * D),
        "w_in": f(w_in).reshape(D, DIN), "w_gate_up": f(w_gate_up).reshape(16, 256), "b_gate_up": f(b_gate_up).reshape(1, 256),
        "gla_norm_gain": f(gla_norm_gain).reshape(1, 128), "w_out": f(w_out).reshape(D, D), "final_gain": f(final_gain).reshape(1, D),
        "tab": tab, "cst": cst,
    }
    in_maps = []
    for i in range(NCORES):
        m = dict(shared)
        m["x"] = x[i * NB:(i + 1) * NB].reshape(NB * SEQ, D)
        m["c"] = c[i * NB:(i + 1) * NB]
        in_maps.append(m)
    res = run_bass_kernel_spmd(nc, in_maps, core_ids=list(range(NCORES)))
    out = np.concatenate([r["out"].reshape(NB, SEQ, D) for r in res.results], axis=0)
    return out.astype(np.float32)
```

```python
import math
from contextlib import ExitStack

import numpy as np
import concourse.bass as bass
import concourse.mybir as mybir
from concourse.bass_utils import run_bass_kernel_spmd

F32 = mybir.dt.float32
BF16 = mybir.dt.bfloat16
AF = mybir.ActivationFunctionType
ALU = mybir.AluOpType

D = 1024
SEQ = 2048
DIN = 3600
NCORES = 8
EPS = 1e-6
G = [1.0 - 2.0 ** (-5.0 - h) for h in range(4)]
import os
SKIP_PE_SELF = os.environ.get('SKIP_PE_SELF', '0') == '1'
SEQ_ORDER = os.environ.get('SEQ_ORDER', '0') == '1'


class Buf:
    __slots__ = ("name",)

    def __init__(self, name):
        self.name = name


class Rec:
    __slots__ = ("kind", "e", "fn", "reads", "writes", "cost", "sem", "pairs", "kw", "deps", "raw", "start", "tag")


class KB:
    ENG = ("pe", "act", "dve", "pool", "sp")

    def __init__(self, nc, stack):
        self.nc = nc
        self.engs = {"pe": nc.tensor, "act": nc.scalar, "dve": nc.vector, "pool": nc.gpsimd, "sp": nc.sync}
        self.sems = {}
        self.cnt = {}
        self.stack = stack
        for e in self.engs:
            self.sems[e] = stack.enter_context(nc.semaphore("s_" + e))
            self.cnt[e] = 0
        self.seen = {e: {} for e in self.engs}
        self.prog = []
        self.tag = -1
        self.nwaits = 0

    def new_sem(self, name):
        self.sems[name] = self.stack.enter_context(self.nc.semaphore("s_" + name))
        self.cnt[name] = 0
        return name

    def op(self, e, fn, reads=(), writes=(), cost=0.3):
        r = Rec()
        r.kind, r.e, r.fn, r.reads, r.writes, r.cost = "op", e, fn, tuple(reads), tuple(writes), cost
        r.tag = self.tag
        self.prog.append(r)

    def dma(self, e, sem, pairs, reads=(), writes=(), cost=6.0, **kw):
        if sem is None:
            sem = self.new_sem(f"d{len(self.sems)}")
        r = Rec()
        r.kind, r.e, r.reads, r.writes, r.cost = "dma", e, tuple(reads), tuple(writes), cost
        r.sem, r.pairs, r.kw = sem, list(pairs), kw
        r.tag = self.tag
        self.prog.append(r)

    def _wait(self, e, toks):
        best = {}
        for (s, v) in toks:
            if v > best.get(s, 0):
                best[s] = v
        for s, v in best.items():
            if self.seen[e].get(s, 0) < v:
                self.engs[e].wait_ge(self.sems[s], v)
                self.seen[e][s] = v
                self.nwaits += 1

    def run(self):
        prog = self.prog
        N = len(prog)
        lastw, readers = {}, {}
        succ = [[] for _ in range(N)]
        indeg = [0] * N
        for i, r in enumerate(prog):
            raw, other = set(), set()
            for b in r.reads:
                if b in lastw:
                    raw.add(lastw[b])
            for b in r.writes:
                if b in lastw:
                    other.add(lastw[b])
                other.update(readers.get(b, ()))
            deps = (raw | other)
            deps.discard(i)
            r.deps, r.raw = deps, raw
            for d in deps:
                succ[d].append(i)
            indeg[i] = len(deps)
            for b in r.writes:
                lastw[b] = i
                readers[b] = []
            for b in r.reads:
                if b not in r.writes:
                    readers.setdefault(b, []).append(i)
        finish = [0.0] * N
        ready_t = [0.0] * N
        avail = {e: [] for e in self.ENG}
        free = {e: 0.0 for e in self.ENG}
        for i in range(N):
            if indeg[i] == 0:
                avail[prog[i].e].append(i)
        order = []
        while len(order) < N:
            best = None
            for e in self.ENG:
                fe = free[e]
                for i in avail[e]:
                    key = (max(ready_t[i], fe), i)
                    if best is None or key < best[0]:
                        best = (key, e, i)
            (st, _), e, i = best
            avail[e].remove(i)
            r = prog[i]
            r.start = st
            if r.kind == "dma":
                free[e] = st + 0.15 * len(r.pairs)
            else:
                free[e] = st + r.cost
            finish[i] = st + r.cost
            order.append(i)
            for s_ in succ[i]:
                lat = 0.06 if prog[s_].e == e and r.kind == "op" else 0.2
                if finish[i] + lat > ready_t[s_]:
                    ready_t[s_] = finish[i] + lat
                indeg[s_] -= 1
                if indeg[s_] == 0:
                    avail[prog[s_].e].append(s_)
        self.est_us = max(finish) if N else 0.0
        self.finish_t = finish
        self.order = order
        if SEQ_ORDER:
            order = list(range(N))
        tok = [None] * N
        dma_last = {}
        for i in order:
            r = prog[i]
            waits = []
            for d in r.deps:
                pd = prog[d]
                if pd.kind == "op" and pd.e == r.e == "pe" and SKIP_PE_SELF:
                    continue
                waits.append(tok[d])
            self._wait(r.e, waits)
            if r.kind == "op":
                inst = r.fn()
                self.cnt[r.e] += 1
                inst.then_inc(self.sems[r.e], 1)
                tok[i] = (r.e, self.cnt[r.e])
            else:
                for (o, a) in r.pairs:
                    inst = self.engs[r.e].dma_start(out=o, in_=a, **r.kw)
                    self.cnt[r.sem] += 16
                    inst.then_inc(self.sems[r.sem], 16)
                tok[i] = (r.sem, self.cnt[r.sem])
                dma_last[r.sem] = tok[i]
        for e in ("sp", "pool"):
            self._wait(e, list(dma_last.values()))


def make_consts():
    C = 128
    pos = np.arange(SEQ, dtype=np.float64)
    invf = 10000.0 ** (-np.arange(0, 128, 2, dtype=np.float64) / 128.0)
    ang = pos[:, None] * invf[None, :]
    cos = np.cos(ang).reshape(16, C, 1, 64)
    sin = np.sin(ang).reshape(16, C, 1, 64)
    p = np.arange(C, dtype=np.float64)
    g = np.array(G, dtype=np.float64)
    sq = g[None, :] ** (p[:, None] + 1.0)
    sk = g[None, :] ** (C - 1.0 - p[:, None]) * (128.0 ** -0.5)
    tab = np.concatenate(
        [cos * sq[None, :, :, None], sin * sq[None, :, :, None], cos * sk[None, :, :, None], sin * sk[None, :, :, None]],
        axis=2,
    )
    tab = tab.reshape(16, C, 1024).astype(np.float32)
    j = np.arange(C)[:, None]
    i = np.arange(C)[None, :]
    caus = (i >= j).astype(np.float64)
    mask_r = np.stack([caus * g[h] ** (-float(C)) for h in range(4)], axis=1).reshape(C, 512)
    Lp = (j <= i).astype(np.float64) * (-1.0 / 16.0)
    Up = (j > i).astype(np.float64) * (-1.0 / 16.0)
    ident = np.eye(C)
    cst = np.concatenate([mask_r, caus, Lp, Up, ident], axis=1).astype(np.float32)
    return tab, cst


def build_nc(NB=4, NT=16):
    NTOK = NB * NT * 128
    nc = bass.Bass("TRN2", target_bir_lowering=False)
    dr = lambda name, shape, kind="ExternalInput": nc.dram_tensor(name, shape, F32, kind=kind).ap()
    x_d = dr("x", [NTOK, D])
    c_d = dr("c", [NB, D])
    ng_d = dr("norm_gain", [1, D])
    wada_d = dr("w_ada", [D, 3 * D])
    bada_d = dr("b_ada", [1, 3 * D])
    win_d = dr("w_in", [D, DIN])
    wg_d = dr("w_gate_up", [16, 256])
    bg_d = dr("b_gate_up", [1, 256])
    gng_d = dr("gla_norm_gain", [1, 128])
    wout_d = dr("w_out", [D, D])
    fg_d = dr("final_gain", [1, D])
    tab_d = dr("tab", [16, 128, 1024])
    cst_d = dr("cst", [128, 1024])
    out_d = dr("out", [NTOK, D], kind="ExternalOutput")

    with ExitStack() as st:
        kb = KB(nc, st)
        _n = [0]

        def sb(shape, dt, name):
            t = st.enter_context(nc.sbuf_tensor("S_" + name, shape, dt))
            return t, Buf(name)

        def sbn(n, shape, dt, name):
            return [sb(shape, dt, f"{name}{i}") for i in range(n)]

        def ps(shape, dt, name):
            t = st.enter_context(nc.psum_tensor(name, shape, dt))
            return t, Buf(name)

        V, S_, P_, T_ = nc.vector, nc.scalar, nc.gpsimd, nc.tensor

        def dve(fn, n, reads, writes, mode=1.0):
            kb.op("dve", fn, reads, writes, cost=(n / mode + 150.0) / 960.0)

        def act(fn, n, reads, writes):
            kb.op("act", fn, reads, writes, cost=(n + 260.0) / 1200.0)

        def pe(fn, cols, reads, writes):
            kb.op("pe", fn, reads, writes, cost=cols / 2000.0 + 0.06)

        def pool(fn, cost, reads, writes):
            kb.op("pool", fn, reads, writes, cost=cost)

        w_in_bf, b_win = sb([128, 8, DIN], BF16, "w_in_bf")
        w_out_bf, b_wout = sb([128, 8, D], BF16, "w_out_bf")
        cst, b_cst = sb([128, 1024], F32, "cst")
        ident_bf, b_idbf = sb([128, 128], BF16, "ident_bf")
        mask_r = cst[:, 0:512]
        mask_g = cst[:, 512:640]
        Lp = cst[:, 640:768]
        Up = cst[:, 768:896]
        identf = cst[:, 896:1024]
        xs = sbn(3, [128, D], F32, "xs")
        tabs = sbn(2, [128, 1024], F32, "tabs")
        outs = sbn(2, [128, D], F32, "outs")
        xn_ = sbn(2, [128, D], BF16, "xn")
        hT = sbn(2, [128, 8, 128], BF16, "hT")
        rA, b_rA = sb([128, 512], F32, "rA")
        rB, b_rB = sb([128, 512], F32, "rB")
        q_r_ = sbn(2, [128, 512], BF16, "q_r")
        k_r_ = sbn(2, [128, 512], BF16, "k_r")
        v_bf_ = sbn(2, [128, 512], BF16, "v_bf")
        gv_bf_ = sbn(2, [128, 512], BF16, "gv_bf")
        szr_ = sbn(2, [128, 512], F32, "szr")
        szg_ = sbn(2, [128, 512], F32, "szg")
        qkT_ = sbn(2, [128, 8, 128], BF16, "qkT")
        geT_ = sbn(2, [128, 6, 128], BF16, "geT")
        q_e_ = sbn(2, [128, 256], BF16, "q_e")
        k_e_ = sbn(2, [128, 256], BF16, "k_e")
        k_l_ = sbn(2, [128, 256], BF16, "k_l")
        e_sb, b_e = sb([128, 256], F32, "e_sb")
        l_sb_ = sbn(2, [128, 256], F32, "l_sb")
        E1_ = sbn(2, [128, 256], F32, "E1")
        E2_ = sbn(2, [128, 256], F32, "E2")
        E3_ = sbn(2, [128, 256], F32, "E3")
        a_fm_ = sbn(2, [128, 2], F32, "a_fm")
        glrT_ = sbn(2, [32, 128], BF16, "glrT")
        wg_aug, b_wg = sb([32, 256], BF16, "wg_aug")
        ST_r_ = sbn(2, [128, 512], BF16, "ST_r")
        ST_g_ = sbn(2, [128, 512], BF16, "ST_g")
        R, b_R = sb([128, 4, 128], F32, "R")
        R_bf, b_Rbf = sb([128, 4, 128], BF16, "R_bf")
        Sg, b_Sg = sb([128, 2, 128], F32, "Sg")
        Sg_bf, b_Sgbf = sb([128, 2, 128], BF16, "Sg_bf")
        mix_ = sbn(2, [128, D], BF16, "mix")
        mixT_ = sbn(2, [128, 8, 128], BF16, "mixT")
        res_ = sbn(2, [128, D], F32, "res")
        gate_bc, b_gbc = sb([128, D], F32, "gate_bc")
        fg_bc, b_fgbc = sb([128, D], F32, "fg_bc")
        grep_, b_grep = sb([128, 8, 128], F32, "grep")
        stats_ = [st.enter_context(nc.sbuf_tensor(f"S_stats{i}", [128, 16], F32)) for i in range(2)]
        msq_ = [st.enter_context(nc.sbuf_tensor(f"S_msq{i}", [128, 16], F32)) for i in range(2)]
        rstd_ = [st.enter_context(nc.sbuf_tensor(f"S_rstd{i}", [128, 16], F32)) for i in range(2)]
        b_stats = [{k: Buf(f"stats{i}{k}") for k in "xyf"} for i in range(2)]
        b_msq = [{k: Buf(f"msq{i}{k}") for k in "xyf"} for i in range(2)]
        b_rstd = [{k: Buf(f"rstd{i}{k}") for k in "xyf"} for i in range(2)]
        eps_t, b_eps = sb([128, 1], F32, "eps_t")
        m16, b_m16 = sb([128, 2], F32, "m16")
        cT, b_cT = sb([128, 8, NB], F32, "cT")
        ng_fm, b_ngfm = sb([128, 8], F32, "ng_fm")
        bada_fm, b_badafm = sb([128, 24], F32, "bada_fm")
        gng_fm, b_gngfm = sb([128, 1], F32, "gng_fm")
        ada_fm, b_ada = sb([128, 24, NB], F32, "ada_fm")
        A_fm, b_Afm = sb([128, 8, NB], F32, "A_fm")

        headb = [ps([128, 512], F32, f"phead{i}") for i in range(2)]
        tailb = [ps([128, 512], F32, f"ptail{i}") for i in range(3)]
        pT_ = [ps([128, 8, 128], BF16, f"pT{i}") for i in range(2)]
        pM, b_pM = ps([128, 512], F32, "pM")
        _rr = {"h": 0, "t": 0}

        def bank(kind="t"):
            pool_ = headb if kind == "h" else tailb
            t = pool_[_rr[kind] % len(pool_)]
            _rr[kind] += 1
            return t

        for s in ["ld_x0", "ld_x1", "ld_x2", "ld_t0", "ld_t1", "ld_s0", "ld_s1", "st0", "st1"]:
            kb.new_sem(s)

        kb.dma("sp", None, [(cst[:], cst_d[:, :])], writes=[b_cst])
        kb.dma("sp", None, [(cT[:, :, b], c_d[b:b + 1, :].rearrange("o (k p) -> p (o k)", p=128)) for b in range(NB)],
               writes=[b_cT], allow_slow_non_contiguous=True)
        kb.dma("sp", None, [(ng_fm[:], ng_d.rearrange("o (k p) -> p (o k)", p=128))], writes=[b_ngfm],
               allow_slow_non_contiguous=True)
        kb.dma("sp", None, [(bada_fm[:, j * 8:(j + 1) * 8], bada_d[:, j * 1024:(j + 1) * 1024].rearrange("o (k p) -> p (o k)", p=128))
                            for j in range(3)], writes=[b_badafm], allow_slow_non_contiguous=True)
        kb.dma("sp", None, [(gng_fm[:], gng_d.rearrange("o (p one) -> (o p) one", one=1))], writes=[b_gngfm],
               allow_slow_non_contiguous=True)
        kb.dma("sp", None, [(fg_bc[:], fg_d[0:1, :].to_broadcast([128, D]))], writes=[b_fgbc])
        kb.dma("pool", None, [(w_in_bf[:, kc, :], win_d[kc * 128:(kc + 1) * 128, :]) for kc in range(8)],
               writes=[b_win], cost=90.0, max_dma_last_dim=4096)
        kb.dma("pool", None, [(w_out_bf[:, kc, :], wout_d[kc * 128:(kc + 1) * 128, :]) for kc in range(4)],
               writes=[b_wout], cost=20.0, max_dma_last_dim=4096)
        kb.dma("pool", None, [(wg_aug[0:16, :], wg_d[:, :]), (wg_aug[16:17, :], bg_d[:, :])], writes=[b_wg])

        dve(lambda: V.memset(eps_t[:], EPS), 1, [], [b_eps])
        dve(lambda: V.memset(m16[:], -1.0 / 16.0), 2, [], [b_m16])
        for i in range(2):
            dve(lambda i=i: V.memset(glrT_[i][0][:], 1.0), 128, [], [glrT_[i][1]])
            dve(lambda i=i: V.memset(geT_[i][0][:], 0.0), 768, [], [geT_[i][1]])
        dve(lambda: V.tensor_copy(out=ident_bf[:], in_=identf), 128, [b_cst], [b_idbf])

        for kc in range(4, 8):
            stg, b_stg = outs[kc % 2]
            kb.dma("sp", f"ld_s{kc % 2}", [(stg[:], wout_d[kc * 128:(kc + 1) * 128, :])], writes=[b_stg])
            dve(lambda kc=kc, stg=stg: V.tensor_scalar(out=w_out_bf[:, kc, :], in0=stg[:], scalar1=gng_fm[:, 0:1], scalar2=None, op0=ALU.mult),
                1024, [b_stg, b_gngfm], [b_wout])

        act(lambda: S_.activation(out=cT[:], in_=cT[:], func=AF.Silu), 8 * NB, [b_cT], [b_cT])
        for j in range(24):
            stg, b_stg = outs[j % 2]
            stg3 = stg[:].rearrange("p (k c) -> p k c", k=8)
            kb.dma("sp", f"ld_s{j % 2}", [(stg3, wada_d[:, j * 128:(j + 1) * 128].rearrange("(k p) c -> p k c", p=128))], writes=[b_stg])

            def mm_ada(stg3=stg3):
                for kc in range(8):
                    i = T_.matmul(pM[:, 0:NB], lhsT=stg3[:, kc, :], rhs=cT[:, kc, :], start=(kc == 0), stop=(kc == 7))
                return i
            pe(mm_ada, 8 * 4 * 128, [b_stg, b_cT], [b_pM])
            dve(lambda j=j: V.tensor_scalar(out=ada_fm[:, j, :], in0=pM[:, 0:NB], scalar1=bada_fm[:, j:j + 1], scalar2=None, op0=ALU.add),
                NB, [b_pM, b_badafm], [b_ada])
        dve(lambda: V.tensor_scalar(out=A_fm[:], in0=ada_fm[:, 8:16, :], scalar1=1.0, scalar2=None, op0=ALU.add), 8 * NB, [b_ada], [b_Afm])
        dve(lambda: V.tensor_tensor(out=A_fm[:], in0=A_fm[:], in1=ng_fm[:].unsqueeze(2).to_broadcast([128, 8, NB]), op=ALU.mult),
            8 * NB, [b_Afm, b_ngfm], [b_Afm])

        cd = [G[h] ** 128.0 for h in range(4)]
        LN_EIGHTH = math.log(0.125)

        def rsqrt_cols(par, grp, c0, c1, inv_n):
            stt, msq, rstd = stats_[par], msq_[par], rstd_[par]

            act(lambda: S_.activation(out=msq[:, c0:c1], in_=stt[:, c0:c1], func=AF.Ln, scale=inv_n, bias=eps_t[:, 0:1]),
                c1 - c0, [b_stats[par][grp], b_eps], [b_msq[par][grp]])
            act(lambda: S_.activation(out=rstd[:, c0:c1], in_=msq[:, c0:c1], func=AF.Exp, scale=-0.5),
                c1 - c0, [b_msq[par][grp]], [b_rstd[par][grp]])

        def load_tile(g):
            t = g % NT
            xt, b_x = xs[g % 3]
            tt, b_t = tabs[g % 2]
            kb.dma("sp", f"ld_x{g % 3}", [(xt[:], x_d[g * 128:(g + 1) * 128, :])], writes=[b_x])
            kb.dma("sp", f"ld_t{g % 2}", [(tt[:], tab_d[t, :, :])], writes=[b_t])

        def seq_start(b):
            dve(lambda: V.memset(R[:], 0.0), 512, [], [b_R])
            dve(lambda: V.memset(R_bf[:], 0.0), 512, [], [b_Rbf], mode=2.0)
            dve(lambda: V.memset(Sg[:], 0.0), 256, [], [b_Sg])
            dve(lambda: V.memset(Sg_bf[:], 0.0), 256, [], [b_Sgbf], mode=2.0)
            dve(lambda: V.tensor_copy(out=grep_[:], in_=ada_fm[:, 16:24, b:b + 1].to_broadcast([128, 8, 128])), 1024, [b_ada], [b_grep])
            for n in range(2):
                pb, b_pb = tailb[1 + n]

                def mm_g(n=n, pb=pb):
                    for q in range(4):
                        kc = n * 4 + q
                        i = T_.matmul(pb[:, q * 128:(q + 1) * 128], lhsT=grep_[:, kc, :], rhs=identf, start=True, stop=True)
                    return i
                pe(mm_g, 4 * 4 * 128, [b_grep, b_cst], [b_pb])
                act(lambda n=n, pb=pb: S_.copy(out=gate_bc[:, n * 512:(n + 1) * 512], in_=pb[:]), 512, [b_pb], [b_gbc])

        def rotary(pb, b_pb, tt, b_t, toff, dst, b_dst):
            src4 = pb[:].rearrange("p (h two f) -> p h two f", h=4, two=2)
            cs4 = tt[:, toff:toff + 256].rearrange("p (h f) -> p h f", h=4).unsqueeze(2).to_broadcast([128, 4, 2, 64])
            sn4 = tt[:, toff + 256:toff + 512].rearrange("p (h f) -> p h f", h=4).unsqueeze(2).to_broadcast([128, 4, 2, 64])
            A4 = rA[:].rearrange("p (h two f) -> p h two f", h=4, two=2)
            B4 = rB[:].rearrange("p (h two f) -> p h two f", h=4, two=2)
            d4 = dst[:].rearrange("p (h two f) -> p h two f", h=4, two=2)
            dve(lambda: V.tensor_tensor(out=A4, in0=src4, in1=cs4, op=ALU.mult), 512, [b_pb, b_t], [b_rA])
            dve(lambda: V.tensor_tensor(out=B4, in0=src4, in1=sn4, op=ALU.mult), 512, [b_pb, b_t], [b_rB])
            dve(lambda: V.tensor_tensor(out=d4[:, :, 0, :], in0=A4[:, :, 0, :], in1=B4[:, :, 1, :], op=ALU.subtract), 256, [b_rA, b_rB], [b_dst])
            dve(lambda: V.tensor_tensor(out=d4[:, :, 1, :], in0=B4[:, :, 0, :], in1=A4[:, :, 1, :], op=ALU.add), 256, [b_rA, b_rB], [b_dst])

        def inproj_group(hTt, b_hT, c0, ncols):
            pb, b_pb = bank("h")

            def mm():
                for kc in range(8):
                    i = T_.matmul(pb[:, 0:ncols], lhsT=hTt[:, kc, :], rhs=w_in_bf[:, kc, c0:c0 + ncols], start=(kc == 0), stop=(kc == 7))
                return i
            pe(mm, 8 * ncols, [b_hT, b_win], [b_pb])
            return pb, b_pb

        def tile_body(g):
            b = g // NT
            t = g % NT
            par = g % 2
            xt, b_x = xs[g % 3]
            tt, b_t = tabs[par]
            hTt, b_hT = hT[par]
            ot, b_o = outs[par]
            xn, b_xn = xn_[par]
            q_r, b_qr = q_r_[par]
            k_r, b_kr = k_r_[par]
            v_bf, b_v = v_bf_[par]
            gv_bf, b_gv = gv_bf_[par]
            szr, b_szr = szr_[par]
            szg, b_szg = szg_[par]
            qkT, b_qkT = qkT_[par]
            geT, b_geT = geT_[par]
            q_e, b_qe = q_e_[par]
            k_e, b_ke = k_e_[par]
            k_l, b_kl = k_l_[par]
            l_sb, b_l = l_sb_[par]
            E1, b_E1 = E1_[par]
            E2, b_E2 = E2_[par]
            E3, b_E3 = E3_[par]
            a_fm, b_afm = a_fm_[par]
            glrT, b_glrT = glrT_[par]
            ST_r, b_STr = ST_r_[par]
            ST_g, b_STg = ST_g_[par]
            mix, b_mix = mix_[par]
            mixT, b_mixT = mixT_[par]
            res, b_res = res_[par]
            stats, rstd = stats_[par], rstd_[par]
            bs, br = b_stats[par], b_rstd[par]
            pT, b_pT = pT_[par]
            if t == 0:
                seq_start(b)
            act(lambda: S_.activation(out=xn[:], in_=xt[:], func=AF.Square, accum_out=stats[:, 0:1]), 1024, [b_x], [bs["x"], b_xn])
            rsqrt_cols(par, "x", 0, 1, 1.0 / D)
            act(lambda: S_.activation(out=xn[:], in_=xt[:], func=AF.Copy, scale=rstd[:, 0:1]), 1024, [b_x, br["x"]], [b_xn])
            def tr_x():
                for kc in range(8):
                    i = T_.transpose(out=pT[:, kc, :], in_=xn[:, kc * 128:(kc + 1) * 128], identity=ident_bf[:])
                return i
            pe(tr_x, 1024, [b_xn, b_idbf], [b_pT])

            def ev_h():
                for kc in range(8):
                    i = S_.activation(out=hTt[:, kc, :], in_=pT[:, kc, :], func=AF.Identity,
                                      scale=A_fm[:, kc, b:b + 1], bias=ada_fm[:, kc, b:b + 1])
                return i
            kb.op("act", ev_h, [b_pT, b_Afm, b_ada], [b_hT], cost=8 * 0.46)
            def mm_glr():
                for kc in range(8):
                    i = T_.matmul(pM[0:16, 256:384], lhsT=w_in_bf[:, kc, 3584:3600], rhs=hTt[:, kc, :], start=(kc == 0), stop=(kc == 7))
                return i
            pe(mm_glr, 1024, [b_hT, b_win], [b_pM])
            act(lambda: S_.copy(out=glrT[0:16, :], in_=pM[0:16, 256:384]), 128, [b_pM], [b_glrT])
            pe(lambda: T_.matmul(pM[:, 0:256], lhsT=glrT[0:17, :], rhs=wg_aug[0:17, :], start=True, stop=True), 256, [b_glrT, b_wg], [b_pM])
            act(lambda: S_.activation(out=e_sb[:], in_=pM[:, 0:256], func=AF.Exp, scale=-1.0), 256, [b_pM], [b_e])
            act(lambda: S_.activation(out=l_sb[:], in_=e_sb[:], func=AF.Ln, bias=1.0), 256, [b_e], [b_l])
            pq, b_pq = inproj_group(hTt, b_hT, 0, 512)
            rotary(pq, b_pq, tt, b_t, 0, q_r, b_qr)
            pk, b_pk = inproj_group(hTt, b_hT, 512, 512)
            rotary(pk, b_pk, tt, b_t, 512, k_r, b_kr)
            pb_b, b_pbb = bank("h")

            pe(lambda: T_.matmul(pb_b[:, 0:256], lhsT=Lp, rhs=l_sb[:], start=True, stop=True), 1024, [b_l, b_cst], [b_pbb])

            def mm_bl():
                T_.matmul(pM[:, 384:385], lhsT=l_sb[:, 0:128], rhs=m16[:, 0:1], start=True, stop=True)
                i = T_.matmul(pM[:, 385:386], lhsT=l_sb[:, 128:256], rhs=m16[:, 0:1], start=True, stop=True)
                return i
            pe(mm_bl, 512, [b_l, b_m16], [b_pM])
            act(lambda: S_.activation(out=E1[:], in_=pb_b[:, 0:256], func=AF.Exp, bias=LN_EIGHTH), 256, [b_pbb], [b_E1])
            act(lambda: S_.activation(out=E2[:], in_=pb_b[:, 0:256], func=AF.Exp, scale=-1.0), 256, [b_pbb], [b_E2])
            act(lambda: S_.activation(out=a_fm[:], in_=pM[:, 384:386], func=AF.Exp), 2, [b_pM], [b_afm])
            pg, b_pg = inproj_group(hTt, b_hT, 2048, 512)
            dve(lambda: V.tensor_tensor(out=q_e[:], in0=pg[:, 0:256], in1=E1[:], op=ALU.mult), 256, [b_pg, b_E1], [b_qe])
            dve(lambda: V.tensor_tensor(out=k_e[:], in0=pg[:, 256:512], in1=E2[:], op=ALU.mult), 256, [b_pg, b_E2], [b_ke])
            pv, b_pv = inproj_group(hTt, b_hT, 1024, 512)
            act(lambda: S_.copy(out=v_bf[:], in_=pv[:]), 512, [b_pv], [b_v])
            pgv, b_pgv = inproj_group(hTt, b_hT, 2560, 512)
            act(lambda: S_.copy(out=gv_bf[:], in_=pgv[:]), 512, [b_pgv], [b_gv])
            pz, b_pz = inproj_group(hTt, b_hT, 1536, 512)
            pgz, b_pgz = inproj_group(hTt, b_hT, 3072, 512)

            def silus():
                S_.activation(out=szr[:], in_=pz[:], func=AF.Silu)
                return S_.activation(out=szg[:], in_=pgz[:], func=AF.Silu)
            kb.op("act", silus, [b_pz, b_pgz], [b_szr, b_szg], cost=2 * 0.64)
            def tr_qk():
                for h in range(4):
                    T_.transpose(out=pT[:, h, :], in_=q_r[:, h * 128:(h + 1) * 128], identity=ident_bf[:])
                for h in range(4):
                    i = T_.transpose(out=pT[:, 4 + h, :], in_=k_r[:, h * 128:(h + 1) * 128], identity=ident_bf[:])
                return i
            pe(tr_qk, 1024, [b_qr, b_kr, b_idbf], [b_pT])
            dve(lambda: V.tensor_copy(out=qkT[:], in_=pT[:]), 1024, [b_pT], [b_qkT], mode=2.0)
            def tr_ge():
                for u in range(2):
                    T_.transpose(out=pT[:, u, :], in_=q_e[:, u * 128:(u + 1) * 128], identity=ident_bf[:])
                for u in range(2):
                    i = T_.transpose(out=pT[:, 2 + u, :], in_=k_e[:, u * 128:(u + 1) * 128], identity=ident_bf[:])
                return i
            pe(tr_ge, 512, [b_qe, b_ke, b_idbf], [b_pT])

            def ev_ge():
                for h in range(4):
                    r0 = (h % 2) * 64
                    V.tensor_copy(out=geT[r0:r0 + 64, h, :], in_=pT[r0:r0 + 64, h // 2, :])
                return V.tensor_copy(out=geT[:, 4:6, :], in_=pT[:, 2:4, :])
            kb.op("dve", ev_ge, [b_pT], [b_geT], cost=5 * 0.25)
            (T0, bT0), (T1, bT1), (T2, bT2) = tailb
            psr, b_psr = T0, bT0
            psg, b_psg = T1, bT1
            pyr, b_pyr = T2, bT2
            pkr, b_pkr = T0, bT0
            pyg, b_pyg = T1, bT1
            pkg, b_pkg = T0, bT0

            def mm_sr():
                for h in range(4):
                    i = T_.matmul(psr[:, h * 128:(h + 1) * 128], lhsT=qkT[:, 4 + h, :], rhs=qkT[:, h, :], start=True, stop=True)
                return i
            pe(mm_sr, 512, [b_qkT], [b_psr])
            dve(lambda: V.tensor_tensor(out=ST_r[:], in0=psr[:], in1=mask_r, op=ALU.mult), 512, [b_psr, b_cst], [b_STr])

            def mm_sg():
                for h in range(4):
                    i = T_.matmul(psg[:, h * 128:(h + 1) * 128], lhsT=geT[:, 4 + h // 2, :], rhs=geT[:, h, :], start=True, stop=True)
                return i
            pe(mm_sg, 512, [b_geT], [b_psg])
            dve(lambda: V.tensor_tensor(out=ST_g[:].rearrange("p (h i) -> p h i", h=4), in0=psg[:].rearrange("p (h i) -> p h i", h=4),
                                        in1=mask_g.unsqueeze(1).to_broadcast([128, 4, 128]), op=ALU.mult), 512, [b_psg, b_cst], [b_STg])

            def mm_yr():
                for h in range(4):
                    T_.matmul(pyr[:, h * 128:(h + 1) * 128], lhsT=ST_r[:, h * 128:(h + 1) * 128], rhs=v_bf[:, h * 128:(h + 1) * 128],
                              start=True, stop=False)
                    i = T_.matmul(pyr[:, h * 128:(h + 1) * 128], lhsT=qkT[:, h, :], rhs=R_bf[:, h, :], start=False, stop=True)
                return i
            pe(mm_yr, 1024, [b_STr, b_v, b_qkT, b_Rbf], [b_pyr])

            def mm_kr():
                for h in range(4):
                    i = T_.matmul(pkr[:, h * 128:(h + 1) * 128], lhsT=k_r[:, h * 128:(h + 1) * 128], rhs=v_bf[:, h * 128:(h + 1) * 128],
                                  start=True, stop=True)
                return i
            pe(mm_kr, 512, [b_kr, b_v], [b_pkr])

            def up_R():
                for h in range(4):
                    i = V.scalar_tensor_tensor(out=R[:, h, :], in0=R[:, h, :], scalar=cd[h], in1=pkr[:, h * 128:(h + 1) * 128],
                                               op0=ALU.mult, op1=ALU.add)
                return i
            kb.op("dve", up_R, [b_pkr], [b_R], cost=4 * 0.3)
            act(lambda: S_.copy(out=R_bf[:], in_=R[:]), 512, [b_R], [b_Rbf])

            def mm_yg():
                for h in range(4):
                    T_.matmul(pyg[:, h * 128:(h + 1) * 128], lhsT=ST_g[:, h * 128:(h + 1) * 128], rhs=gv_bf[:, h * 128:(h + 1) * 128],
                              start=True, stop=False)
                    i = T_.matmul(pyg[:, h * 128:(h + 1) * 128], lhsT=geT[:, h, :], rhs=Sg_bf[:, h // 2, :], start=False, stop=True)
                return i
            pe(mm_yg, 1024, [b_STg, b_gv, b_geT, b_Sgbf], [b_pyg])

            def mm_kg():
                for h in range(4):
                    r0 = (h % 2) * 64
                    u = h // 2
                    i = T_.matmul(pkg[r0:r0 + 64, u * 128:(u + 1) * 128], lhsT=k_e[:, h * 64:(h + 1) * 64], rhs=gv_bf[:, h * 128:(h + 1) * 128],
                                  start=True, stop=True)
                return i
            pe(mm_kg, 512, [b_ke, b_gv], [b_pkg])
            Sg2 = Sg[:].rearrange("p u e -> p (u e)")
            dve(lambda: V.tensor_tensor(out=Sg2, in0=Sg2, in1=pkg[:, 0:256], op=ALU.add), 256, [b_pkg], [b_Sg])
            dve(lambda: V.tensor_tensor(out=Sg[:], in0=Sg[:], in1=a_fm[:].unsqueeze(2).to_broadcast([128, 2, 128]), op=ALU.mult), 256, [b_afm], [b_Sg])
            act(lambda: S_.copy(out=Sg_bf[:], in_=Sg[:]), 256, [b_Sg], [b_Sgbf])
            def sq_y():
                for h in range(4):
                    S_.activation(out=mix[:, h * 128:(h + 1) * 128], in_=pyr[:, h * 128:(h + 1) * 128], func=AF.Square, accum_out=stats[:, 1 + h:2 + h])
                for h in range(4):
                    i = S_.activation(out=mix[:, 512 + h * 128:512 + (h + 1) * 128], in_=pyg[:, h * 128:(h + 1) * 128], func=AF.Square, accum_out=stats[:, 5 + h:6 + h])
                return i
            kb.op("act", sq_y, [b_pyr, b_pyg], [bs["y"], b_mix], cost=8 * 0.42)
            rsqrt_cols(par, "y", 1, 9, 1.0 / 128.0)

            def mk_mix():
                for h in range(4):
                    V.scalar_tensor_tensor(out=mix[:, h * 128:(h + 1) * 128], in0=pyr[:, h * 128:(h + 1) * 128], scalar=rstd[:, 1 + h:2 + h],
                                           in1=szr[:, h * 128:(h + 1) * 128], op0=ALU.mult, op1=ALU.mult)
                for h in range(4):
                    i = V.scalar_tensor_tensor(out=mix[:, 512 + h * 128:512 + (h + 1) * 128], in0=pyg[:, h * 128:(h + 1) * 128],
                                               scalar=rstd[:, 5 + h:6 + h], in1=szg[:, h * 128:(h + 1) * 128], op0=ALU.mult, op1=ALU.mult)
                return i
            kb.op("dve", mk_mix, [b_pyr, b_pyg, br["y"], b_szr, b_szg], [b_mix], cost=8 * 0.3)
            def tr_m():
                for kc in range(8):
                    i = T_.transpose(out=pT[:, kc, :], in_=mix[:, kc * 128:(kc + 1) * 128], identity=ident_bf[:])
                return i
            pe(tr_m, 1024, [b_mix, b_idbf], [b_pT])
            dve(lambda: V.tensor_copy(out=mixT[:], in_=pT[:]), 1024, [b_pT], [b_mixT], mode=2.0)
            for n in range(2):
                po, b_po = tailb[0] if n == 0 else tailb[2]

                def mm_o(n=n, po=po):
                    for kc in range(8):
                        i = T_.matmul(po[:], lhsT=mixT[:, kc, :], rhs=w_out_bf[:, kc, n * 512:(n + 1) * 512], start=(kc == 0), stop=(kc == 7))
                    return i
                pe(mm_o, 8 * 512, [b_mixT, b_wout], [b_po])
                sl = slice(n * 512, (n + 1) * 512)
                dve(lambda po=po, sl=sl: V.tensor_tensor(out=res[:, sl], in0=po[:], in1=gate_bc[:, sl], op=ALU.mult), 512, [b_po, b_gbc], [b_res])
            dve(lambda: V.tensor_tensor(out=res[:], in0=res[:], in1=xt[:], op=ALU.add), 1024, [b_res, b_x], [b_res])
            act(lambda: S_.activation(out=ot[:], in_=res[:], func=AF.Square, accum_out=stats[:, 9:10]), 1024, [b_res], [bs["f"], b_o])
            rsqrt_cols(par, "f", 9, 10, 1.0 / D)
            dve(lambda: V.scalar_tensor_tensor(out=ot[:], in0=res[:], scalar=rstd[:, 9:10], in1=fg_bc[:], op0=ALU.mult, op1=ALU.mult),
                1024, [b_res, br["f"], b_fgbc], [b_o])
            kb.dma("sp", f"st{par}", [(out_d[g * 128:(g + 1) * 128, :], ot[:])], reads=[b_o])

        NG = NB * NT
        for g in range(NG):
            kb.tag = g
            load_tile(g)
            tile_body(g)
        kb.run()
        build_nc.last_est_us = kb.est_us
        build_nc.last_kb = kb
    return nc


_CACHE = {}


def kernel(x, c, norm_gain, w_ada, b_ada, w_in, w_gate_up, b_gate_up, gla_norm_gain, w_out, final_gain):
    NB, NT = 4, 16
    f = lambda a: np.ascontiguousarray(np.asarray(a, dtype=np.float32))
    x = f(x)
    c = f(c)
    if "nc" not in _CACHE:
        _CACHE["nc"] = build_nc(NB, NT)
        _CACHE["consts"] = make_consts()
    nc = _CACHE["nc"]
    tab, cst = _CACHE["consts"]
    shared = {
        "norm_gain": f(norm_gain).reshape(1, D), "w_ada": f(w_ada).reshape(D, 3 * D), "b_ada": f(b_ada).reshape(1, 3 * D),
        "w_in": f(w_in).reshape(D, DIN), "w_gate_up": f(w_gate_up).reshape(16, 256), "b_gate_up": f(b_gate_up).reshape(1, 256),
        "gla_norm_gain": f(gla_norm_gain).reshape(1, 128), "w_out": f(w_out).reshape(D, D), "final_gain": f(final_gain).reshape(1, D),
        "tab": tab, "cst": cst,
    }
    in_maps = []
    for i in range(NCORES):
        m = dict(shared)
        m["x"] = x[i * NB:(i + 1) * NB].reshape(NB * SEQ, D)
        m["c"] = c[i * NB:(i + 1) * NB]
        in_maps.append(m)
    res = run_bass_kernel_spmd(nc, in_maps, core_ids=list(range(NCORES)))
    out = np.concatenate([r["out"].reshape(NB, SEQ, D) for r in res.results], axis=0)
    return out.astype(np.float32)
```

```python
import math
from contextlib import ExitStack

import numpy as np
import concourse.bass as bass
import concourse.mybir as mybir
from concourse.bass_utils import run_bass_kernel_spmd

F32 = mybir.dt.float32
BF16 = mybir.dt.bfloat16
AF = mybir.ActivationFunctionType
ALU = mybir.AluOpType

D = 1024
SEQ = 2048
DIN = 3600
NCORES = 8
EPS = 1e-6
G = [1.0 - 2.0 ** (-5.0 - h) for h in range(4)]
import os
SKIP_PE_SELF = os.environ.get('SKIP_PE_SELF', '0') == '1'
SEQ_ORDER = os.environ.get('SEQ_ORDER', '0') == '1'
EVH2 = os.environ.get('EVH2', '0') == '1'
BSPLIT = os.environ.get('BSPLIT', '1') == '1'
LAT_SAME = float(os.environ.get('LAT_SAME', '0.1'))
LAT_X = float(os.environ.get('LAT_X', '0.3'))


class Buf:
    __slots__ = ("name",)

    def __init__(self, name):
        self.name = name


class Rec:
    __slots__ = ("kind", "e", "fn", "reads", "writes", "cost", "sem", "pairs", "kw", "deps", "raw", "start", "tag")


class KB:
    ENG = ("pe", "act", "dve", "pool", "sp")

    def __init__(self, nc, stack):
        self.nc = nc
        self.engs = {"pe": nc.tensor, "act": nc.scalar, "dve": nc.vector, "pool": nc.gpsimd, "sp": nc.sync}
        self.sems = {}
        self.cnt = {}
        self.stack = stack
        for e in self.engs:
            self.sems[e] = stack.enter_context(nc.semaphore("s_" + e))
            self.cnt[e] = 0
        self.seen = {e: {} for e in self.engs}
        self.prog = []
        self.tag = -1
        self.nwaits = 0

    def new_sem(self, name):
        self.sems[name] = self.stack.enter_context(self.nc.semaphore("s_" + name))
        self.cnt[name] = 0
        return name

    def op(self, e, fn, reads=(), writes=(), cost=0.3):
        r = Rec()
        r.kind, r.e, r.fn, r.reads, r.writes, r.cost = "op", e, fn, tuple(reads), tuple(writes), cost
        r.tag = self.tag
        self.prog.append(r)

    def dma(self, e, sem, pairs, reads=(), writes=(), cost=6.0, **kw):
        if sem is None:
            sem = self.new_sem(f"d{len(self.sems)}")
        r = Rec()
        r.kind, r.e, r.reads, r.writes, r.cost = "dma", e, tuple(reads), tuple(writes), cost
        r.sem, r.pairs, r.kw = sem, list(pairs), kw
        r.tag = self.tag
        self.prog.append(r)

    def _wait(self, e, toks):
        best = {}
        for (s, v) in toks:
            if v > best.get(s, 0):
                best[s] = v
        for s, v in best.items():
            if self.seen[e].get(s, 0) < v:
                self.engs[e].wait_ge(self.sems[s], v)
                self.seen[e][s] = v
                self.nwaits += 1

    def run(self):
        prog = self.prog
        N = len(prog)
        lastw, readers = {}, {}
        succ = [[] for _ in range(N)]
        indeg = [0] * N
        for i, r in enumerate(prog):
            raw, other = set(), set()
            for b in r.reads:
                if b in lastw:
                    raw.add(lastw[b])
            for b in r.writes:
                if b in lastw:
                    other.add(lastw[b])
                other.update(readers.get(b, ()))
            deps = (raw | other)
            deps.discard(i)
            r.deps, r.raw = deps, raw
            for d in deps:
                succ[d].append(i)
            indeg[i] = len(deps)
            for b in r.writes:
                lastw[b] = i
                readers[b] = []
            for b in r.reads:
                if b not in r.writes:
                    readers.setdefault(b, []).append(i)
        finish = [0.0] * N
        ready_t = [0.0] * N
        avail = {e: [] for e in self.ENG}
        free = {e: 0.0 for e in self.ENG}
        for i in range(N):
            if indeg[i] == 0:
                avail[prog[i].e].append(i)
        order = []
        while len(order) < N:
            best = None
            for e in self.ENG:
                fe = free[e]
                for i in avail[e]:
                    key = (max(ready_t[i], fe), i)
                    if best is None or key < best[0]:
                        best = (key, e, i)
            (st, _), e, i = best
            avail[e].remove(i)
            r = prog[i]
            r.start = st
            if r.kind == "dma":
                free[e] = st + 0.15 * len(r.pairs)
            else:
                free[e] = st + r.cost
            finish[i] = st + r.cost
            order.append(i)
            for s_ in succ[i]:
                lat = LAT_SAME if prog[s_].e == e and r.kind == "op" else LAT_X
                if finish[i] + lat > ready_t[s_]:
                    ready_t[s_] = finish[i] + lat
                indeg[s_] -= 1
                if indeg[s_] == 0:
                    avail[prog[s_].e].append(s_)
        self.est_us = max(finish) if N else 0.0
        self.finish_t = finish
        self.order = order
        if SEQ_ORDER:
            order = list(range(N))
        tok = [None] * N
        dma_last = {}
        for i in order:
            r = prog[i]
            waits = []
            for d in r.deps:
                pd = prog[d]
                if pd.kind == "op" and pd.e == r.e == "pe" and SKIP_PE_SELF:
                    continue
                waits.append(tok[d])
            self._wait(r.e, waits)
            if r.kind == "op":
                inst = r.fn()
                self.cnt[r.e] += 1
                inst.then_inc(self.sems[r.e], 1)
                tok[i] = (r.e, self.cnt[r.e])
            else:
                for (o, a) in r.pairs:
                    inst = self.engs[r.e].dma_start(out=o, in_=a, **r.kw)
                    self.cnt[r.sem] += 16
                    inst.then_inc(self.sems[r.sem], 16)
                tok[i] = (r.sem, self.cnt[r.sem])
                dma_last[r.sem] = tok[i]
        for e in ("sp", "pool"):
            self._wait(e, list(dma_last.values()))


def make_consts():
    C = 128
    pos = np.arange(SEQ, dtype=np.float64)
    invf = 10000.0 ** (-np.arange(0, 128, 2, dtype=np.float64) / 128.0)
    ang = pos[:, None] * invf[None, :]
    cos = np.cos(ang).reshape(16, C, 1, 64)
    sin = np.sin(ang).reshape(16, C, 1, 64)
    p = np.arange(C, dtype=np.float64)
    g = np.array(G, dtype=np.float64)
    sq = g[None, :] ** (p[:, None] + 1.0)
    sk = g[None, :] ** (C - 1.0 - p[:, None]) * (128.0 ** -0.5)
    tab = np.concatenate(
        [cos * sq[None, :, :, None], sin * sq[None, :, :, None], cos * sk[None, :, :, None], sin * sk[None, :, :, None]],
        axis=2,
    )
    tab = tab.reshape(16, C, 1024).astype(np.float32)
    j = np.arange(C)[:, None]
    i = np.arange(C)[None, :]
    caus = (i >= j).astype(np.float64)
    mask_r = np.stack([caus * g[h] ** (-float(C)) for h in range(4)], axis=1).reshape(C, 512)
    Lp = (j <= i).astype(np.float64) * (-1.0 / 16.0)
    Up = (j > i).astype(np.float64) * (-1.0 / 16.0)
    ident = np.eye(C)
    cst = np.concatenate([mask_r, caus, Lp, Up, ident], axis=1).astype(np.float32)
    return tab, cst


def build_nc(NB=4, NT=16):
    NTOK = NB * NT * 128
    nc = bass.Bass("TRN2", target_bir_lowering=False)
    dr = lambda name, shape, kind="ExternalInput": nc.dram_tensor(name, shape, F32, kind=kind).ap()
    x_d = dr("x", [NTOK, D])
    c_d = dr("c", [NB, D])
    ng_d = dr("norm_gain", [1, D])
    wada_d = dr("w_ada", [D, 3 * D])
    bada_d = dr("b_ada", [1, 3 * D])
    win_d = dr("w_in", [D, DIN])
    wg_d = dr("w_gate_up", [16, 256])
    bg_d = dr("b_gate_up", [1, 256])
    gng_d = dr("gla_norm_gain", [1, 128])
    wout_d = dr("w_out", [D, D])
    fg_d = dr("final_gain", [1, D])
    tab_d = dr("tab", [16, 128, 1024])
    cst_d = dr("cst", [128, 1024])
    out_d = dr("out", [NTOK, D], kind="ExternalOutput")

    with ExitStack() as st:
        kb = KB(nc, st)
        _n = [0]

        def sb(shape, dt, name):
            t = st.enter_context(nc.sbuf_tensor("S_" + name, shape, dt))
            return t, Buf(name)

        def sbn(n, shape, dt, name):
            return [sb(shape, dt, f"{name}{i}") for i in range(n)]

        def ps(shape, dt, name):
            t = st.enter_context(nc.psum_tensor(name, shape, dt))
            return t, Buf(name)

        V, S_, P_, T_ = nc.vector, nc.scalar, nc.gpsimd, nc.tensor

        def dve(fn, n, reads, writes, mode=1.0):
            kb.op("dve", fn, reads, writes, cost=(n / mode + 150.0) / 960.0)

        def act(fn, n, reads, writes):
            kb.op("act", fn, reads, writes, cost=(n + 260.0) / 1200.0)

        def pe(fn, cols, reads, writes):
            kb.op("pe", fn, reads, writes, cost=cols / 2000.0 + 0.06)

        def pool(fn, cost, reads, writes):
            kb.op("pool", fn, reads, writes, cost=cost)

        w_in_bf, b_win = sb([128, 8, DIN], BF16, "w_in_bf")
        w_out_bf, b_wout = sb([128, 8, D], BF16, "w_out_bf")
        cst, b_cst = sb([128, 1024], F32, "cst")
        ident_bf, b_idbf = sb([128, 128], BF16, "ident_bf")
        mask_r = cst[:, 0:512]
        mask_g = cst[:, 512:640]
        Lp = cst[:, 640:768]
        Up = cst[:, 768:896]
        identf = cst[:, 896:1024]
        xs = sbn(3, [128, D], F32, "xs")
        tabs = sbn(2, [128, 1024], F32, "tabs")
        outs = sbn(2, [128, D], F32, "outs")
        xn_ = sbn(2, [128, D], BF16, "xn")
        hT = sbn(2, [128, 8, 128], BF16, "hT")
        b_hTb = [Buf("hTb0"), Buf("hTb1")]
        rA, b_rA = sb([128, 512], F32, "rA")
        rB, b_rB = sb([128, 512], F32, "rB")
        q_r_ = sbn(2, [128, 512], BF16, "q_r")
        k_r_ = sbn(2, [128, 512], BF16, "k_r")
        v_bf_ = sbn(2, [128, 512], BF16, "v_bf")
        gv_bf_ = sbn(2, [128, 512], BF16, "gv_bf")
        szr_ = sbn(2, [128, 512], F32, "szr")
        szg_ = sbn(2, [128, 512], F32, "szg")
        qkT_ = sbn(2, [128, 8, 128], BF16, "qkT")
        geT_ = sbn(2, [128, 6, 128], BF16, "geT")
        q_e_ = sbn(2, [128, 256], BF16, "q_e")
        k_e_ = sbn(2, [128, 256], BF16, "k_e")
        k_l_ = sbn(2, [128, 256], BF16, "k_l")
        e_sb, b_e = sb([128, 256], F32, "e_sb")
        l_sb_ = sbn(2, [128, 256], F32, "l_sb")
        E1_ = sbn(2, [128, 256], F32, "E1")
        E2_ = sbn(2, [128, 256], F32, "E2")
        E3_ = sbn(2, [128, 256], F32, "E3")
        a_fm_ = sbn(2, [128, 2], F32, "a_fm")
        glrT_ = sbn(2, [32, 128], BF16, "glrT")
        wg_aug, b_wg = sb([32, 256], BF16, "wg_aug")
        ST_r_ = sbn(2, [128, 512], BF16, "ST_r")
        ST_g_ = sbn(2, [128, 512], BF16, "ST_g")
        R, b_R = sb([128, 4, 128], F32, "R")
        R_bf, b_Rbf = sb([128, 4, 128], BF16, "R_bf")
        Sg, b_Sg = sb([128, 2, 128], F32, "Sg")
        Sg_bf, b_Sgbf = sb([128, 2, 128], BF16, "Sg_bf")
        mix_ = sbn(2, [128, D], BF16, "mix")
        mixT_ = sbn(2, [128, 8, 128], BF16, "mixT")
        res_ = sbn(2, [128, D], F32, "res")
        gate_bc, b_gbc = sb([128, D], F32, "gate_bc")
        fg_bc, b_fgbc = sb([128, D], F32, "fg_bc")
        grep_, b_grep = sb([128, 8, 128], F32, "grep")
        stats_ = [st.enter_context(nc.sbuf_tensor(f"S_stats{i}", [128, 16], F32)) for i in range(2)]
        msq_ = [st.enter_context(nc.sbuf_tensor(f"S_msq{i}", [128, 16], F32)) for i in range(2)]
        rstd_ = [st.enter_context(nc.sbuf_tensor(f"S_rstd{i}", [128, 16], F32)) for i in range(2)]
        b_stats = [{k: Buf(f"stats{i}{k}") for k in "xyf"} for i in range(2)]
        b_msq = [{k: Buf(f"msq{i}{k}") for k in "xyf"} for i in range(2)]
        b_rstd = [{k: Buf(f"rstd{i}{k}") for k in "xyf"} for i in range(2)]
        eps_t, b_eps = sb([128, 1], F32, "eps_t")
        m16, b_m16 = sb([128, 2], F32, "m16")
        cT, b_cT = sb([128, 8, NB], F32, "cT")
        ng_fm, b_ngfm = sb([128, 8], F32, "ng_fm")
        bada_fm, b_badafm = sb([128, 24], F32, "bada_fm")
        gng_fm, b_gngfm = sb([128, 1], F32, "gng_fm")
        ada_fm, b_ada = sb([128, 24, NB], F32, "ada_fm")
        A_fm, b_Afm = sb([128, 8, NB], F32, "A_fm")

        headb = [ps([128, 512], F32, f"phead{i}") for i in range(2)]
        tailb = [ps([128, 512], F32, f"ptail{i}") for i in range(3)]
        pT_ = [ps([128, 8, 128], BF16, f"pT{i}") for i in range(2)]
        pM, b_pM = ps([128, 512], F32, "pM")
        _rr = {"h": 0, "t": 0}

        def bank(kind="t"):
            pool_ = headb if kind == "h" else tailb
            t = pool_[_rr[kind] % len(pool_)]
            _rr[kind] += 1
            return t

        for s in ["ld_x0", "ld_x1", "ld_x2", "ld_t0", "ld_t1", "ld_s0", "ld_s1", "st0", "st1"]:
            kb.new_sem(s)

        kb.dma("sp", None, [(cst[:], cst_d[:, :])], writes=[b_cst])
        kb.dma("sp", None, [(cT[:, :, b], c_d[b:b + 1, :].rearrange("o (k p) -> p (o k)", p=128)) for b in range(NB)],
               writes=[b_cT], allow_slow_non_contiguous=True)
        kb.dma("sp", None, [(ng_fm[:], ng_d.rearrange("o (k p) -> p (o k)", p=128))], writes=[b_ngfm],
               allow_slow_non_contiguous=True)
        kb.dma("sp", None, [(bada_fm[:, j * 8:(j + 1) * 8], bada_d[:, j * 1024:(j + 1) * 1024].rearrange("o (k p) -> p (o k)", p=128))
                            for j in range(3)], writes=[b_badafm], allow_slow_non_contiguous=True)
        kb.dma("sp", None, [(gng_fm[:], gng_d.rearrange("o (p one) -> (o p) one", one=1))], writes=[b_gngfm],
               allow_slow_non_contiguous=True)
        kb.dma("sp", None, [(fg_bc[:], fg_d[0:1, :].to_broadcast([128, D]))], writes=[b_fgbc])
        kb.dma("pool", None, [(w_in_bf[:, kc, :], win_d[kc * 128:(kc + 1) * 128, :]) for kc in range(8)],
               writes=[b_win], cost=90.0, max_dma_last_dim=4096)
        kb.dma("pool", None, [(w_out_bf[:, kc, :], wout_d[kc * 128:(kc + 1) * 128, :]) for kc in range(4)],
               writes=[b_wout], cost=20.0, max_dma_last_dim=4096)
        kb.dma("pool", None, [(wg_aug[0:16, :], wg_d[:, :]), (wg_aug[16:17, :], bg_d[:, :])], writes=[b_wg])

        dve(lambda: V.memset(eps_t[:], EPS), 1, [], [b_eps])
        dve(lambda: V.memset(m16[:], -1.0 / 16.0), 2, [], [b_m16])
        for i in range(2):
            dve(lambda i=i: V.memset(glrT_[i][0][:], 1.0), 128, [], [glrT_[i][1]])
            dve(lambda i=i: V.memset(geT_[i][0][:], 0.0), 768, [], [geT_[i][1]])
        dve(lambda: V.tensor_copy(out=ident_bf[:], in_=identf), 128, [b_cst], [b_idbf])

        for kc in range(4, 8):
            stg, b_stg = outs[kc % 2]
            kb.dma("sp", f"ld_s{kc % 2}", [(stg[:], wout_d[kc * 128:(kc + 1) * 128, :])], writes=[b_stg])
            dve(lambda kc=kc, stg=stg: V.tensor_scalar(out=w_out_bf[:, kc, :], in0=stg[:], scalar1=gng_fm[:, 0:1], scalar2=None, op0=ALU.mult),
                1024, [b_stg, b_gngfm], [b_wout])

        act(lambda: S_.activation(out=cT[:], in_=cT[:], func=AF.Silu), 8 * NB, [b_cT], [b_cT])
        for j in range(24):
            stg, b_stg = outs[j % 2]
            stg3 = stg[:].rearrange("p (k c) -> p k c", k=8)
            kb.dma("sp", f"ld_s{j % 2}", [(stg3, wada_d[:, j * 128:(j + 1) * 128].rearrange("(k p) c -> p k c", p=128))], writes=[b_stg])

            def mm_ada(stg3=stg3):
                for kc in range(8):
                    i = T_.matmul(pM[:, 0:NB], lhsT=stg3[:, kc, :], rhs=cT[:, kc, :], start=(kc == 0), stop=(kc == 7))
                return i
            pe(mm_ada, 8 * 4 * 128, [b_stg, b_cT], [b_pM])
            dve(lambda j=j: V.tensor_scalar(out=ada_fm[:, j, :], in0=pM[:, 0:NB], scalar1=bada_fm[:, j:j + 1], scalar2=None, op0=ALU.add),
                NB, [b_pM, b_badafm], [b_ada])
        dve(lambda: V.tensor_scalar(out=A_fm[:], in0=ada_fm[:, 8:16, :], scalar1=1.0, scalar2=None, op0=ALU.add), 8 * NB, [b_ada], [b_Afm])
        dve(lambda: V.tensor_tensor(out=A_fm[:], in0=A_fm[:], in1=ng_fm[:].unsqueeze(2).to_broadcast([128, 8, NB]), op=ALU.mult),
            8 * NB, [b_Afm, b_ngfm], [b_Afm])

        cd = [G[h] ** 128.0 for h in range(4)]
        LN_EIGHTH = math.log(0.125)

        def rsqrt_cols(par, grp, c0, c1, inv_n):
            stt, msq, rstd = stats_[par], msq_[par], rstd_[par]

            act(lambda: S_.activation(out=msq[:, c0:c1], in_=stt[:, c0:c1], func=AF.Ln, scale=inv_n, bias=eps_t[:, 0:1]),
                c1 - c0, [b_stats[par][grp], b_eps], [b_msq[par][grp]])
            act(lambda: S_.activation(out=rstd[:, c0:c1], in_=msq[:, c0:c1], func=AF.Exp, scale=-0.5),
                c1 - c0, [b_msq[par][grp]], [b_rstd[par][grp]])

        def load_tile(g):
            t = g % NT
            xt, b_x = xs[g % 3]
            tt, b_t = tabs[g % 2]
            kb.dma("sp", f"ld_x{g % 3}", [(xt[:], x_d[g * 128:(g + 1) * 128, :])], writes=[b_x])
            kb.dma("sp", f"ld_t{g % 2}", [(tt[:], tab_d[t, :, :])], writes=[b_t])

        def seq_start(b):
            dve(lambda: V.memset(R[:], 0.0), 512, [], [b_R])
            dve(lambda: V.memset(R_bf[:], 0.0), 512, [], [b_Rbf], mode=2.0)
            dve(lambda: V.memset(Sg[:], 0.0), 256, [], [b_Sg])
            dve(lambda: V.memset(Sg_bf[:], 0.0), 256, [], [b_Sgbf], mode=2.0)
            dve(lambda: V.tensor_copy(out=grep_[:], in_=ada_fm[:, 16:24, b:b + 1].to_broadcast([128, 8, 128])), 1024, [b_ada], [b_grep])
            for n in range(2):
                pb, b_pb = tailb[1 + n]

                def mm_g(n=n, pb=pb):
                    for q in range(4):
                        kc = n * 4 + q
                        i = T_.matmul(pb[:, q * 128:(q + 1) * 128], lhsT=grep_[:, kc, :], rhs=identf, start=True, stop=True)
                    return i
                pe(mm_g, 4 * 4 * 128, [b_grep, b_cst], [b_pb])
                act(lambda n=n, pb=pb: S_.copy(out=gate_bc[:, n * 512:(n + 1) * 512], in_=pb[:]), 512, [b_pb], [b_gbc])

        def rotary(pb, b_pb, tt, b_t, toff, dst, b_dst):
            src4 = pb[:].rearrange("p (h two f) -> p h two f", h=4, two=2)
            cs4 = tt[:, toff:toff + 256].rearrange("p (h f) -> p h f", h=4).unsqueeze(2).to_broadcast([128, 4, 2, 64])
            sn4 = tt[:, toff + 256:toff + 512].rearrange("p (h f) -> p h f", h=4).unsqueeze(2).to_broadcast([128, 4, 2, 64])
            A4 = rA[:].rearrange("p (h two f) -> p h two f", h=4, two=2)
            B4 = rB[:].rearrange("p (h two f) -> p h two f", h=4, two=2)
            d4 = dst[:].rearrange("p (h two f) -> p h two f", h=4, two=2)
            dve(lambda: V.tensor_tensor(out=A4, in0=src4, in1=cs4, op=ALU.mult), 512, [b_pb, b_t], [b_rA])
            dve(lambda: V.tensor_tensor(out=B4, in0=src4, in1=sn4, op=ALU.mult), 512, [b_pb, b_t], [b_rB])
            dve(lambda: V.tensor_tensor(out=d4[:, :, 0, :], in0=A4[:, :, 0, :], in1=B4[:, :, 1, :], op=ALU.subtract), 256, [b_rA, b_rB], [b_dst])
            dve(lambda: V.tensor_tensor(out=d4[:, :, 1, :], in0=B4[:, :, 0, :], in1=A4[:, :, 1, :], op=ALU.add), 256, [b_rA, b_rB], [b_dst])

        def inproj_group(hTt, b_hT, c0, ncols):
            pb, b_pb = bank("h")

            def mm():
                for kc in range(8):
                    i = T_.matmul(pb[:, 0:ncols], lhsT=hTt[:, kc, :], rhs=w_in_bf[:, kc, c0:c0 + ncols], start=(kc == 0), stop=(kc == 7))
                return i
            pe(mm, 8 * ncols, list(b_hT) + [b_win], [b_pb])
            return pb, b_pb

        def tile_body(g):
            b = g // NT
            t = g % NT
            par = g % 2
            xt, b_x = xs[g % 3]
            tt, b_t = tabs[par]
            hTt, b_hT = hT[par]
            ot, b_o = outs[par]
            xn, b_xn = xn_[par]
            q_r, b_qr = q_r_[par]
            k_r, b_kr = k_r_[par]
            v_bf, b_v = v_bf_[par]
            gv_bf, b_gv = gv_bf_[par]
            szr, b_szr = szr_[par]
            szg, b_szg = szg_[par]
            qkT, b_qkT = qkT_[par]
            geT, b_geT = geT_[par]
            q_e, b_qe = q_e_[par]
            k_e, b_ke = k_e_[par]
            k_l, b_kl = k_l_[par]
            l_sb, b_l = l_sb_[par]
            E1, b_E1 = E1_[par]
            E2, b_E2 = E2_[par]
            E3, b_E3 = E3_[par]
            a_fm, b_afm = a_fm_[par]
            glrT, b_glrT = glrT_[par]
            ST_r, b_STr = ST_r_[par]
            ST_g, b_STg = ST_g_[par]
            mix, b_mix = mix_[par]
            mixT, b_mixT = mixT_[par]
            res, b_res = res_[par]
            stats, rstd = stats_[par], rstd_[par]
            bs, br = b_stats[par], b_rstd[par]
            pT, b_pT = pT_[par]
            if t == 0:
                seq_start(b)
            act(lambda: S_.activation(out=xn[:], in_=xt[:], func=AF.Square, accum_out=stats[:, 0:1]), 1024, [b_x], [bs["x"], b_xn])
            rsqrt_cols(par, "x", 0, 1, 1.0 / D)
            act(lambda: S_.activation(out=xn[:], in_=xt[:], func=AF.Copy, scale=rstd[:, 0:1]), 1024, [b_x, br["x"]], [b_xn])
            def tr_x():
                for kc in range(8):
                    i = T_.transpose(out=pT[:, kc, :], in_=xn[:, kc * 128:(kc + 1) * 128], identity=ident_bf[:])
                return i
            pe(tr_x, 1024, [b_xn, b_idbf], [b_pT])

            b_hT2 = b_hTb[par]

            def ev_h():
                for kc in range(4):
                    i = S_.activation(out=hTt[:, kc, :], in_=pT[:, kc, :], func=AF.Identity,
                                      scale=A_fm[:, kc, b:b + 1], bias=ada_fm[:, kc, b:b + 1])
                return i
            kb.op("act", ev_h, [b_pT, b_Afm, b_ada], [b_hT], cost=4 * 0.38)

            def ev_h2():
                for kc in range(4, 8):
                    i = V.tensor_scalar(out=hTt[:, kc, :], in0=pT[:, kc, :], scalar1=A_fm[:, kc, b:b + 1], scalar2=ada_fm[:, kc, b:b + 1],
                                        op0=ALU.mult, op1=ALU.add)
                return i
            def ev_h2a():
                for kc in range(4, 8):
                    i = S_.activation(out=hTt[:, kc, :], in_=pT[:, kc, :], func=AF.Identity,
                                      scale=A_fm[:, kc, b:b + 1], bias=ada_fm[:, kc, b:b + 1])
                return i
            if EVH2:
                kb.op("dve", ev_h2, [b_pT, b_Afm, b_ada], [b_hT2], cost=4 * 0.25)
            else:
                kb.op("act", ev_h2a, [b_pT, b_Afm, b_ada], [b_hT2], cost=4 * 0.38)
            def mm_glr():
                for kc in range(8):
                    i = T_.matmul(pM[0:16, 256:384], lhsT=w_in_bf[:, kc, 3584:3600], rhs=hTt[:, kc, :], start=(kc == 0), stop=(kc == 7))
                return i
            pe(mm_glr, 1024, [b_hT, b_hT2, b_win], [b_pM])
            act(lambda: S_.copy(out=glrT[0:16, :], in_=pM[0:16, 256:384]), 128, [b_pM], [b_glrT])
            pe(lambda: T_.matmul(pM[:, 0:256], lhsT=glrT[0:17, :], rhs=wg_aug[0:17, :], start=True, stop=True), 256, [b_glrT, b_wg], [b_pM])
            act(lambda: S_.activation(out=e_sb[:], in_=pM[:, 0:256], func=AF.Exp, scale=-1.0), 256, [b_pM], [b_e])
            act(lambda: S_.activation(out=l_sb[:], in_=e_sb[:], func=AF.Ln, bias=1.0), 256, [b_e], [b_l])
            pq, b_pq = inproj_group(hTt, (b_hT, b_hT2), 0, 512)
            rotary(pq, b_pq, tt, b_t, 0, q_r, b_qr)
            pk, b_pk = inproj_group(hTt, (b_hT, b_hT2), 512, 512)
            rotary(pk, b_pk, tt, b_t, 512, k_r, b_kr)

            def mm_bl():
                T_.matmul(pM[:, 384:385], lhsT=l_sb[:, 0:128], rhs=m16[:, 0:1], start=True, stop=True)
                i = T_.matmul(pM[:, 385:386], lhsT=l_sb[:, 128:256], rhs=m16[:, 0:1], start=True, stop=True)
                return i
            if BSPLIT:
                pe(mm_bl, 512, [b_l, b_m16], [b_pM])
                pe(lambda: T_.matmul(pM[:, 0:256], lhsT=Lp, rhs=l_sb[:], start=True, stop=True), 1024, [b_l, b_cst], [b_pM])
            else:
                def mm_b():
                    mm_bl()
                    return T_.matmul(pM[:, 0:256], lhsT=Lp, rhs=l_sb[:], start=True, stop=True)
                pe(mm_b, 512 + 1024, [b_l, b_m16, b_cst], [b_pM])
            act(lambda: S_.activation(out=E1[:], in_=pM[:, 0:256], func=AF.Exp, bias=LN_EIGHTH), 256, [b_pM], [b_E1])
            act(lambda: S_.activation(out=E2[:], in_=pM[:, 0:256], func=AF.Exp, scale=-1.0), 256, [b_pM], [b_E2])
            act(lambda: S_.activation(out=a_fm[:], in_=pM[:, 384:386], func=AF.Exp), 2, [b_pM], [b_afm])
            pg, b_pg = inproj_group(hTt, (b_hT, b_hT2), 2048, 512)
            dve(lambda: V.tensor_tensor(out=q_e[:], in0=pg[:, 0:256], in1=E1[:], op=ALU.mult), 256, [b_pg, b_E1], [b_qe])
            dve(lambda: V.tensor_tensor(out=k_e[:], in0=pg[:, 256:512], in1=E2[:], op=ALU.mult), 256, [b_pg, b_E2], [b_ke])
            pv, b_pv = inproj_group(hTt, (b_hT, b_hT2), 1024, 512)
            act(lambda: S_.copy(out=v_bf[:], in_=pv[:]), 512, [b_pv], [b_v])
            pgv, b_pgv = inproj_group(hTt, (b_hT, b_hT2), 2560, 512)
            act(lambda: S_.copy(out=gv_bf[:], in_=pgv[:]), 512, [b_pgv], [b_gv])
            pz, b_pz = inproj_group(hTt, (b_hT, b_hT2), 1536, 512)
            pgz, b_pgz = inproj_group(hTt, (b_hT, b_hT2), 3072, 512)

            def silus():
                S_.activation(out=szr[:], in_=pz[:], func=AF.Silu)
                return S_.activation(out=szg[:], in_=pgz[:], func=AF.Silu)
            kb.op("act", silus, [b_pz, b_pgz], [b_szr, b_szg], cost=2 * 0.64)
            def tr_qk():
                for h in range(4):
                    T_.transpose(out=pT[:, h, :], in_=q_r[:, h * 128:(h + 1) * 128], identity=ident_bf[:])
                for h in range(4):
                    i = T_.transpose(out=pT[:, 4 + h, :], in_=k_r[:, h * 128:(h + 1) * 128], identity=ident_bf[:])
                return i
            pe(tr_qk, 1024, [b_qr, b_kr, b_idbf], [b_pT])
            dve(lambda: V.tensor_copy(out=qkT[:], in_=pT[:]), 1024, [b_pT], [b_qkT], mode=2.0)
            def tr_ge():
                for u in range(2):
                    T_.transpose(out=pT[:, u, :], in_=q_e[:, u * 128:(u + 1) * 128], identity=ident_bf[:])
                for u in range(2):
                    i = T_.transpose(out=pT[:, 2 + u, :], in_=k_e[:, u * 128:(u + 1) * 128], identity=ident_bf[:])
                return i
            pe(tr_ge, 512, [b_qe, b_ke, b_idbf], [b_pT])

            def ev_ge():
                for h in range(4):
                    r0 = (h % 2) * 64
                    V.tensor_copy(out=geT[r0:r0 + 64, h, :], in_=pT[r0:r0 + 64, h // 2, :])
                return V.tensor_copy(out=geT[:, 4:6, :], in_=pT[:, 2:4, :])
            kb.op("dve", ev_ge, [b_pT], [b_geT], cost=5 * 0.25)
            (T0, bT0), (T1, bT1), (T2, bT2) = tailb
            psr, b_psr = T0, bT0
            psg, b_psg = T1, bT1
            pyr, b_pyr = T2, bT2
            pkr, b_pkr = T0, bT0
            pyg, b_pyg = T1, bT1
            pkg, b_pkg = T0, bT0

            def mm_sr():
                for h in range(4):
                    i = T_.matmul(psr[:, h * 128:(h + 1) * 128], lhsT=qkT[:, 4 + h, :], rhs=qkT[:, h, :], start=True, stop=True)
                return i
            pe(mm_sr, 512, [b_qkT], [b_psr])
            dve(lambda: V.tensor_tensor(out=ST_r[:], in0=psr[:], in1=mask_r, op=ALU.mult), 512, [b_psr, b_cst], [b_STr])

            def mm_sg():
                for h in range(4):
                    i = T_.matmul(psg[:, h * 128:(h + 1) * 128], lhsT=geT[:, 4 + h // 2, :], rhs=geT[:, h, :], start=True, stop=True)
                return i
            pe(mm_sg, 512, [b_geT], [b_psg])
            dve(lambda: V.tensor_tensor(out=ST_g[:].rearrange("p (h i) -> p h i", h=4), in0=psg[:].rearrange("p (h i) -> p h i", h=4),
                                        in1=mask_g.unsqueeze(1).to_broadcast([128, 4, 128]), op=ALU.mult), 512, [b_psg, b_cst], [b_STg])

            def mm_yr():
                for h in range(4):
                    T_.matmul(pyr[:, h * 128:(h + 1) * 128], lhsT=ST_r[:, h * 128:(h + 1) * 128], rhs=v_bf[:, h * 128:(h + 1) * 128],
                              start=True, stop=False)
                    i = T_.matmul(pyr[:, h * 128:(h + 1) * 128], lhsT=qkT[:, h, :], rhs=R_bf[:, h, :], start=False, stop=True)
                return i
            pe(mm_yr, 1024, [b_STr, b_v, b_qkT, b_Rbf], [b_pyr])

            def mm_kr():
                for h in range(4):
                    i = T_.matmul(pkr[:, h * 128:(h + 1) * 128], lhsT=k_r[:, h * 128:(h + 1) * 128], rhs=v_bf[:, h * 128:(h + 1) * 128],
                                  start=True, stop=True)
                return i
            pe(mm_kr, 512, [b_kr, b_v], [b_pkr])

            def up_R():
                for h in range(4):
                    i = V.scalar_tensor_tensor(out=R[:, h, :], in0=R[:, h, :], scalar=cd[h], in1=pkr[:, h * 128:(h + 1) * 128],
                                               op0=ALU.mult, op1=ALU.add)
                return i
            kb.op("dve", up_R, [b_pkr], [b_R], cost=4 * 0.3)
            act(lambda: S_.copy(out=R_bf[:], in_=R[:]), 512, [b_R], [b_Rbf])

            def mm_yg():
                for h in range(4):
                    T_.matmul(pyg[:, h * 128:(h + 1) * 128], lhsT=ST_g[:, h * 128:(h + 1) * 128], rhs=gv_bf[:, h * 128:(h + 1) * 128],
                              start=True, stop=False)
                    i = T_.matmul(pyg[:, h * 128:(h + 1) * 128], lhsT=geT[:, h, :], rhs=Sg_bf[:, h // 2, :], start=False, stop=True)
                return i
            pe(mm_yg, 1024, [b_STg, b_gv, b_geT, b_Sgbf], [b_pyg])

            def mm_kg():
                for h in range(4):
                    r0 = (h % 2) * 64
                    u = h // 2
                    i = T_.matmul(pkg[r0:r0 + 64, u * 128:(u + 1) * 128], lhsT=k_e[:, h * 64:(h + 1) * 64], rhs=gv_bf[:, h * 128:(h + 1) * 128],
                                  start=True, stop=True)
                return i
            pe(mm_kg, 512, [b_ke, b_gv], [b_pkg])
            Sg2 = Sg[:].rearrange("p u e -> p (u e)")
            dve(lambda: V.tensor_tensor(out=Sg2, in0=Sg2, in1=pkg[:, 0:256], op=ALU.add), 256, [b_pkg], [b_Sg])
            dve(lambda: V.tensor_tensor(out=Sg[:], in0=Sg[:], in1=a_fm[:].unsqueeze(2).to_broadcast([128, 2, 128]), op=ALU.mult), 256, [b_afm], [b_Sg])
            act(lambda: S_.copy(out=Sg_bf[:], in_=Sg[:]), 256, [b_Sg], [b_Sgbf])
            def sq_y():
                for h in range(4):
                    S_.activation(out=mix[:, h * 128:(h + 1) * 128], in_=pyr[:, h * 128:(h + 1) * 128], func=AF.Square, accum_out=stats[:, 1 + h:2 + h])
                for h in range(4):
                    i = S_.activation(out=mix[:, 512 + h * 128:512 + (h + 1) * 128], in_=pyg[:, h * 128:(h + 1) * 128], func=AF.Square, accum_out=stats[:, 5 + h:6 + h])
                return i
            kb.op("act", sq_y, [b_pyr, b_pyg], [bs["y"], b_mix], cost=8 * 0.42)
            rsqrt_cols(par, "y", 1, 9, 1.0 / 128.0)

            def mk_mix():
                for h in range(4):
                    V.scalar_tensor_tensor(out=mix[:, h * 128:(h + 1) * 128], in0=pyr[:, h * 128:(h + 1) * 128], scalar=rstd[:, 1 + h:2 + h],
                                           in1=szr[:, h * 128:(h + 1) * 128], op0=ALU.mult, op1=ALU.mult)
                for h in range(4):
                    i = V.scalar_tensor_tensor(out=mix[:, 512 + h * 128:512 + (h + 1) * 128], in0=pyg[:, h * 128:(h + 1) * 128],
                                               scalar=rstd[:, 5 + h:6 + h], in1=szg[:, h * 128:(h + 1) * 128], op0=ALU.mult, op1=ALU.mult)
                return i
            kb.op("dve", mk_mix, [b_pyr, b_pyg, br["y"], b_szr, b_szg], [b_mix], cost=8 * 0.3)
            def tr_m():
                for kc in range(8):
                    i = T_.transpose(out=pT[:, kc, :], in_=mix[:, kc * 128:(kc + 1) * 128], identity=ident_bf[:])
                return i
            pe(tr_m, 1024, [b_mix, b_idbf], [b_pT])
            dve(lambda: V.tensor_copy(out=mixT[:], in_=pT[:]), 1024, [b_pT], [b_mixT], mode=2.0)
            for n in range(2):
                po, b_po = tailb[0] if n == 0 else tailb[2]

                def mm_o(n=n, po=po):
                    for kc in range(8):
                        i = T_.matmul(po[:], lhsT=mixT[:, kc, :], rhs=w_out_bf[:, kc, n * 512:(n + 1) * 512], start=(kc == 0), stop=(kc == 7))
                    return i
                pe(mm_o, 8 * 512, [b_mixT, b_wout], [b_po])
                sl = slice(n * 512, (n + 1) * 512)
                dve(lambda po=po, sl=sl: V.tensor_tensor(out=res[:, sl], in0=po[:], in1=gate_bc[:, sl], op=ALU.mult), 512, [b_po, b_gbc], [b_res])
            dve(lambda: V.tensor_tensor(out=res[:], in0=res[:], in1=xt[:], op=ALU.add), 1024, [b_res, b_x], [b_res])
            act(lambda: S_.activation(out=ot[:], in_=res[:], func=AF.Square, accum_out=stats[:, 9:10]), 1024, [b_res], [bs["f"], b_o])
            rsqrt_cols(par, "f", 9, 10, 1.0 / D)
            dve(lambda: V.scalar_tensor_tensor(out=ot[:], in0=res[:], scalar=rstd[:, 9:10], in1=fg_bc[:], op0=ALU.mult, op1=ALU.mult),
                1024, [b_res, br["f"], b_fgbc], [b_o])
            kb.dma("sp", f"st{par}", [(out_d[g * 128:(g + 1) * 128, :], ot[:])], reads=[b_o])

        NG = NB * NT
        for g in range(NG):
            kb.tag = g
            load_tile(g)
            tile_body(g)
        kb.run()
        build_nc.last_est_us = kb.est_us
        build_nc.last_kb = kb
    return nc


_CACHE = {}


def kernel(x, c, norm_gain, w_ada, b_ada, w_in, w_gate_up, b_gate_up, gla_norm_gain, w_out, final_gain):
    NB, NT = 4, 16
    f = lambda a: np.ascontiguousarray(np.asarray(a, dtype=np.float32))
    x = f(x)
    c = f(c)
    if "nc" not in _CACHE:
        _CACHE["nc"] = build_nc(NB, NT)
        _CACHE["consts"] = make_consts()
    nc = _CACHE["nc"]
    tab, cst = _CACHE["consts"]
    shared = {
        "norm_gain": f(norm_gain).reshape(1, D), "w_ada": f(w_ada).reshape(D, 3 * D), "b_ada": f(b_ada).reshape(1, 3 * D),
        "w_in": f(w_in).reshape(D, DIN), "w_gate_up": f(w_gate_up).reshape(16, 256), "b_gate_up": f(b_gate_up).reshape(1, 256),
        "gla_norm_gain": f(gla_norm_gain).reshape(1, 128), "w_out": f(w_out).reshape(D, D), "final_gain": f(final_gain).reshape(1, D),
        "tab": tab, "cst": cst,
    }
    in_maps = []
    for i in range(NCORES):
        m = dict(shared)
        m["x"] = x[i * NB:(i + 1) * NB].reshape(NB * SEQ, D)
        m["c"] = c[i * NB:(i + 1) * NB]
        in_maps.append(m)
    res = run_bass_kernel_spmd(nc, in_maps, core_ids=list(range(NCORES)))
    out = np.concatenate([r["out"].reshape(NB, SEQ, D) for r in res.results], axis=0)
    return out.astype(np.float32)
```

```python
import math
from contextlib import ExitStack

import numpy as np
import concourse.bass as bass
import concourse.mybir as mybir
from concourse.bass_utils import run_bass_kernel_spmd

F32 = mybir.dt.float32
BF16 = mybir.dt.bfloat16
AF = mybir.ActivationFunctionType
ALU = mybir.AluOpType

D = 1024
SEQ = 2048
DIN = 3600
NCORES = 8
EPS = 1e-6
G = [1.0 - 2.0 ** (-5.0 - h) for h in range(4)]
import os
SKIP_PE_SELF = os.environ.get('SKIP_PE_SELF', '0') == '1'
SEQ_ORDER = os.environ.get('SEQ_ORDER', '0') == '1'
EVH2 = os.environ.get('EVH2', '0') == '1'
BSPLIT = os.environ.get('BSPLIT', '1') == '1'
LAT_PE = float(os.environ.get('LAT_PE', '0.7'))
LAT_SAME = float(os.environ.get('LAT_SAME', '0.1'))
LAT_X = float(os.environ.get('LAT_X', '0.3'))


class Buf:
    __slots__ = ("name",)

    def __init__(self, name):
        self.name = name


class Rec:
    __slots__ = ("kind", "e", "fn", "reads", "writes", "cost", "sem", "pairs", "kw", "deps", "raw", "start", "tag")


class KB:
    ENG = ("pe", "act", "dve", "pool", "sp")

    def __init__(self, nc, stack):
        self.nc = nc
        self.engs = {"pe": nc.tensor, "act": nc.scalar, "dve": nc.vector, "pool": nc.gpsimd, "sp": nc.sync}
        self.sems = {}
        self.cnt = {}
        self.stack = stack
        for e in self.engs:
            self.sems[e] = stack.enter_context(nc.semaphore("s_" + e))
            self.cnt[e] = 0
        self.seen = {e: {} for e in self.engs}
        self.prog = []
        self.tag = -1
        self.nwaits = 0

    def new_sem(self, name):
        self.sems[name] = self.stack.enter_context(self.nc.semaphore("s_" + name))
        self.cnt[name] = 0
        return name

    def op(self, e, fn, reads=(), writes=(), cost=0.3):
        r = Rec()
        r.kind, r.e, r.fn, r.reads, r.writes, r.cost = "op", e, fn, tuple(reads), tuple(writes), cost
        r.tag = self.tag
        self.prog.append(r)

    def dma(self, e, sem, pairs, reads=(), writes=(), cost=6.0, **kw):
        if sem is None:
            sem = self.new_sem(f"d{len(self.sems)}")
        r = Rec()
        r.kind, r.e, r.reads, r.writes, r.cost = "dma", e, tuple(reads), tuple(writes), cost
        r.sem, r.pairs, r.kw = sem, list(pairs), kw
        r.tag = self.tag
        self.prog.append(r)

    def _wait(self, e, toks):
        best = {}
        for (s, v) in toks:
            if v > best.get(s, 0):
                best[s] = v
        for s, v in best.items():
            if self.seen[e].get(s, 0) < v:
                self.engs[e].wait_ge(self.sems[s], v)
                self.seen[e][s] = v
                self.nwaits += 1

    def run(self):
        prog = self.prog
        N = len(prog)
        lastw, readers = {}, {}
        succ = [[] for _ in range(N)]
        indeg = [0] * N
        for i, r in enumerate(prog):
            raw, other = set(), set()
            for b in r.reads:
                if b in lastw:
                    raw.add(lastw[b])
            for b in r.writes:
                if b in lastw:
                    other.add(lastw[b])
                other.update(readers.get(b, ()))
            deps = (raw | other)
            deps.discard(i)
            r.deps, r.raw = deps, raw
            for d in deps:
                succ[d].append(i)
            indeg[i] = len(deps)
            for b in r.writes:
                lastw[b] = i
                readers[b] = []
            for b in r.reads:
                if b not in r.writes:
                    readers.setdefault(b, []).append(i)
        finish = [0.0] * N
        ready_t = [0.0] * N
        avail = {e: [] for e in self.ENG}
        free = {e: 0.0 for e in self.ENG}
        for i in range(N):
            if indeg[i] == 0:
                avail[prog[i].e].append(i)
        order = []
        while len(order) < N:
            best = None
            for e in self.ENG:
                fe = free[e]
                for i in avail[e]:
                    key = (max(ready_t[i], fe), i)
                    if best is None or key < best[0]:
                        best = (key, e, i)
            (st, _), e, i = best
            avail[e].remove(i)
            r = prog[i]
            r.start = st
            if r.kind == "dma":
                free[e] = st + 0.15 * len(r.pairs)
            else:
                free[e] = st + r.cost
            finish[i] = st + r.cost
            order.append(i)
            for s_ in succ[i]:
                lat = LAT_SAME if prog[s_].e == e and r.kind == "op" else (LAT_PE if prog[s_].e == "pe" else LAT_X)
                if finish[i] + lat > ready_t[s_]:
                    ready_t[s_] = finish[i] + lat
                indeg[s_] -= 1
                if indeg[s_] == 0:
                    avail[prog[s_].e].append(s_)
        self.est_us = max(finish) if N else 0.0
        self.finish_t = finish
        self.order = order
        if SEQ_ORDER:
            order = list(range(N))
        tok = [None] * N
        dma_last = {}
        for i in order:
            r = prog[i]
            waits = []
            for d in r.deps:
                pd = prog[d]
                if pd.kind == "op" and pd.e == r.e == "pe" and SKIP_PE_SELF:
                    continue
                waits.append(tok[d])
            self._wait(r.e, waits)
            if r.kind == "op":
                inst = r.fn()
                self.cnt[r.e] += 1
                inst.then_inc(self.sems[r.e], 1)
                tok[i] = (r.e, self.cnt[r.e])
            else:
                for (o, a) in r.pairs:
                    inst = self.engs[r.e].dma_start(out=o, in_=a, **r.kw)
                    self.cnt[r.sem] += 16
                    inst.then_inc(self.sems[r.sem], 16)
                tok[i] = (r.sem, self.cnt[r.sem])
                dma_last[r.sem] = tok[i]
        for e in ("sp", "pool"):
            self._wait(e, list(dma_last.values()))


def make_consts():
    C = 128
    pos = np.arange(SEQ, dtype=np.float64)
    invf = 10000.0 ** (-np.arange(0, 128, 2, dtype=np.float64) / 128.0)
    ang = pos[:, None] * invf[None, :]
    cos = np.cos(ang).reshape(16, C, 1, 64)
    sin = np.sin(ang).reshape(16, C, 1, 64)
    p = np.arange(C, dtype=np.float64)
    g = np.array(G, dtype=np.float64)
    sq = g[None, :] ** (p[:, None] + 1.0)
    sk = g[None, :] ** (C - 1.0 - p[:, None]) * (128.0 ** -0.5)
    tab = np.concatenate(
        [cos * sq[None, :, :, None], sin * sq[None, :, :, None], cos * sk[None, :, :, None], sin * sk[None, :, :, None]],
        axis=2,
    )
    tab = tab.reshape(16, C, 1024).astype(np.float32)
    j = np.arange(C)[:, None]
    i = np.arange(C)[None, :]
    caus = (i >= j).astype(np.float64)
    mask_r = np.stack([caus * g[h] ** (-float(C)) for h in range(4)], axis=1).reshape(C, 512)
    Lp = (j <= i).astype(np.float64) * (-1.0 / 16.0)
    Up = (j > i).astype(np.float64) * (-1.0 / 16.0)
    ident = np.eye(C)
    cst = np.concatenate([mask_r, caus, Lp, Up, ident], axis=1).astype(np.float32)
    return tab, cst


def build_nc(NB=4, NT=16):
    NTOK = NB * NT * 128
    nc = bass.Bass("TRN2", target_bir_lowering=False)
    dr = lambda name, shape, kind="ExternalInput": nc.dram_tensor(name, shape, F32, kind=kind).ap()
    x_d = dr("x", [NTOK, D])
    c_d = dr("c", [NB, D])
    ng_d = dr("norm_gain", [1, D])
    wada_d = dr("w_ada", [D, 3 * D])
    bada_d = dr("b_ada", [1, 3 * D])
    win_d = dr("w_in", [D, DIN])
    wg_d = dr("w_gate_up", [16, 256])
    bg_d = dr("b_gate_up", [1, 256])
    gng_d = dr("gla_norm_gain", [1, 128])
    wout_d = dr("w_out", [D, D])
    fg_d = dr("final_gain", [1, D])
    tab_d = dr("tab", [16, 128, 1024])
    cst_d = dr("cst", [128, 1024])
    out_d = dr("out", [NTOK, D], kind="ExternalOutput")

    with ExitStack() as st:
        kb = KB(nc, st)
        _n = [0]

        def sb(shape, dt, name):
            t = st.enter_context(nc.sbuf_tensor("S_" + name, shape, dt))
            return t, Buf(name)

        def sbn(n, shape, dt, name):
            return [sb(shape, dt, f"{name}{i}") for i in range(n)]

        def ps(shape, dt, name):
            t = st.enter_context(nc.psum_tensor(name, shape, dt))
            return t, Buf(name)

        V, S_, P_, T_ = nc.vector, nc.scalar, nc.gpsimd, nc.tensor

        def dve(fn, n, reads, writes, mode=1.0):
            kb.op("dve", fn, reads, writes, cost=(n / mode + 150.0) / 960.0)

        def act(fn, n, reads, writes):
            kb.op("act", fn, reads, writes, cost=(n + 260.0) / 1200.0)

        def pe(fn, cols, reads, writes):
            kb.op("pe", fn, reads, writes, cost=cols / 2000.0 + 0.06)

        def pool(fn, cost, reads, writes):
            kb.op("pool", fn, reads, writes, cost=cost)

        w_in_bf, b_win = sb([128, 8, DIN], BF16, "w_in_bf")
        w_out_bf, b_wout = sb([128, 8, D], BF16, "w_out_bf")
        cst, b_cst = sb([128, 1024], F32, "cst")
        ident_bf, b_idbf = sb([128, 128], BF16, "ident_bf")
        mask_r = cst[:, 0:512]
        mask_g = cst[:, 512:640]
        Lp = cst[:, 640:768]
        Up = cst[:, 768:896]
        identf = cst[:, 896:1024]
        xs = sbn(3, [128, D], F32, "xs")
        tabs = sbn(2, [128, 1024], F32, "tabs")
        outs = sbn(2, [128, D], F32, "outs")
        xn_ = sbn(2, [128, D], BF16, "xn")
        hT = sbn(2, [128, 8, 128], BF16, "hT")
        b_hTb = [Buf("hTb0"), Buf("hTb1")]
        rA, b_rA = sb([128, 512], F32, "rA")
        rB, b_rB = sb([128, 512], F32, "rB")
        q_r_ = sbn(2, [128, 512], BF16, "q_r")
        k_r_ = sbn(2, [128, 512], BF16, "k_r")
        v_bf_ = sbn(2, [128, 512], BF16, "v_bf")
        gv_bf_ = sbn(2, [128, 512], BF16, "gv_bf")
        szr_ = sbn(2, [128, 512], F32, "szr")
        szg_ = sbn(2, [128, 512], F32, "szg")
        qkT_ = sbn(2, [128, 8, 128], BF16, "qkT")
        geT_ = sbn(2, [128, 6, 128], BF16, "geT")
        q_e_ = sbn(2, [128, 256], BF16, "q_e")
        k_e_ = sbn(2, [128, 256], BF16, "k_e")
        k_l_ = sbn(2, [128, 256], BF16, "k_l")
        e_sb, b_e = sb([128, 256], F32, "e_sb")
        l_sb_ = sbn(2, [128, 256], F32, "l_sb")
        E1_ = sbn(2, [128, 256], F32, "E1")
        E2_ = sbn(2, [128, 256], F32, "E2")
        E3_ = sbn(2, [128, 256], F32, "E3")
        a_fm_ = sbn(2, [128, 2], F32, "a_fm")
        glrT_ = sbn(2, [32, 128], BF16, "glrT")
        wg_aug, b_wg = sb([32, 256], BF16, "wg_aug")
        ST_r_ = sbn(2, [128, 512], BF16, "ST_r")
        ST_g_ = sbn(2, [128, 512], BF16, "ST_g")
        R, b_R = sb([128, 4, 128], F32, "R")
        R_bf, b_Rbf = sb([128, 4, 128], BF16, "R_bf")
        Sg, b_Sg = sb([128, 2, 128], F32, "Sg")
        Sg_bf, b_Sgbf = sb([128, 2, 128], BF16, "Sg_bf")
        mix_ = sbn(2, [128, D], BF16, "mix")
        mixT_ = sbn(2, [128, 8, 128], BF16, "mixT")
        res_ = sbn(2, [128, D], F32, "res")
        gate_bc, b_gbc = sb([128, D], F32, "gate_bc")
        fg_bc, b_fgbc = sb([128, D], F32, "fg_bc")
        grep_, b_grep = sb([128, 8, 128], F32, "grep")
        stats_ = [st.enter_context(nc.sbuf_tensor(f"S_stats{i}", [128, 16], F32)) for i in range(2)]
        msq_ = [st.enter_context(nc.sbuf_tensor(f"S_msq{i}", [128, 16], F32)) for i in range(2)]
        rstd_ = [st.enter_context(nc.sbuf_tensor(f"S_rstd{i}", [128, 16], F32)) for i in range(2)]
        b_stats = [{k: Buf(f"stats{i}{k}") for k in "xyf"} for i in range(2)]
        b_msq = [{k: Buf(f"msq{i}{k}") for k in "xyf"} for i in range(2)]
        b_rstd = [{k: Buf(f"rstd{i}{k}") for k in "xyf"} for i in range(2)]
        eps_t, b_eps = sb([128, 1], F32, "eps_t")
        m16, b_m16 = sb([128, 2], F32, "m16")
        cT, b_cT = sb([128, 8, NB], F32, "cT")
        ng_fm, b_ngfm = sb([128, 8], F32, "ng_fm")
        bada_fm, b_badafm = sb([128, 24], F32, "bada_fm")
        gng_fm, b_gngfm = sb([128, 1], F32, "gng_fm")
        ada_fm, b_ada = sb([128, 24, NB], F32, "ada_fm")
        A_fm, b_Afm = sb([128, 8, NB], F32, "A_fm")

        headb = [ps([128, 512], F32, f"phead{i}") for i in range(2)]
        tailb = [ps([128, 512], F32, f"ptail{i}") for i in range(3)]
        pT_ = [ps([128, 8, 128], BF16, f"pT{i}") for i in range(2)]
        pM, b_pM = ps([128, 512], F32, "pM")
        _rr = {"h": 0, "t": 0}

        def bank(kind="t"):
            pool_ = headb if kind == "h" else tailb
            t = pool_[_rr[kind] % len(pool_)]
            _rr[kind] += 1
            return t

        for s in ["ld_x0", "ld_x1", "ld_x2", "ld_t0", "ld_t1", "ld_s0", "ld_s1", "st0", "st1"]:
            kb.new_sem(s)

        kb.dma("sp", None, [(cst[:], cst_d[:, :])], writes=[b_cst])
        kb.dma("sp", None, [(cT[:, :, b], c_d[b:b + 1, :].rearrange("o (k p) -> p (o k)", p=128)) for b in range(NB)],
               writes=[b_cT], allow_slow_non_contiguous=True)
        kb.dma("sp", None, [(ng_fm[:], ng_d.rearrange("o (k p) -> p (o k)", p=128))], writes=[b_ngfm],
               allow_slow_non_contiguous=True)
        kb.dma("sp", None, [(bada_fm[:, j * 8:(j + 1) * 8], bada_d[:, j * 1024:(j + 1) * 1024].rearrange("o (k p) -> p (o k)", p=128))
                            for j in range(3)], writes=[b_badafm], allow_slow_non_contiguous=True)
        kb.dma("sp", None, [(gng_fm[:], gng_d.rearrange("o (p one) -> (o p) one", one=1))], writes=[b_gngfm],
               allow_slow_non_contiguous=True)
        kb.dma("sp", None, [(fg_bc[:], fg_d[0:1, :].to_broadcast([128, D]))], writes=[b_fgbc])
        kb.dma("pool", None, [(w_in_bf[:, kc, :], win_d[kc * 128:(kc + 1) * 128, :]) for kc in range(8)],
               writes=[b_win], cost=90.0, max_dma_last_dim=4096)
        kb.dma("pool", None, [(w_out_bf[:, kc, :], wout_d[kc * 128:(kc + 1) * 128, :]) for kc in range(4)],
               writes=[b_wout], cost=20.0, max_dma_last_dim=4096)
        kb.dma("pool", None, [(wg_aug[0:16, :], wg_d[:, :]), (wg_aug[16:17, :], bg_d[:, :])], writes=[b_wg])

        dve(lambda: V.memset(eps_t[:], EPS), 1, [], [b_eps])
        dve(lambda: V.memset(m16[:], -1.0 / 16.0), 2, [], [b_m16])
        for i in range(2):
            dve(lambda i=i: V.memset(glrT_[i][0][:], 1.0), 128, [], [glrT_[i][1]])
            dve(lambda i=i: V.memset(geT_[i][0][:], 0.0), 768, [], [geT_[i][1]])
        dve(lambda: V.tensor_copy(out=ident_bf[:], in_=identf), 128, [b_cst], [b_idbf])

        for kc in range(4, 8):
            stg, b_stg = outs[kc % 2]
            kb.dma("sp", f"ld_s{kc % 2}", [(stg[:], wout_d[kc * 128:(kc + 1) * 128, :])], writes=[b_stg])
            dve(lambda kc=kc, stg=stg: V.tensor_scalar(out=w_out_bf[:, kc, :], in0=stg[:], scalar1=gng_fm[:, 0:1], scalar2=None, op0=ALU.mult),
                1024, [b_stg, b_gngfm], [b_wout])

        act(lambda: S_.activation(out=cT[:], in_=cT[:], func=AF.Silu), 8 * NB, [b_cT], [b_cT])
        for j in range(24):
            stg, b_stg = outs[j % 2]
            stg3 = stg[:].rearrange("p (k c) -> p k c", k=8)
            kb.dma("sp", f"ld_s{j % 2}", [(stg3, wada_d[:, j * 128:(j + 1) * 128].rearrange("(k p) c -> p k c", p=128))], writes=[b_stg])

            def mm_ada(stg3=stg3):
                for kc in range(8):
                    i = T_.matmul(pM[:, 0:NB], lhsT=stg3[:, kc, :], rhs=cT[:, kc, :], start=(kc == 0), stop=(kc == 7))
                return i
            pe(mm_ada, 8 * 4 * 128, [b_stg, b_cT], [b_pM])
            dve(lambda j=j: V.tensor_scalar(out=ada_fm[:, j, :], in0=pM[:, 0:NB], scalar1=bada_fm[:, j:j + 1], scalar2=None, op0=ALU.add),
                NB, [b_pM, b_badafm], [b_ada])
        dve(lambda: V.tensor_scalar(out=A_fm[:], in0=ada_fm[:, 8:16, :], scalar1=1.0, scalar2=None, op0=ALU.add), 8 * NB, [b_ada], [b_Afm])
        dve(lambda: V.tensor_tensor(out=A_fm[:], in0=A_fm[:], in1=ng_fm[:].unsqueeze(2).to_broadcast([128, 8, NB]), op=ALU.mult),
            8 * NB, [b_Afm, b_ngfm], [b_Afm])

        cd = [G[h] ** 128.0 for h in range(4)]
        LN_EIGHTH = math.log(0.125)

        def rsqrt_cols(par, grp, c0, c1, inv_n):
            stt, msq, rstd = stats_[par], msq_[par], rstd_[par]

            act(lambda: S_.activation(out=msq[:, c0:c1], in_=stt[:, c0:c1], func=AF.Ln, scale=inv_n, bias=eps_t[:, 0:1]),
                c1 - c0, [b_stats[par][grp], b_eps], [b_msq[par][grp]])
            act(lambda: S_.activation(out=rstd[:, c0:c1], in_=msq[:, c0:c1], func=AF.Exp, scale=-0.5),
                c1 - c0, [b_msq[par][grp]], [b_rstd[par][grp]])

        def load_tile(g):
            t = g % NT
            xt, b_x = xs[g % 3]
            tt, b_t = tabs[g % 2]
            kb.dma("sp", f"ld_x{g % 3}", [(xt[:], x_d[g * 128:(g + 1) * 128, :])], writes=[b_x])
            kb.dma("sp", f"ld_t{g % 2}", [(tt[:], tab_d[t, :, :])], writes=[b_t])

        def seq_start(b):
            dve(lambda: V.memset(R[:], 0.0), 512, [], [b_R])
            dve(lambda: V.memset(R_bf[:], 0.0), 512, [], [b_Rbf], mode=2.0)
            dve(lambda: V.memset(Sg[:], 0.0), 256, [], [b_Sg])
            dve(lambda: V.memset(Sg_bf[:], 0.0), 256, [], [b_Sgbf], mode=2.0)
            dve(lambda: V.tensor_copy(out=grep_[:], in_=ada_fm[:, 16:24, b:b + 1].to_broadcast([128, 8, 128])), 1024, [b_ada], [b_grep])
            for n in range(2):
                pb, b_pb = tailb[1 + n]

                def mm_g(n=n, pb=pb):
                    for q in range(4):
                        kc = n * 4 + q
                        i = T_.matmul(pb[:, q * 128:(q + 1) * 128], lhsT=grep_[:, kc, :], rhs=identf, start=True, stop=True)
                    return i
                pe(mm_g, 4 * 4 * 128, [b_grep, b_cst], [b_pb])
                act(lambda n=n, pb=pb: S_.copy(out=gate_bc[:, n * 512:(n + 1) * 512], in_=pb[:]), 512, [b_pb], [b_gbc])

        def rotary(pb, b_pb, tt, b_t, toff, dst, b_dst):
            src4 = pb[:].rearrange("p (h two f) -> p h two f", h=4, two=2)
            cs4 = tt[:, toff:toff + 256].rearrange("p (h f) -> p h f", h=4).unsqueeze(2).to_broadcast([128, 4, 2, 64])
            sn4 = tt[:, toff + 256:toff + 512].rearrange("p (h f) -> p h f", h=4).unsqueeze(2).to_broadcast([128, 4, 2, 64])
            A4 = rA[:].rearrange("p (h two f) -> p h two f", h=4, two=2)
            B4 = rB[:].rearrange("p (h two f) -> p h two f", h=4, two=2)
            d4 = dst[:].rearrange("p (h two f) -> p h two f", h=4, two=2)
            dve(lambda: V.tensor_tensor(out=A4, in0=src4, in1=cs4, op=ALU.mult), 512, [b_pb, b_t], [b_rA])
            dve(lambda: V.tensor_tensor(out=B4, in0=src4, in1=sn4, op=ALU.mult), 512, [b_pb, b_t], [b_rB])
            dve(lambda: V.tensor_tensor(out=d4[:, :, 0, :], in0=A4[:, :, 0, :], in1=B4[:, :, 1, :], op=ALU.subtract), 256, [b_rA, b_rB], [b_dst])
            dve(lambda: V.tensor_tensor(out=d4[:, :, 1, :], in0=B4[:, :, 0, :], in1=A4[:, :, 1, :], op=ALU.add), 256, [b_rA, b_rB], [b_dst])

        def inproj_group(hTt, b_hT, c0, ncols):
            pb, b_pb = bank("h")

            def mm():
                for kc in range(8):
                    i = T_.matmul(pb[:, 0:ncols], lhsT=hTt[:, kc, :], rhs=w_in_bf[:, kc, c0:c0 + ncols], start=(kc == 0), stop=(kc == 7))
                return i
            pe(mm, 8 * ncols, list(b_hT) + [b_win], [b_pb])
            return pb, b_pb

        def tile_body(g):
            b = g // NT
            t = g % NT
            par = g % 2
            xt, b_x = xs[g % 3]
            tt, b_t = tabs[par]
            hTt, b_hT = hT[par]
            ot, b_o = outs[par]
            xn, b_xn = xn_[par]
            q_r, b_qr = q_r_[par]
            k_r, b_kr = k_r_[par]
            v_bf, b_v = v_bf_[par]
            gv_bf, b_gv = gv_bf_[par]
            szr, b_szr = szr_[par]
            szg, b_szg = szg_[par]
            qkT, b_qkT = qkT_[par]
            geT, b_geT = geT_[par]
            q_e, b_qe = q_e_[par]
            k_e, b_ke = k_e_[par]
            k_l, b_kl = k_l_[par]
            l_sb, b_l = l_sb_[par]
            E1, b_E1 = E1_[par]
            E2, b_E2 = E2_[par]
            E3, b_E3 = E3_[par]
            a_fm, b_afm = a_fm_[par]
            glrT, b_glrT = glrT_[par]
            ST_r, b_STr = ST_r_[par]
            ST_g, b_STg = ST_g_[par]
            mix, b_mix = mix_[par]
            mixT, b_mixT = mixT_[par]
            res, b_res = res_[par]
            stats, rstd = stats_[par], rstd_[par]
            bs, br = b_stats[par], b_rstd[par]
            pT, b_pT = pT_[par]
            if t == 0:
                seq_start(b)
            act(lambda: S_.activation(out=xn[:], in_=xt[:], func=AF.Square, accum_out=stats[:, 0:1]), 1024, [b_x], [bs["x"], b_xn])
            rsqrt_cols(par, "x", 0, 1, 1.0 / D)
            act(lambda: S_.activation(out=xn[:], in_=xt[:], func=AF.Copy, scale=rstd[:, 0:1]), 1024, [b_x, br["x"]], [b_xn])
            def tr_x():
                for kc in range(8):
                    i = T_.transpose(out=pT[:, kc, :], in_=xn[:, kc * 128:(kc + 1) * 128], identity=ident_bf[:])
                return i
            pe(tr_x, 1024, [b_xn, b_idbf], [b_pT])

            b_hT2 = b_hTb[par]

            def ev_h():
                for kc in range(4):
                    i = S_.activation(out=hTt[:, kc, :], in_=pT[:, kc, :], func=AF.Identity,
                                      scale=A_fm[:, kc, b:b + 1], bias=ada_fm[:, kc, b:b + 1])
                return i
            kb.op("act", ev_h, [b_pT, b_Afm, b_ada], [b_hT], cost=4 * 0.38)

            def ev_h2():
                for kc in range(4, 8):
                    i = V.tensor_scalar(out=hTt[:, kc, :], in0=pT[:, kc, :], scalar1=A_fm[:, kc, b:b + 1], scalar2=ada_fm[:, kc, b:b + 1],
                                        op0=ALU.mult, op1=ALU.add)
                return i
            def ev_h2a():
                for kc in range(4, 8):
                    i = S_.activation(out=hTt[:, kc, :], in_=pT[:, kc, :], func=AF.Identity,
                                      scale=A_fm[:, kc, b:b + 1], bias=ada_fm[:, kc, b:b + 1])
                return i
            if EVH2:
                kb.op("dve", ev_h2, [b_pT, b_Afm, b_ada], [b_hT2], cost=4 * 0.25)
            else:
                kb.op("act", ev_h2a, [b_pT, b_Afm, b_ada], [b_hT2], cost=4 * 0.38)
            def mm_glr():
                for kc in range(8):
                    i = T_.matmul(pM[0:16, 256:384], lhsT=w_in_bf[:, kc, 3584:3600], rhs=hTt[:, kc, :], start=(kc == 0), stop=(kc == 7))
                return i
            pe(mm_glr, 1024, [b_hT, b_hT2, b_win], [b_pM])
            act(lambda: S_.copy(out=glrT[0:16, :], in_=pM[0:16, 256:384]), 128, [b_pM], [b_glrT])
            pe(lambda: T_.matmul(pM[:, 0:256], lhsT=glrT[0:17, :], rhs=wg_aug[0:17, :], start=True, stop=True), 256, [b_glrT, b_wg], [b_pM])
            act(lambda: S_.activation(out=e_sb[:], in_=pM[:, 0:256], func=AF.Exp, scale=-1.0), 256, [b_pM], [b_e])
            act(lambda: S_.activation(out=l_sb[:], in_=e_sb[:], func=AF.Ln, bias=1.0), 256, [b_e], [b_l])
            pq, b_pq = inproj_group(hTt, (b_hT, b_hT2), 0, 512)
            rotary(pq, b_pq, tt, b_t, 0, q_r, b_qr)
            pk, b_pk = inproj_group(hTt, (b_hT, b_hT2), 512, 512)
            rotary(pk, b_pk, tt, b_t, 512, k_r, b_kr)

            def mm_bl():
                T_.matmul(pM[:, 384:385], lhsT=l_sb[:, 0:128], rhs=m16[:, 0:1], start=True, stop=True)
                i = T_.matmul(pM[:, 385:386], lhsT=l_sb[:, 128:256], rhs=m16[:, 0:1], start=True, stop=True)
                return i
            if BSPLIT:
                pe(mm_bl, 512, [b_l, b_m16], [b_pM])
                pe(lambda: T_.matmul(pM[:, 0:256], lhsT=Lp, rhs=l_sb[:], start=True, stop=True), 1024, [b_l, b_cst], [b_pM])
            else:
                def mm_b():
                    mm_bl()
                    return T_.matmul(pM[:, 0:256], lhsT=Lp, rhs=l_sb[:], start=True, stop=True)
                pe(mm_b, 512 + 1024, [b_l, b_m16, b_cst], [b_pM])
            act(lambda: S_.activation(out=E1[:], in_=pM[:, 0:256], func=AF.Exp, bias=LN_EIGHTH), 256, [b_pM], [b_E1])
            act(lambda: S_.activation(out=E2[:], in_=pM[:, 0:256], func=AF.Exp, scale=-1.0), 256, [b_pM], [b_E2])
            act(lambda: S_.activation(out=a_fm[:], in_=pM[:, 384:386], func=AF.Exp), 2, [b_pM], [b_afm])
            pg, b_pg = inproj_group(hTt, (b_hT, b_hT2), 2048, 512)
            dve(lambda: V.tensor_tensor(out=q_e[:], in0=pg[:, 0:256], in1=E1[:], op=ALU.mult), 256, [b_pg, b_E1], [b_qe])
            dve(lambda: V.tensor_tensor(out=k_e[:], in0=pg[:, 256:512], in1=E2[:], op=ALU.mult), 256, [b_pg, b_E2], [b_ke])
            pv, b_pv = inproj_group(hTt, (b_hT, b_hT2), 1024, 512)
            act(lambda: S_.copy(out=v_bf[:], in_=pv[:]), 512, [b_pv], [b_v])
            pgv, b_pgv = inproj_group(hTt, (b_hT, b_hT2), 2560, 512)
            act(lambda: S_.copy(out=gv_bf[:], in_=pgv[:]), 512, [b_pgv], [b_gv])
            pz, b_pz = inproj_group(hTt, (b_hT, b_hT2), 1536, 512)
            pgz, b_pgz = inproj_group(hTt, (b_hT, b_hT2), 3072, 512)

            def silus():
                S_.activation(out=szr[:], in_=pz[:], func=AF.Silu)
                return S_.activation(out=szg[:], in_=pgz[:], func=AF.Silu)
            kb.op("act", silus, [b_pz, b_pgz], [b_szr, b_szg], cost=2 * 0.64)
            def tr_qk():
                for h in range(4):
                    T_.transpose(out=pT[:, h, :], in_=q_r[:, h * 128:(h + 1) * 128], identity=ident_bf[:])
                for h in range(4):
                    i = T_.transpose(out=pT[:, 4 + h, :], in_=k_r[:, h * 128:(h + 1) * 128], identity=ident_bf[:])
                return i
            pe(tr_qk, 1024, [b_qr, b_kr, b_idbf], [b_pT])
            dve(lambda: V.tensor_copy(out=qkT[:], in_=pT[:]), 1024, [b_pT], [b_qkT], mode=2.0)
            def tr_ge():
                for u in range(2):
                    T_.transpose(out=pT[:, u, :], in_=q_e[:, u * 128:(u + 1) * 128], identity=ident_bf[:])
                for u in range(2):
                    i = T_.transpose(out=pT[:, 2 + u, :], in_=k_e[:, u * 128:(u + 1) * 128], identity=ident_bf[:])
                return i
            pe(tr_ge, 512, [b_qe, b_ke, b_idbf], [b_pT])

            def ev_ge():
                for h in range(4):
                    r0 = (h % 2) * 64
                    V.tensor_copy(out=geT[r0:r0 + 64, h, :], in_=pT[r0:r0 + 64, h // 2, :])
                return V.tensor_copy(out=geT[:, 4:6, :], in_=pT[:, 2:4, :])
            kb.op("dve", ev_ge, [b_pT], [b_geT], cost=5 * 0.25)
            (T0, bT0), (T1, bT1), (T2, bT2) = tailb
            psr, b_psr = T0, bT0
            psg, b_psg = T1, bT1
            pyr, b_pyr = T2, bT2
            pkr, b_pkr = T0, bT0
            pyg, b_pyg = T1, bT1
            pkg, b_pkg = T0, bT0

            def mm_sr():
                for h in range(4):
                    i = T_.matmul(psr[:, h * 128:(h + 1) * 128], lhsT=qkT[:, 4 + h, :], rhs=qkT[:, h, :], start=True, stop=True)
                return i
            pe(mm_sr, 512, [b_qkT], [b_psr])
            dve(lambda: V.tensor_tensor(out=ST_r[:], in0=psr[:], in1=mask_r, op=ALU.mult), 512, [b_psr, b_cst], [b_STr])

            def mm_sg():
                for h in range(4):
                    i = T_.matmul(psg[:, h * 128:(h + 1) * 128], lhsT=geT[:, 4 + h // 2, :], rhs=geT[:, h, :], start=True, stop=True)
                return i
            pe(mm_sg, 512, [b_geT], [b_psg])
            dve(lambda: V.tensor_tensor(out=ST_g[:].rearrange("p (h i) -> p h i", h=4), in0=psg[:].rearrange("p (h i) -> p h i", h=4),
                                        in1=mask_g.unsqueeze(1).to_broadcast([128, 4, 128]), op=ALU.mult), 512, [b_psg, b_cst], [b_STg])

            def mm_yr():
                for h in range(4):
                    T_.matmul(pyr[:, h * 128:(h + 1) * 128], lhsT=ST_r[:, h * 128:(h + 1) * 128], rhs=v_bf[:, h * 128:(h + 1) * 128],
                              start=True, stop=False)
                    i = T_.matmul(pyr[:, h * 128:(h + 1) * 128], lhsT=qkT[:, h, :], rhs=R_bf[:, h, :], start=False, stop=True)
                return i
            pe(mm_yr, 1024, [b_STr, b_v, b_qkT, b_Rbf], [b_pyr])

            def mm_kr():
                for h in range(4):
                    i = T_.matmul(pkr[:, h * 128:(h + 1) * 128], lhsT=k_r[:, h * 128:(h + 1) * 128], rhs=v_bf[:, h * 128:(h + 1) * 128],
                                  start=True, stop=True)
                return i
            pe(mm_kr, 512, [b_kr, b_v], [b_pkr])

            def up_R():
                for h in range(4):
                    i = V.scalar_tensor_tensor(out=R[:, h, :], in0=R[:, h, :], scalar=cd[h], in1=pkr[:, h * 128:(h + 1) * 128],
                                               op0=ALU.mult, op1=ALU.add)
                return i
            kb.op("dve", up_R, [b_pkr], [b_R], cost=4 * 0.3)
            act(lambda: S_.copy(out=R_bf[:], in_=R[:]), 512, [b_R], [b_Rbf])

            def mm_yg():
                for h in range(4):
                    T_.matmul(pyg[:, h * 128:(h + 1) * 128], lhsT=ST_g[:, h * 128:(h + 1) * 128], rhs=gv_bf[:, h * 128:(h + 1) * 128],
                              start=True, stop=False)
                    i = T_.matmul(pyg[:, h * 128:(h + 1) * 128], lhsT=geT[:, h, :], rhs=Sg_bf[:, h // 2, :], start=False, stop=True)
                return i
            pe(mm_yg, 1024, [b_STg, b_gv, b_geT, b_Sgbf], [b_pyg])

            def mm_kg():
                for h in range(4):
                    r0 = (h % 2) * 64
                    u = h // 2
                    i = T_.matmul(pkg[r0:r0 + 64, u * 128:(u + 1) * 128], lhsT=k_e[:, h * 64:(h + 1) * 64], rhs=gv_bf[:, h * 128:(h + 1) * 128],
                                  start=True, stop=True)
                return i
            pe(mm_kg, 512, [b_ke, b_gv], [b_pkg])
            Sg2 = Sg[:].rearrange("p u e -> p (u e)")
            dve(lambda: V.tensor_tensor(out=Sg2, in0=Sg2, in1=pkg[:, 0:256], op=ALU.add), 256, [b_pkg], [b_Sg])
            dve(lambda: V.tensor_tensor(out=Sg[:], in0=Sg[:], in1=a_fm[:].unsqueeze(2).to_broadcast([128, 2, 128]), op=ALU.mult), 256, [b_afm], [b_Sg])
            act(lambda: S_.copy(out=Sg_bf[:], in_=Sg[:]), 256, [b_Sg], [b_Sgbf])
            def sq_y():
                for h in range(4):
                    S_.activation(out=mix[:, h * 128:(h + 1) * 128], in_=pyr[:, h * 128:(h + 1) * 128], func=AF.Square, accum_out=stats[:, 1 + h:2 + h])
                for h in range(4):
                    i = S_.activation(out=mix[:, 512 + h * 128:512 + (h + 1) * 128], in_=pyg[:, h * 128:(h + 1) * 128], func=AF.Square, accum_out=stats[:, 5 + h:6 + h])
                return i
            kb.op("act", sq_y, [b_pyr, b_pyg], [bs["y"], b_mix], cost=8 * 0.42)
            rsqrt_cols(par, "y", 1, 9, 1.0 / 128.0)

            def mk_mix():
                for h in range(4):
                    V.scalar_tensor_tensor(out=mix[:, h * 128:(h + 1) * 128], in0=pyr[:, h * 128:(h + 1) * 128], scalar=rstd[:, 1 + h:2 + h],
                                           in1=szr[:, h * 128:(h + 1) * 128], op0=ALU.mult, op1=ALU.mult)
                for h in range(4):
                    i = V.scalar_tensor_tensor(out=mix[:, 512 + h * 128:512 + (h + 1) * 128], in0=pyg[:, h * 128:(h + 1) * 128],
                                               scalar=rstd[:, 5 + h:6 + h], in1=szg[:, h * 128:(h + 1) * 128], op0=ALU.mult, op1=ALU.mult)
                return i
            kb.op("dve", mk_mix, [b_pyr, b_pyg, br["y"], b_szr, b_szg], [b_mix], cost=8 * 0.3)
            def tr_m():
                for kc in range(8):
                    i = T_.transpose(out=pT[:, kc, :], in_=mix[:, kc * 128:(kc + 1) * 128], identity=ident_bf[:])
                return i
            pe(tr_m, 1024, [b_mix, b_idbf], [b_pT])
            dve(lambda: V.tensor_copy(out=mixT[:], in_=pT[:]), 1024, [b_pT], [b_mixT], mode=2.0)
            for n in range(2):
                po, b_po = tailb[0] if n == 0 else tailb[2]

                def mm_o(n=n, po=po):
                    for kc in range(8):
                        i = T_.matmul(po[:], lhsT=mixT[:, kc, :], rhs=w_out_bf[:, kc, n * 512:(n + 1) * 512], start=(kc == 0), stop=(kc == 7))
                    return i
                pe(mm_o, 8 * 512, [b_mixT, b_wout], [b_po])
                sl = slice(n * 512, (n + 1) * 512)
                dve(lambda po=po, sl=sl: V.tensor_tensor(out=res[:, sl], in0=po[:], in1=gate_bc[:, sl], op=ALU.mult), 512, [b_po, b_gbc], [b_res])
            dve(lambda: V.tensor_tensor(out=res[:], in0=res[:], in1=xt[:], op=ALU.add), 1024, [b_res, b_x], [b_res])
            act(lambda: S_.activation(out=ot[:], in_=res[:], func=AF.Square, accum_out=stats[:, 9:10]), 1024, [b_res], [bs["f"], b_o])
            rsqrt_cols(par, "f", 9, 10, 1.0 / D)
            dve(lambda: V.scalar_tensor_tensor(out=ot[:], in0=res[:], scalar=rstd[:, 9:10], in1=fg_bc[:], op0=ALU.mult, op1=ALU.mult),
                1024, [b_res, br["f"], b_fgbc], [b_o])
            kb.dma("sp", f"st{par}", [(out_d[g * 128:(g + 1) * 128, :], ot[:])], reads=[b_o])

        NG = NB * NT
        for g in range(NG):
            kb.tag = g
            load_tile(g)
            tile_body(g)
        kb.run()
        build_nc.last_est_us = kb.est_us
        build_nc.last_kb = kb
    return nc


_CACHE = {}


def kernel(x, c, norm_gain, w_ada, b_ada, w_in, w_gate_up, b_gate_up, gla_norm_gain, w_out, final_gain):
    NB, NT = 4, 16
    f = lambda a: np.ascontiguousarray(np.asarray(a, dtype=np.float32))
    x = f(x)
    c = f(c)
    if "nc" not in _CACHE:
        _CACHE["nc"] = build_nc(NB, NT)
        _CACHE["consts"] = make_consts()
    nc = _CACHE["nc"]
    tab, cst = _CACHE["consts"]
    shared = {
        "norm_gain": f(norm_gain).reshape(1, D), "w_ada": f(w_ada).reshape(D, 3 * D), "b_ada": f(b_ada).reshape(1, 3 * D),
        "w_in": f(w_in).reshape(D, DIN), "w_gate_up": f(w_gate_up).reshape(16, 256), "b_gate_up": f(b_gate_up).reshape(1, 256),
        "gla_norm_gain": f(gla_norm_gain).reshape(1, 128), "w_out": f(w_out).reshape(D, D), "final_gain": f(final_gain).reshape(1, D),
        "tab": tab, "cst": cst,
    }
    in_maps = []
    for i in range(NCORES):
        m = dict(shared)
        m["x"] = x[i * NB:(i + 1) * NB].reshape(NB * SEQ, D)
        m["c"] = c[i * NB:(i + 1) * NB]
        in_maps.append(m)
    res = run_bass_kernel_spmd(nc, in_maps, core_ids=list(range(NCORES)))
    out = np.concatenate([r["out"].reshape(NB, SEQ, D) for r in res.results], axis=0)
    return out.astype(np.float32)
```

```python
import math
from contextlib import ExitStack

import numpy as np
import concourse.bass as bass
import concourse.mybir as mybir
from concourse.bass_utils import run_bass_kernel_spmd

F32 = mybir.dt.float32
BF16 = mybir.dt.bfloat16
AF = mybir.ActivationFunctionType
ALU = mybir.AluOpType

D = 1024
SEQ = 2048
DIN = 3600
NCORES = 8
EPS = 1e-6
G = [1.0 - 2.0 ** (-5.0 - h) for h in range(4)]
import os
SKIP_PE_SELF = os.environ.get('SKIP_PE_SELF', '0') == '1'
SEQ_ORDER = os.environ.get('SEQ_ORDER', '0') == '1'
EVH2 = os.environ.get('EVH2', '0') == '1'
BSPLIT = os.environ.get('BSPLIT', '1') == '1'
LAT_PE = float(os.environ.get('LAT_PE', '0.7'))
LAT_SAME = float(os.environ.get('LAT_SAME', '0.1'))
LAT_X = float(os.environ.get('LAT_X', '0.3'))


class Buf:
    __slots__ = ("name",)

    def __init__(self, name):
        self.name = name


class Rec:
    __slots__ = ("kind", "e", "fn", "reads", "writes", "cost", "sem", "pairs", "kw", "deps", "raw", "start", "tag")


class KB:
    ENG = ("pe", "act", "dve", "pool", "sp")

    def __init__(self, nc, stack):
        self.nc = nc
        self.engs = {"pe": nc.tensor, "act": nc.scalar, "dve": nc.vector, "pool": nc.gpsimd, "sp": nc.sync}
        self.sems = {}
        self.cnt = {}
        self.stack = stack
        for e in self.engs:
            self.sems[e] = stack.enter_context(nc.semaphore("s_" + e))
            self.cnt[e] = 0
        self.seen = {e: {} for e in self.engs}
        self.prog = []
        self.tag = -1
        self.nwaits = 0

    def new_sem(self, name):
        self.sems[name] = self.stack.enter_context(self.nc.semaphore("s_" + name))
        self.cnt[name] = 0
        return name

    def op(self, e, fn, reads=(), writes=(), cost=0.3):
        r = Rec()
        r.kind, r.e, r.fn, r.reads, r.writes, r.cost = "op", e, fn, tuple(reads), tuple(writes), cost
        r.tag = self.tag
        self.prog.append(r)

    def dma(self, e, sem, pairs, reads=(), writes=(), cost=6.0, **kw):
        if sem is None:
            sem = self.new_sem(f"d{len(self.sems)}")
        r = Rec()
        r.kind, r.e, r.reads, r.writes, r.cost = "dma", e, tuple(reads), tuple(writes), cost
        r.sem, r.pairs, r.kw = sem, list(pairs), kw
        r.tag = self.tag
        self.prog.append(r)

    def _wait(self, e, toks):
        best = {}
        for (s, v) in toks:
            if v > best.get(s, 0):
                best[s] = v
        for s, v in best.items():
            if self.seen[e].get(s, 0) < v:
                self.engs[e].wait_ge(self.sems[s], v)
                self.seen[e][s] = v
                self.nwaits += 1

    def run(self):
        prog = self.prog
        N = len(prog)
        lastw, readers = {}, {}
        succ = [[] for _ in range(N)]
        indeg = [0] * N
        for i, r in enumerate(prog):
            raw, other = set(), set()
            for b in r.reads:
                if b in lastw:
                    raw.add(lastw[b])
            for b in r.writes:
                if b in lastw:
                    other.add(lastw[b])
                other.update(readers.get(b, ()))
            deps = (raw | other)
            deps.discard(i)
            r.deps, r.raw = deps, raw
            for d in deps:
                succ[d].append(i)
            indeg[i] = len(deps)
            for b in r.writes:
                lastw[b] = i
                readers[b] = []
            for b in r.reads:
                if b not in r.writes:
                    readers.setdefault(b, []).append(i)
        finish = [0.0] * N
        ready_t = [0.0] * N
        avail = {e: [] for e in self.ENG}
        free = {e: 0.0 for e in self.ENG}
        for i in range(N):
            if indeg[i] == 0:
                avail[prog[i].e].append(i)
        order = []
        while len(order) < N:
            best = None
            for e in self.ENG:
                fe = free[e]
                for i in avail[e]:
                    key = (max(ready_t[i], fe), i)
                    if best is None or key < best[0]:
                        best = (key, e, i)
            (st, _), e, i = best
            avail[e].remove(i)
            r = prog[i]
            r.start = st
            if r.kind == "dma":
                free[e] = st + 0.15 * len(r.pairs)
            else:
                free[e] = st + r.cost
            finish[i] = st + r.cost
            order.append(i)
            for s_ in succ[i]:
                lat = LAT_SAME if prog[s_].e == e and r.kind == "op" else (LAT_PE if prog[s_].e == "pe" else LAT_X)
                if finish[i] + lat > ready_t[s_]:
                    ready_t[s_] = finish[i] + lat
                indeg[s_] -= 1
                if indeg[s_] == 0:
                    avail[prog[s_].e].append(s_)
        self.est_us = max(finish) if N else 0.0
        self.finish_t = finish
        self.order = order
        if SEQ_ORDER:
            order = list(range(N))
        tok = [None] * N
        dma_last = {}
        for i in order:
            r = prog[i]
            waits = []
            for d in r.deps:
                pd = prog[d]
                if pd.kind == "op" and pd.e == r.e == "pe" and SKIP_PE_SELF:
                    continue
                waits.append(tok[d])
            self._wait(r.e, waits)
            if r.kind == "op":
                inst = r.fn()
                self.cnt[r.e] += 1
                inst.then_inc(self.sems[r.e], 1)
                tok[i] = (r.e, self.cnt[r.e])
            else:
                for (o, a) in r.pairs:
                    inst = self.engs[r.e].dma_start(out=o, in_=a, **r.kw)
                    self.cnt[r.sem] += 16
                    inst.then_inc(self.sems[r.sem], 16)
                tok[i] = (r.sem, self.cnt[r.sem])
                dma_last[r.sem] = tok[i]
        for e in ("sp", "pool"):
            self._wait(e, list(dma_last.values()))


def make_consts():
    C = 128
    pos = np.arange(SEQ, dtype=np.float64)
    invf = 10000.0 ** (-np.arange(0, 128, 2, dtype=np.float64) / 128.0)
    ang = pos[:, None] * invf[None, :]
    cos = np.cos(ang).reshape(16, C, 1, 64)
    sin = np.sin(ang).reshape(16, C, 1, 64)
    p = np.arange(C, dtype=np.float64)
    g = np.array(G, dtype=np.float64)
    sq = g[None, :] ** (p[:, None] + 1.0)
    sk = g[None, :] ** (C - 1.0 - p[:, None]) * (128.0 ** -0.5)
    tab = np.concatenate(
        [cos * sq[None, :, :, None], sin * sq[None, :, :, None], cos * sk[None, :, :, None], sin * sk[None, :, :, None]],
        axis=2,
    )
    tab = tab.reshape(16, C, 1024).astype(np.float32)
    j = np.arange(C)[:, None]
    i = np.arange(C)[None, :]
    caus = (i >= j).astype(np.float64)
    mask_r = np.stack([caus * g[h] ** (-float(C)) for h in range(4)], axis=1).reshape(C, 512)
    Lp = (j <= i).astype(np.float64) * (-1.0 / 16.0)
    Up = (j > i).astype(np.float64) * (-1.0 / 16.0)
    ident = np.eye(C)
    cst = np.concatenate([mask_r, caus, Lp, Up, ident], axis=1).astype(np.float32)
    return tab, cst


def build_nc(NB=4, NT=16):
    NTOK = NB * NT * 128
    nc = bass.Bass("TRN2", target_bir_lowering=False)
    dr = lambda name, shape, kind="ExternalInput": nc.dram_tensor(name, shape, F32, kind=kind).ap()
    x_d = dr("x", [NTOK, D])
    c_d = dr("c", [NB, D])
    ng_d = dr("norm_gain", [1, D])
    wada_d = dr("w_ada", [D, 3 * D])
    bada_d = dr("b_ada", [1, 3 * D])
    win_d = dr("w_in", [D, DIN])
    wg_d = dr("w_gate_up", [16, 256])
    bg_d = dr("b_gate_up", [1, 256])
    gng_d = dr("gla_norm_gain", [1, 128])
    wout_d = dr("w_out", [D, D])
    fg_d = dr("final_gain", [1, D])
    tab_d = dr("tab", [16, 128, 1024])
    cst_d = dr("cst", [128, 1024])
    out_d = dr("out", [NTOK, D], kind="ExternalOutput")

    with ExitStack() as st:
        kb = KB(nc, st)
        _n = [0]

        def sb(shape, dt, name):
            t = st.enter_context(nc.sbuf_tensor("S_" + name, shape, dt))
            return t, Buf(name)

        def sbn(n, shape, dt, name):
            return [sb(shape, dt, f"{name}{i}") for i in range(n)]

        def ps(shape, dt, name):
            t = st.enter_context(nc.psum_tensor(name, shape, dt))
            return t, Buf(name)

        V, S_, P_, T_ = nc.vector, nc.scalar, nc.gpsimd, nc.tensor

        def dve(fn, n, reads, writes, mode=1.0):
            kb.op("dve", fn, reads, writes, cost=(n / mode + 150.0) / 960.0)

        def act(fn, n, reads, writes):
            kb.op("act", fn, reads, writes, cost=(n + 260.0) / 1200.0)

        def pe(fn, cols, reads, writes):
            kb.op("pe", fn, reads, writes, cost=cols / 2000.0 + 0.06)

        def pool(fn, cost, reads, writes):
            kb.op("pool", fn, reads, writes, cost=cost)

        w_in_bf, b_win = sb([128, 8, DIN], BF16, "w_in_bf")
        w_out_bf, b_wout = sb([128, 8, D], BF16, "w_out_bf")
        cst, b_cst = sb([128, 1024], F32, "cst")
        ident_bf, b_idbf = sb([128, 128], BF16, "ident_bf")
        mask_r = cst[:, 0:512]
        mask_g = cst[:, 512:640]
        Lp = cst[:, 640:768]
        Up = cst[:, 768:896]
        identf = cst[:, 896:1024]
        xs = sbn(3, [128, D], F32, "xs")
        tabs = sbn(2, [128, 1024], F32, "tabs")
        outs = sbn(2, [128, D], F32, "outs")
        xn_ = sbn(2, [128, D], BF16, "xn")
        hT = sbn(2, [128, 8, 128], BF16, "hT")
        b_hTb = [Buf("hTb0"), Buf("hTb1")]
        rA, b_rA = sb([128, 512], F32, "rA")
        rB, b_rB = sb([128, 512], F32, "rB")
        q_r_ = sbn(2, [128, 512], BF16, "q_r")
        k_r_ = sbn(2, [128, 512], BF16, "k_r")
        v_bf_ = sbn(2, [128, 512], BF16, "v_bf")
        gv_bf_ = sbn(2, [128, 512], BF16, "gv_bf")
        szr_ = sbn(2, [128, 512], F32, "szr")
        szg_ = sbn(2, [128, 512], F32, "szg")
        qkT_ = sbn(2, [128, 8, 128], BF16, "qkT")
        geT_ = sbn(2, [128, 6, 128], BF16, "geT")
        q_e_ = sbn(2, [128, 256], BF16, "q_e")
        k_e_ = sbn(2, [128, 256], BF16, "k_e")
        k_l_ = sbn(2, [128, 256], BF16, "k_l")
        e_sb, b_e = sb([128, 256], F32, "e_sb")
        l_sb_ = sbn(2, [128, 256], F32, "l_sb")
        E1_ = sbn(2, [128, 256], F32, "E1")
        E2_ = sbn(2, [128, 256], F32, "E2")
        E3_ = sbn(2, [128, 256], F32, "E3")
        a_fm_ = sbn(2, [128, 2], F32, "a_fm")
        glrT_ = sbn(2, [32, 128], BF16, "glrT")
        wg_aug, b_wg = sb([32, 256], BF16, "wg_aug")
        ST_r_ = sbn(2, [128, 512], BF16, "ST_r")
        ST_g_ = sbn(2, [128, 512], BF16, "ST_g")
        R, b_R = sb([128, 4, 128], F32, "R")
        R_bf, b_Rbf = sb([128, 4, 128], BF16, "R_bf")
        Sg, b_Sg = sb([128, 2, 128], F32, "Sg")
        Sg_bf, b_Sgbf = sb([128, 2, 128], BF16, "Sg_bf")
        mix_ = sbn(2, [128, D], BF16, "mix")
        mixT_ = sbn(2, [128, 8, 128], BF16, "mixT")
        res_ = sbn(2, [128, D], F32, "res")
        gate_bc, b_gbc = sb([128, D], F32, "gate_bc")
        fg_bc, b_fgbc = sb([128, D], F32, "fg_bc")
        grep_, b_grep = sb([128, 8, 128], F32, "grep")
        stats_ = [st.enter_context(nc.sbuf_tensor(f"S_stats{i}", [128, 16], F32)) for i in range(2)]
        msq_ = [st.enter_context(nc.sbuf_tensor(f"S_msq{i}", [128, 16], F32)) for i in range(2)]
        rstd_ = [st.enter_context(nc.sbuf_tensor(f"S_rstd{i}", [128, 16], F32)) for i in range(2)]
        b_stats = [{k: Buf(f"stats{i}{k}") for k in "xyf"} for i in range(2)]
        b_msq = [{k: Buf(f"msq{i}{k}") for k in "xyf"} for i in range(2)]
        b_rstd = [{k: Buf(f"rstd{i}{k}") for k in "xyf"} for i in range(2)]
        eps_t, b_eps = sb([128, 1], F32, "eps_t")
        m16, b_m16 = sb([128, 2], F32, "m16")
        cT, b_cT = sb([128, 8, NB], F32, "cT")
        ng_fm, b_ngfm = sb([128, 8], F32, "ng_fm")
        bada_fm, b_badafm = sb([128, 24], F32, "bada_fm")
        gng_fm, b_gngfm = sb([128, 1], F32, "gng_fm")
        ada_fm, b_ada = sb([128, 24, NB], F32, "ada_fm")
        A_fm, b_Afm = sb([128, 8, NB], F32, "A_fm")

        headb = [ps([128, 512], F32, f"phead{i}") for i in range(2)]
        tailb = [ps([128, 512], F32, f"ptail{i}") for i in range(3)]
        pT_ = [ps([128, 8, 128], BF16, f"pT{i}") for i in range(2)]
        pM, b_pM = ps([128, 512], F32, "pM")
        _rr = {"h": 0, "t": 0}

        def bank(kind="t"):
            pool_ = headb if kind == "h" else tailb
            t = pool_[_rr[kind] % len(pool_)]
            _rr[kind] += 1
            return t

        for s in ["ld_x0", "ld_x1", "ld_x2", "ld_t0", "ld_t1", "ld_s0", "ld_s1", "st0", "st1"]:
            kb.new_sem(s)

        kb.dma("sp", None, [(cst[:], cst_d[:, :])], writes=[b_cst])
        kb.dma("sp", None, [(cT[:, :, b], c_d[b:b + 1, :].rearrange("o (k p) -> p (o k)", p=128)) for b in range(NB)],
               writes=[b_cT], allow_slow_non_contiguous=True)
        kb.dma("sp", None, [(ng_fm[:], ng_d.rearrange("o (k p) -> p (o k)", p=128))], writes=[b_ngfm],
               allow_slow_non_contiguous=True)
        kb.dma("sp", None, [(bada_fm[:, j * 8:(j + 1) * 8], bada_d[:, j * 1024:(j + 1) * 1024].rearrange("o (k p) -> p (o k)", p=128))
                            for j in range(3)], writes=[b_badafm], allow_slow_non_contiguous=True)
        kb.dma("sp", None, [(gng_fm[:], gng_d.rearrange("o (p one) -> (o p) one", one=1))], writes=[b_gngfm],
               allow_slow_non_contiguous=True)
        kb.dma("sp", None, [(fg_bc[:], fg_d[0:1, :].to_broadcast([128, D]))], writes=[b_fgbc])
        kb.dma("pool", None, [(w_in_bf[:, kc, :], win_d[kc * 128:(kc + 1) * 128, :]) for kc in range(8)],
               writes=[b_win], cost=90.0, max_dma_last_dim=4096)
        kb.dma("pool", None, [(w_out_bf[:, kc, :], wout_d[kc * 128:(kc + 1) * 128, :]) for kc in range(4)],
               writes=[b_wout], cost=20.0, max_dma_last_dim=4096)
        kb.dma("pool", None, [(wg_aug[0:16, :], wg_d[:, :]), (wg_aug[16:17, :], bg_d[:, :])], writes=[b_wg])

        dve(lambda: V.memset(eps_t[:], EPS), 1, [], [b_eps])
        dve(lambda: V.memset(m16[:], -1.0 / 16.0), 2, [], [b_m16])
        for i in range(2):
            dve(lambda i=i: V.memset(glrT_[i][0][:], 1.0), 128, [], [glrT_[i][1]])
            dve(lambda i=i: V.memset(geT_[i][0][:], 0.0), 768, [], [geT_[i][1]])
        dve(lambda: V.tensor_copy(out=ident_bf[:], in_=identf), 128, [b_cst], [b_idbf])

        for kc in range(4, 8):
            stg, b_stg = outs[kc % 2]
            kb.dma("sp", f"ld_s{kc % 2}", [(stg[:], wout_d[kc * 128:(kc + 1) * 128, :])], writes=[b_stg])
            dve(lambda kc=kc, stg=stg: V.tensor_scalar(out=w_out_bf[:, kc, :], in0=stg[:], scalar1=gng_fm[:, 0:1], scalar2=None, op0=ALU.mult),
                1024, [b_stg, b_gngfm], [b_wout])

        act(lambda: S_.activation(out=cT[:], in_=cT[:], func=AF.Silu), 8 * NB, [b_cT], [b_cT])
        for j in range(24):
            stg, b_stg = outs[j % 2]
            stg3 = stg[:].rearrange("p (k c) -> p k c", k=8)
            kb.dma("sp", f"ld_s{j % 2}", [(stg3, wada_d[:, j * 128:(j + 1) * 128].rearrange("(k p) c -> p k c", p=128))], writes=[b_stg])

            def mm_ada(stg3=stg3):
                for kc in range(8):
                    i = T_.matmul(pM[:, 0:NB], lhsT=stg3[:, kc, :], rhs=cT[:, kc, :], start=(kc == 0), stop=(kc == 7))
                return i
            pe(mm_ada, 8 * 4 * 128, [b_stg, b_cT], [b_pM])
            dve(lambda j=j: V.tensor_scalar(out=ada_fm[:, j, :], in0=pM[:, 0:NB], scalar1=bada_fm[:, j:j + 1], scalar2=None, op0=ALU.add),
                NB, [b_pM, b_badafm], [b_ada])
        dve(lambda: V.tensor_scalar(out=A_fm[:], in0=ada_fm[:, 8:16, :], scalar1=1.0, scalar2=None, op0=ALU.add), 8 * NB, [b_ada], [b_Afm])
        dve(lambda: V.tensor_tensor(out=A_fm[:], in0=A_fm[:], in1=ng_fm[:].unsqueeze(2).to_broadcast([128, 8, NB]), op=ALU.mult),
            8 * NB, [b_Afm, b_ngfm], [b_Afm])

        cd = [G[h] ** 128.0 for h in range(4)]
        LN_EIGHTH = math.log(0.125)

        def rsqrt_cols(par, grp, c0, c1, inv_n):
            stt, msq, rstd = stats_[par], msq_[par], rstd_[par]

            act(lambda: S_.activation(out=msq[:, c0:c1], in_=stt[:, c0:c1], func=AF.Ln, scale=inv_n, bias=eps_t[:, 0:1]),
                c1 - c0, [b_stats[par][grp], b_eps], [b_msq[par][grp]])
            act(lambda: S_.activation(out=rstd[:, c0:c1], in_=msq[:, c0:c1], func=AF.Exp, scale=-0.5),
                c1 - c0, [b_msq[par][grp]], [b_rstd[par][grp]])

        def load_tile(g):
            t = g % NT
            xt, b_x = xs[g % 3]
            tt, b_t = tabs[g % 2]
            kb.dma("sp", f"ld_x{g % 3}", [(xt[:], x_d[g * 128:(g + 1) * 128, :])], writes=[b_x])
            kb.dma("sp", f"ld_t{g % 2}", [(tt[:], tab_d[t, :, :])], writes=[b_t])

        def seq_start(b):
            dve(lambda: V.memset(R[:], 0.0), 512, [], [b_R])
            dve(lambda: V.memset(R_bf[:], 0.0), 512, [], [b_Rbf], mode=2.0)
            dve(lambda: V.memset(Sg[:], 0.0), 256, [], [b_Sg])
            dve(lambda: V.memset(Sg_bf[:], 0.0), 256, [], [b_Sgbf], mode=2.0)
            dve(lambda: V.tensor_copy(out=grep_[:], in_=ada_fm[:, 16:24, b:b + 1].to_broadcast([128, 8, 128])), 1024, [b_ada], [b_grep])
            for n in range(2):
                pb, b_pb = tailb[1 + n]

                def mm_g(n=n, pb=pb):
                    for q in range(4):
                        kc = n * 4 + q
                        i = T_.matmul(pb[:, q * 128:(q + 1) * 128], lhsT=grep_[:, kc, :], rhs=identf, start=True, stop=True)
                    return i
                pe(mm_g, 4 * 4 * 128, [b_grep, b_cst], [b_pb])
                act(lambda n=n, pb=pb: S_.copy(out=gate_bc[:, n * 512:(n + 1) * 512], in_=pb[:]), 512, [b_pb], [b_gbc])

        def rotary(pb, b_pb, tt, b_t, toff, dst, b_dst):
            src4 = pb[:].rearrange("p (h two f) -> p h two f", h=4, two=2)
            cs4 = tt[:, toff:toff + 256].rearrange("p (h f) -> p h f", h=4).unsqueeze(2).to_broadcast([128, 4, 2, 64])
            sn4 = tt[:, toff + 256:toff + 512].rearrange("p (h f) -> p h f", h=4).unsqueeze(2).to_broadcast([128, 4, 2, 64])
            A4 = rA[:].rearrange("p (h two f) -> p h two f", h=4, two=2)
            B4 = rB[:].rearrange("p (h two f) -> p h two f", h=4, two=2)
            d4 = dst[:].rearrange("p (h two f) -> p h two f", h=4, two=2)
            dve(lambda: V.tensor_tensor(out=A4, in0=src4, in1=cs4, op=ALU.mult), 512, [b_pb, b_t], [b_rA])
            dve(lambda: V.tensor_tensor(out=B4, in0=src4, in1=sn4, op=ALU.mult), 512, [b_pb, b_t], [b_rB])
            dve(lambda: V.tensor_tensor(out=d4[:, :, 0, :], in0=A4[:, :, 0, :], in1=B4[:, :, 1, :], op=ALU.subtract), 256, [b_rA, b_rB], [b_dst])
            dve(lambda: V.tensor_tensor(out=d4[:, :, 1, :], in0=B4[:, :, 0, :], in1=A4[:, :, 1, :], op=ALU.add), 256, [b_rA, b_rB], [b_dst])

        def inproj_group(hTt, b_hT, c0, ncols):
            pb, b_pb = bank("h")

            def mm():
                for kc in range(8):
                    i = T_.matmul(pb[:, 0:ncols], lhsT=hTt[:, kc, :], rhs=w_in_bf[:, kc, c0:c0 + ncols], start=(kc == 0), stop=(kc == 7))
                return i
            pe(mm, 8 * ncols, list(b_hT) + [b_win], [b_pb])
            return pb, b_pb

        def tile_body(g, phase):
            b = g // NT
            t = g % NT
            par = g % 2
            xt, b_x = xs[g % 3]
            tt, b_t = tabs[par]
            hTt, b_hT = hT[par]
            ot, b_o = outs[par]
            xn, b_xn = xn_[par]
            q_r, b_qr = q_r_[par]
            k_r, b_kr = k_r_[par]
            v_bf, b_v = v_bf_[par]
            gv_bf, b_gv = gv_bf_[par]
            szr, b_szr = szr_[par]
            szg, b_szg = szg_[par]
            qkT, b_qkT = qkT_[par]
            geT, b_geT = geT_[par]
            q_e, b_qe = q_e_[par]
            k_e, b_ke = k_e_[par]
            k_l, b_kl = k_l_[par]
            l_sb, b_l = l_sb_[par]
            E1, b_E1 = E1_[par]
            E2, b_E2 = E2_[par]
            E3, b_E3 = E3_[par]
            a_fm, b_afm = a_fm_[par]
            glrT, b_glrT = glrT_[par]
            ST_r, b_STr = ST_r_[par]
            ST_g, b_STg = ST_g_[par]
            mix, b_mix = mix_[par]
            mixT, b_mixT = mixT_[par]
            res, b_res = res_[par]
            stats, rstd = stats_[par], rstd_[par]
            bs, br = b_stats[par], b_rstd[par]
            pT, b_pT = pT_[par]
            if phase == 1:
                if t == 0:
                    seq_start(b)
                return tile_main(locals())
            act(lambda: S_.activation(out=xn[:], in_=xt[:], func=AF.Square, accum_out=stats[:, 0:1]), 1024, [b_x], [bs["x"], b_xn])
            rsqrt_cols(par, "x", 0, 1, 1.0 / D)
            act(lambda: S_.activation(out=xn[:], in_=xt[:], func=AF.Copy, scale=rstd[:, 0:1]), 1024, [b_x, br["x"]], [b_xn])
            def tr_x():
                for kc in range(8):
                    i = T_.transpose(out=pT[:, kc, :], in_=xn[:, kc * 128:(kc + 1) * 128], identity=ident_bf[:])
                return i
            pe(tr_x, 1024, [b_xn, b_idbf], [b_pT])

            b_hT2 = b_hTb[par]

            def ev_h():
                for kc in range(4):
                    i = S_.activation(out=hTt[:, kc, :], in_=pT[:, kc, :], func=AF.Identity,
                                      scale=A_fm[:, kc, b:b + 1], bias=ada_fm[:, kc, b:b + 1])
                return i
            kb.op("act", ev_h, [b_pT, b_Afm, b_ada], [b_hT], cost=4 * 0.38)

            def ev_h2():
                for kc in range(4, 8):
                    i = V.tensor_scalar(out=hTt[:, kc, :], in0=pT[:, kc, :], scalar1=A_fm[:, kc, b:b + 1], scalar2=ada_fm[:, kc, b:b + 1],
                                        op0=ALU.mult, op1=ALU.add)
                return i
            def ev_h2a():
                for kc in range(4, 8):
                    i = S_.activation(out=hTt[:, kc, :], in_=pT[:, kc, :], func=AF.Identity,
                                      scale=A_fm[:, kc, b:b + 1], bias=ada_fm[:, kc, b:b + 1])
                return i
            if EVH2:
                kb.op("dve", ev_h2, [b_pT, b_Afm, b_ada], [b_hT2], cost=4 * 0.25)
            else:
                kb.op("act", ev_h2a, [b_pT, b_Afm, b_ada], [b_hT2], cost=4 * 0.38)
            def mm_glr():
                for kc in range(8):
                    i = T_.matmul(pM[0:16, 256:384], lhsT=w_in_bf[:, kc, 3584:3600], rhs=hTt[:, kc, :], start=(kc == 0), stop=(kc == 7))
                return i
            pe(mm_glr, 1024, [b_hT, b_hT2, b_win], [b_pM])
            act(lambda: S_.copy(out=glrT[0:16, :], in_=pM[0:16, 256:384]), 128, [b_pM], [b_glrT])
            pe(lambda: T_.matmul(pM[:, 0:256], lhsT=glrT[0:17, :], rhs=wg_aug[0:17, :], start=True, stop=True), 256, [b_glrT, b_wg], [b_pM])
            act(lambda: S_.activation(out=e_sb[:], in_=pM[:, 0:256], func=AF.Exp, scale=-1.0), 256, [b_pM], [b_e])
            act(lambda: S_.activation(out=l_sb[:], in_=e_sb[:], func=AF.Ln, bias=1.0), 256, [b_e], [b_l])

            def mm_bl():
                T_.matmul(pM[:, 384:385], lhsT=l_sb[:, 0:128], rhs=m16[:, 0:1], start=True, stop=True)
                i = T_.matmul(pM[:, 385:386], lhsT=l_sb[:, 128:256], rhs=m16[:, 0:1], start=True, stop=True)
                return i
            if BSPLIT:
                pe(mm_bl, 512, [b_l, b_m16], [b_pM])
                pe(lambda: T_.matmul(pM[:, 0:256], lhsT=Lp, rhs=l_sb[:], start=True, stop=True), 1024, [b_l, b_cst], [b_pM])
            else:
                def mm_b():
                    mm_bl()
                    return T_.matmul(pM[:, 0:256], lhsT=Lp, rhs=l_sb[:], start=True, stop=True)
                pe(mm_b, 512 + 1024, [b_l, b_m16, b_cst], [b_pM])
            act(lambda: S_.activation(out=E1[:], in_=pM[:, 0:256], func=AF.Exp, bias=LN_EIGHTH), 256, [b_pM], [b_E1])
            act(lambda: S_.activation(out=E2[:], in_=pM[:, 0:256], func=AF.Exp, scale=-1.0), 256, [b_pM], [b_E2])
            act(lambda: S_.activation(out=a_fm[:], in_=pM[:, 384:386], func=AF.Exp), 2, [b_pM], [b_afm])
            return None

        def tile_main(L):
            g, b, t, par = L["g"], L["b"], L["t"], L["par"]
            xt, b_x, tt, b_t, hTt, b_hT, ot, b_o = L["xt"], L["b_x"], L["tt"], L["b_t"], L["hTt"], L["b_hT"], L["ot"], L["b_o"]
            b_hT2 = b_hTb[par]
            q_r, b_qr, k_r, b_kr, v_bf, b_v, gv_bf, b_gv = L["q_r"], L["b_qr"], L["k_r"], L["b_kr"], L["v_bf"], L["b_v"], L["gv_bf"], L["b_gv"]
            szr, b_szr, szg, b_szg, qkT, b_qkT, geT, b_geT = L["szr"], L["b_szr"], L["szg"], L["b_szg"], L["qkT"], L["b_qkT"], L["geT"], L["b_geT"]
            q_e, b_qe, k_e, b_ke = L["q_e"], L["b_qe"], L["k_e"], L["b_ke"]
            E1, b_E1, E2, b_E2, a_fm, b_afm = L["E1"], L["b_E1"], L["E2"], L["b_E2"], L["a_fm"], L["b_afm"]
            ST_r, b_STr, ST_g, b_STg, mix, b_mix, mixT, b_mixT, res, b_res = (L["ST_r"], L["b_STr"], L["ST_g"], L["b_STg"], L["mix"], L["b_mix"],
                                                                                L["mixT"], L["b_mixT"], L["res"], L["b_res"])
            stats, rstd, bs, br, pT, b_pT = L["stats"], L["rstd"], L["bs"], L["br"], L["pT"], L["b_pT"]
            pq, b_pq = inproj_group(hTt, (b_hT, b_hT2), 0, 512)
            rotary(pq, b_pq, tt, b_t, 0, q_r, b_qr)
            pk, b_pk = inproj_group(hTt, (b_hT, b_hT2), 512, 512)
            rotary(pk, b_pk, tt, b_t, 512, k_r, b_kr)
            pg, b_pg = inproj_group(hTt, (b_hT, b_hT2), 2048, 512)
            dve(lambda: V.tensor_tensor(out=q_e[:], in0=pg[:, 0:256], in1=E1[:], op=ALU.mult), 256, [b_pg, b_E1], [b_qe])
            dve(lambda: V.tensor_tensor(out=k_e[:], in0=pg[:, 256:512], in1=E2[:], op=ALU.mult), 256, [b_pg, b_E2], [b_ke])
            pv, b_pv = inproj_group(hTt, (b_hT, b_hT2), 1024, 512)
            act(lambda: S_.copy(out=v_bf[:], in_=pv[:]), 512, [b_pv], [b_v])
            pgv, b_pgv = inproj_group(hTt, (b_hT, b_hT2), 2560, 512)
            act(lambda: S_.copy(out=gv_bf[:], in_=pgv[:]), 512, [b_pgv], [b_gv])
            pz, b_pz = inproj_group(hTt, (b_hT, b_hT2), 1536, 512)
            pgz, b_pgz = inproj_group(hTt, (b_hT, b_hT2), 3072, 512)

            def silus():
                S_.activation(out=szr[:], in_=pz[:], func=AF.Silu)
                return S_.activation(out=szg[:], in_=pgz[:], func=AF.Silu)
            kb.op("act", silus, [b_pz, b_pgz], [b_szr, b_szg], cost=2 * 0.64)
            def tr_qk():
                for h in range(4):
                    T_.transpose(out=pT[:, h, :], in_=q_r[:, h * 128:(h + 1) * 128], identity=ident_bf[:])
                for h in range(4):
                    i = T_.transpose(out=pT[:, 4 + h, :], in_=k_r[:, h * 128:(h + 1) * 128], identity=ident_bf[:])
                return i
            pe(tr_qk, 1024, [b_qr, b_kr, b_idbf], [b_pT])
            dve(lambda: V.tensor_copy(out=qkT[:], in_=pT[:]), 1024, [b_pT], [b_qkT], mode=2.0)
            def tr_ge():
                for u in range(2):
                    T_.transpose(out=pT[:, u, :], in_=q_e[:, u * 128:(u + 1) * 128], identity=ident_bf[:])
                for u in range(2):
                    i = T_.transpose(out=pT[:, 2 + u, :], in_=k_e[:, u * 128:(u + 1) * 128], identity=ident_bf[:])
                return i
            pe(tr_ge, 512, [b_qe, b_ke, b_idbf], [b_pT])

            def ev_ge():
                for h in range(4):
                    r0 = (h % 2) * 64
                    V.tensor_copy(out=geT[r0:r0 + 64, h, :], in_=pT[r0:r0 + 64, h // 2, :])
                return V.tensor_copy(out=geT[:, 4:6, :], in_=pT[:, 2:4, :])
            kb.op("dve", ev_ge, [b_pT], [b_geT], cost=5 * 0.25)
            (T0, bT0), (T1, bT1), (T2, bT2) = tailb
            psr, b_psr = T0, bT0
            psg, b_psg = T1, bT1
            pyr, b_pyr = T2, bT2
            pkr, b_pkr = T0, bT0
            pyg, b_pyg = T1, bT1
            pkg, b_pkg = T0, bT0

            def mm_sr():
                for h in range(4):
                    i = T_.matmul(psr[:, h * 128:(h + 1) * 128], lhsT=qkT[:, 4 + h, :], rhs=qkT[:, h, :], start=True, stop=True)
                return i
            pe(mm_sr, 512, [b_qkT], [b_psr])
            dve(lambda: V.tensor_tensor(out=ST_r[:], in0=psr[:], in1=mask_r, op=ALU.mult), 512, [b_psr, b_cst], [b_STr])

            def mm_sg():
                for h in range(4):
                    i = T_.matmul(psg[:, h * 128:(h + 1) * 128], lhsT=geT[:, 4 + h // 2, :], rhs=geT[:, h, :], start=True, stop=True)
                return i
            pe(mm_sg, 512, [b_geT], [b_psg])
            dve(lambda: V.tensor_tensor(out=ST_g[:].rearrange("p (h i) -> p h i", h=4), in0=psg[:].rearrange("p (h i) -> p h i", h=4),
                                        in1=mask_g.unsqueeze(1).to_broadcast([128, 4, 128]), op=ALU.mult), 512, [b_psg, b_cst], [b_STg])

            def mm_yr():
                for h in range(4):
                    T_.matmul(pyr[:, h * 128:(h + 1) * 128], lhsT=ST_r[:, h * 128:(h + 1) * 128], rhs=v_bf[:, h * 128:(h + 1) * 128],
                              start=True, stop=False)
                    i = T_.matmul(pyr[:, h * 128:(h + 1) * 128], lhsT=qkT[:, h, :], rhs=R_bf[:, h, :], start=False, stop=True)
                return i
            pe(mm_yr, 1024, [b_STr, b_v, b_qkT, b_Rbf], [b_pyr])

            def mm_kr():
                for h in range(4):
                    i = T_.matmul(pkr[:, h * 128:(h + 1) * 128], lhsT=k_r[:, h * 128:(h + 1) * 128], rhs=v_bf[:, h * 128:(h + 1) * 128],
                                  start=True, stop=True)
                return i
            pe(mm_kr, 512, [b_kr, b_v], [b_pkr])

            def up_R():
                for h in range(4):
                    i = V.scalar_tensor_tensor(out=R[:, h, :], in0=R[:, h, :], scalar=cd[h], in1=pkr[:, h * 128:(h + 1) * 128],
                                               op0=ALU.mult, op1=ALU.add)
                return i
            kb.op("dve", up_R, [b_pkr], [b_R], cost=4 * 0.3)
            act(lambda: S_.copy(out=R_bf[:], in_=R[:]), 512, [b_R], [b_Rbf])

            def mm_yg():
                for h in range(4):
                    T_.matmul(pyg[:, h * 128:(h + 1) * 128], lhsT=ST_g[:, h * 128:(h + 1) * 128], rhs=gv_bf[:, h * 128:(h + 1) * 128],
                              start=True, stop=False)
                    i = T_.matmul(pyg[:, h * 128:(h + 1) * 128], lhsT=geT[:, h, :], rhs=Sg_bf[:, h // 2, :], start=False, stop=True)
                return i
            pe(mm_yg, 1024, [b_STg, b_gv, b_geT, b_Sgbf], [b_pyg])

            def mm_kg():
                for h in range(4):
                    r0 = (h % 2) * 64
                    u = h // 2
                    i = T_.matmul(pkg[r0:r0 + 64, u * 128:(u + 1) * 128], lhsT=k_e[:, h * 64:(h + 1) * 64], rhs=gv_bf[:, h * 128:(h + 1) * 128],
                                  start=True, stop=True)
                return i
            pe(mm_kg, 512, [b_ke, b_gv], [b_pkg])
            Sg2 = Sg[:].rearrange("p u e -> p (u e)")
            dve(lambda: V.tensor_tensor(out=Sg2, in0=Sg2, in1=pkg[:, 0:256], op=ALU.add), 256, [b_pkg], [b_Sg])
            dve(lambda: V.tensor_tensor(out=Sg[:], in0=Sg[:], in1=a_fm[:].unsqueeze(2).to_broadcast([128, 2, 128]), op=ALU.mult), 256, [b_afm], [b_Sg])
            act(lambda: S_.copy(out=Sg_bf[:], in_=Sg[:]), 256, [b_Sg], [b_Sgbf])
            def sq_y():
                for h in range(4):
                    S_.activation(out=mix[:, h * 128:(h + 1) * 128], in_=pyr[:, h * 128:(h + 1) * 128], func=AF.Square, accum_out=stats[:, 1 + h:2 + h])
                for h in range(4):
                    i = S_.activation(out=mix[:, 512 + h * 128:512 + (h + 1) * 128], in_=pyg[:, h * 128:(h + 1) * 128], func=AF.Square, accum_out=stats[:, 5 + h:6 + h])
                return i
            kb.op("act", sq_y, [b_pyr, b_pyg], [bs["y"], b_mix], cost=8 * 0.42)
            rsqrt_cols(par, "y", 1, 9, 1.0 / 128.0)

            def mk_mix():
                for h in range(4):
                    V.scalar_tensor_tensor(out=mix[:, h * 128:(h + 1) * 128], in0=pyr[:, h * 128:(h + 1) * 128], scalar=rstd[:, 1 + h:2 + h],
                                           in1=szr[:, h * 128:(h + 1) * 128], op0=ALU.mult, op1=ALU.mult)
                for h in range(4):
                    i = V.scalar_tensor_tensor(out=mix[:, 512 + h * 128:512 + (h + 1) * 128], in0=pyg[:, h * 128:(h + 1) * 128],
                                               scalar=rstd[:, 5 + h:6 + h], in1=szg[:, h * 128:(h + 1) * 128], op0=ALU.mult, op1=ALU.mult)
                return i
            kb.op("dve", mk_mix, [b_pyr, b_pyg, br["y"], b_szr, b_szg], [b_mix], cost=8 * 0.3)
            def tr_m():
                for kc in range(8):
                    i = T_.transpose(out=pT[:, kc, :], in_=mix[:, kc * 128:(kc + 1) * 128], identity=ident_bf[:])
                return i
            pe(tr_m, 1024, [b_mix, b_idbf], [b_pT])
            dve(lambda: V.tensor_copy(out=mixT[:], in_=pT[:]), 1024, [b_pT], [b_mixT], mode=2.0)
            for n in range(2):
                po, b_po = tailb[0] if n == 0 else tailb[2]

                def mm_o(n=n, po=po):
                    for kc in range(8):
                        i = T_.matmul(po[:], lhsT=mixT[:, kc, :], rhs=w_out_bf[:, kc, n * 512:(n + 1) * 512], start=(kc == 0), stop=(kc == 7))
                    return i
                pe(mm_o, 8 * 512, [b_mixT, b_wout], [b_po])
                sl = slice(n * 512, (n + 1) * 512)
                dve(lambda po=po, sl=sl: V.tensor_tensor(out=res[:, sl], in0=po[:], in1=gate_bc[:, sl], op=ALU.mult), 512, [b_po, b_gbc], [b_res])
            dve(lambda: V.tensor_tensor(out=res[:], in0=res[:], in1=xt[:], op=ALU.add), 1024, [b_res, b_x], [b_res])
            act(lambda: S_.activation(out=ot[:], in_=res[:], func=AF.Square, accum_out=stats[:, 9:10]), 1024, [b_res], [bs["f"], b_o])
            rsqrt_cols(par, "f", 9, 10, 1.0 / D)
            dve(lambda: V.scalar_tensor_tensor(out=ot[:], in0=res[:], scalar=rstd[:, 9:10], in1=fg_bc[:], op0=ALU.mult, op1=ALU.mult),
                1024, [b_res, br["f"], b_fgbc], [b_o])
            kb.dma("sp", f"st{par}", [(out_d[g * 128:(g + 1) * 128, :], ot[:])], reads=[b_o])

        NG = NB * NT
        kb.tag = 0
        load_tile(0)
        tile_body(0, 0)
        for g in range(NG):
            if g + 1 < NG:
                kb.tag = g + 1
                load_tile(g + 1)
                tile_body(g + 1, 0)
            kb.tag = g
            tile_body(g, 1)
        kb.run()
        build_nc.last_est_us = kb.est_us
        build_nc.last_kb = kb
    return nc


_CACHE = {}


def kernel(x, c, norm_gain, w_ada, b_ada, w_in, w_gate_up, b_gate_up, gla_norm_gain, w_out, final_gain):
    NB, NT = 4, 16
    f = lambda a: np.ascontiguousarray(np.asarray(a, dtype=np.float32))
    x = f(x)
    c = f(c)
    if "nc" not in _CACHE:
        _CACHE["nc"] = build_nc(NB, NT)
        _CACHE["consts"] = make_consts()
    nc = _CACHE["nc"]
    tab, cst = _CACHE["consts"]
    shared = {
        "norm_gain": f(norm_gain).reshape(1, D), "w_ada": f(w_ada).reshape(D, 3 * D), "b_ada": f(b_ada).reshape(1, 3 * D),
        "w_in": f(w_in).reshape(D, DIN), "w_gate_up": f(w_gate_up).reshape(16, 256), "b_gate_up": f(b_gate_up).reshape(1, 256),
        "gla_norm_gain": f(gla_norm_gain).reshape(1, 128), "w_out": f(w_out).reshape(D, D), "final_gain": f(final_gain).reshape(1, D),
        "tab": tab, "cst": cst,
    }
    in_maps = []
    for i in range(NCORES):
        m = dict(shared)
        m["x"] = x[i * NB:(i + 1) * NB].reshape(NB * SEQ, D)
        m["c"] = c[i * NB:(i + 1) * NB]
        in_maps.append(m)
    res = run_bass_kernel_spmd(nc, in_maps, core_ids=list(range(NCORES)))
    out = np.concatenate([r["out"].reshape(NB, SEQ, D) for r in res.results], axis=0)
    return out.astype(np.float32)
```

```python
import math
from contextlib import ExitStack

import numpy as np
import concourse.bass as bass
import concourse.mybir as mybir
from concourse.bass_utils import run_bass_kernel_spmd

F32 = mybir.dt.float32
BF16 = mybir.dt.bfloat16
AF = mybir.ActivationFunctionType
ALU = mybir.AluOpType

D = 1024
SEQ = 2048
DIN = 3600
NCORES = 8
EPS = 1e-6
G = [1.0 - 2.0 ** (-5.0 - h) for h in range(4)]
import os
SKIP_PE_SELF = os.environ.get('SKIP_PE_SELF', '0') == '1'
SEQ_ORDER = os.environ.get('SEQ_ORDER', '0') == '1'
EVH2 = os.environ.get('EVH2', '0') == '1'
BSPLIT = os.environ.get('BSPLIT', '1') == '1'
LAT_PE = float(os.environ.get('LAT_PE', '0.7'))
LAT_SAME = float(os.environ.get('LAT_SAME', '0.1'))
LAT_X = float(os.environ.get('LAT_X', '0.3'))


class Buf:
    __slots__ = ("name",)

    def __init__(self, name):
        self.name = name


class Rec:
    __slots__ = ("kind", "e", "fn", "reads", "writes", "cost", "sem", "pairs", "kw", "deps", "raw", "start", "tag")


class KB:
    ENG = ("pe", "act", "dve", "pool", "sp")

    def __init__(self, nc, stack):
        self.nc = nc
        self.engs = {"pe": nc.tensor, "act": nc.scalar, "dve": nc.vector, "pool": nc.gpsimd, "sp": nc.sync}
        self.sems = {}
        self.cnt = {}
        self.stack = stack
        for e in self.engs:
            self.sems[e] = stack.enter_context(nc.semaphore("s_" + e))
            self.cnt[e] = 0
        self.seen = {e: {} for e in self.engs}
        self.prog = []
        self.tag = -1
        self.nwaits = 0

    def new_sem(self, name):
        self.sems[name] = self.stack.enter_context(self.nc.semaphore("s_" + name))
        self.cnt[name] = 0
        return name

    def op(self, e, fn, reads=(), writes=(), cost=0.3):
        r = Rec()
        r.kind, r.e, r.fn, r.reads, r.writes, r.cost = "op", e, fn, tuple(reads), tuple(writes), cost
        r.tag = self.tag
        self.prog.append(r)

    def dma(self, e, sem, pairs, reads=(), writes=(), cost=6.0, **kw):
        if sem is None:
            sem = self.new_sem(f"d{len(self.sems)}")
        r = Rec()
        r.kind, r.e, r.reads, r.writes, r.cost = "dma", e, tuple(reads), tuple(writes), cost
        r.sem, r.pairs, r.kw = sem, list(pairs), kw
        r.tag = self.tag
        self.prog.append(r)

    def _wait(self, e, toks):
        best = {}
        for (s, v) in toks:
            if v > best.get(s, 0):
                best[s] = v
        for s, v in best.items():
            if self.seen[e].get(s, 0) < v:
                self.engs[e].wait_ge(self.sems[s], v)
                self.seen[e][s] = v
                self.nwaits += 1

    def run(self):
        prog = self.prog
        N = len(prog)
        lastw, readers = {}, {}
        succ = [[] for _ in range(N)]
        indeg = [0] * N
        for i, r in enumerate(prog):
            raw, other = set(), set()
            for b in r.reads:
                if b in lastw:
                    raw.add(lastw[b])
            for b in r.writes:
                if b in lastw:
                    other.add(lastw[b])
                other.update(readers.get(b, ()))
            deps = (raw | other)
            deps.discard(i)
            r.deps, r.raw = deps, raw
            for d in deps:
                succ[d].append(i)
            indeg[i] = len(deps)
            for b in r.writes:
                lastw[b] = i
                readers[b] = []
            for b in r.reads:
                if b not in r.writes:
                    readers.setdefault(b, []).append(i)
        finish = [0.0] * N
        ready_t = [0.0] * N
        avail = {e: [] for e in self.ENG}
        free = {e: 0.0 for e in self.ENG}
        for i in range(N):
            if indeg[i] == 0:
                avail[prog[i].e].append(i)
        order = []
        while len(order) < N:
            best = None
            for e in self.ENG:
                fe = free[e]
                for i in avail[e]:
                    key = (max(ready_t[i], fe), i)
                    if best is None or key < best[0]:
                        best = (key, e, i)
            (st, _), e, i = best
            avail[e].remove(i)
            r = prog[i]
            r.start = st
            if r.kind == "dma":
                free[e] = st + 0.15 * len(r.pairs)
            else:
                free[e] = st + r.cost
            finish[i] = st + r.cost
            order.append(i)
            for s_ in succ[i]:
                lat = LAT_SAME if prog[s_].e == e and r.kind == "op" else (LAT_PE if prog[s_].e == "pe" else LAT_X)
                if finish[i] + lat > ready_t[s_]:
                    ready_t[s_] = finish[i] + lat
                indeg[s_] -= 1
                if indeg[s_] == 0:
                    avail[prog[s_].e].append(s_)
        self.est_us = max(finish) if N else 0.0
        self.finish_t = finish
        self.order = order
        if SEQ_ORDER:
            order = list(range(N))
        tok = [None] * N
        dma_last = {}
        for i in order:
            r = prog[i]
            waits = []
            for d in r.deps:
                pd = prog[d]
                if pd.kind == "op" and pd.e == r.e == "pe" and SKIP_PE_SELF:
                    continue
                waits.append(tok[d])
            self._wait(r.e, waits)
            if r.kind == "op":
                inst = r.fn()
                self.cnt[r.e] += 1
                inst.then_inc(self.sems[r.e], 1)
                tok[i] = (r.e, self.cnt[r.e])
            else:
                for (o, a) in r.pairs:
                    inst = self.engs[r.e].dma_start(out=o, in_=a, **r.kw)
                    self.cnt[r.sem] += 16
                    inst.then_inc(self.sems[r.sem], 16)
                tok[i] = (r.sem, self.cnt[r.sem])
                dma_last[r.sem] = tok[i]
        for e in ("sp", "pool"):
            self._wait(e, list(dma_last.values()))


def make_consts():
    C = 128
    pos = np.arange(SEQ, dtype=np.float64)
    invf = 10000.0 ** (-np.arange(0, 128, 2, dtype=np.float64) / 128.0)
    ang = pos[:, None] * invf[None, :]
    cos = np.cos(ang).reshape(16, C, 1, 64)
    sin = np.sin(ang).reshape(16, C, 1, 64)
    p = np.arange(C, dtype=np.float64)
    g = np.array(G, dtype=np.float64)
    sq = g[None, :] ** (p[:, None] + 1.0)
    sk = g[None, :] ** (C - 1.0 - p[:, None]) * (128.0 ** -0.5)
    tab = np.concatenate(
        [cos * sq[None, :, :, None], sin * sq[None, :, :, None], cos * sk[None, :, :, None], sin * sk[None, :, :, None]],
        axis=2,
    )
    tab = tab.reshape(16, C, 1024).astype(np.float32)
    j = np.arange(C)[:, None]
    i = np.arange(C)[None, :]
    caus = (i >= j).astype(np.float64)
    mask_r = np.stack([caus * g[h] ** (-float(C)) for h in range(4)], axis=1).reshape(C, 512)
    Lp = (j <= i).astype(np.float64) * (-1.0 / 16.0)
    Up = (j > i).astype(np.float64) * (-1.0 / 16.0)
    ident = np.eye(C)
    cst = np.concatenate([mask_r, caus, Lp, Up, ident], axis=1).astype(np.float32)
    return tab, cst


def build_nc(NB=4, NT=16):
    NTOK = NB * NT * 128
    nc = bass.Bass("TRN2", target_bir_lowering=False)
    dr = lambda name, shape, kind="ExternalInput": nc.dram_tensor(name, shape, F32, kind=kind).ap()
    x_d = dr("x", [NTOK, D])
    c_d = dr("c", [NB, D])
    ng_d = dr("norm_gain", [1, D])
    wada_d = dr("w_ada", [D, 3 * D])
    bada_d = dr("b_ada", [1, 3 * D])
    win_d = dr("w_in", [D, DIN])
    wg_d = dr("w_gate_up", [16, 256])
    bg_d = dr("b_gate_up", [1, 256])
    gng_d = dr("gla_norm_gain", [1, 128])
    wout_d = dr("w_out", [D, D])
    fg_d = dr("final_gain", [1, D])
    tab_d = dr("tab", [16, 128, 1024])
    cst_d = dr("cst", [128, 1024])
    out_d = dr("out", [NTOK, D], kind="ExternalOutput")

    with ExitStack() as st:
        kb = KB(nc, st)
        _n = [0]

        def sb(shape, dt, name):
            t = st.enter_context(nc.sbuf_tensor("S_" + name, shape, dt))
            return t, Buf(name)

        def sbn(n, shape, dt, name):
            return [sb(shape, dt, f"{name}{i}") for i in range(n)]

        def ps(shape, dt, name):
            t = st.enter_context(nc.psum_tensor(name, shape, dt))
            return t, Buf(name)

        V, S_, P_, T_ = nc.vector, nc.scalar, nc.gpsimd, nc.tensor

        def dve(fn, n, reads, writes, mode=1.0):
            kb.op("dve", fn, reads, writes, cost=(n / mode + 150.0) / 960.0)

        def act(fn, n, reads, writes):
            kb.op("act", fn, reads, writes, cost=(n + 260.0) / 1200.0)

        def pe(fn, cols, reads, writes):
            kb.op("pe", fn, reads, writes, cost=cols / 2000.0 + 0.06)

        def pool(fn, cost, reads, writes):
            kb.op("pool", fn, reads, writes, cost=cost)

        w_in_bf, b_win = sb([128, 8, DIN], BF16, "w_in_bf")
        w_out_bf, b_wout = sb([128, 8, D], BF16, "w_out_bf")
        cst, b_cst = sb([128, 1024], F32, "cst")
        ident_bf, b_idbf = sb([128, 128], BF16, "ident_bf")
        mask_r = cst[:, 0:512]
        mask_g = cst[:, 512:640]
        Lp = cst[:, 640:768]
        Up = cst[:, 768:896]
        identf = cst[:, 896:1024]
        xs = sbn(3, [128, D], F32, "xs")
        tabs = sbn(2, [128, 1024], F32, "tabs")
        outs = sbn(2, [128, D], F32, "outs")
        xn_ = sbn(2, [128, D], BF16, "xn")
        hT = sbn(2, [128, 8, 128], BF16, "hT")
        b_hTb = [Buf("hTb0"), Buf("hTb1")]
        rA, b_rA = sb([128, 512], F32, "rA")
        rB, b_rB = sb([128, 512], F32, "rB")
        q_r_ = sbn(2, [128, 512], BF16, "q_r")
        k_r_ = sbn(2, [128, 512], BF16, "k_r")
        v_bf_ = sbn(2, [128, 512], BF16, "v_bf")
        gv_bf_ = sbn(2, [128, 512], BF16, "gv_bf")
        szr_ = sbn(2, [128, 512], F32, "szr")
        szg_ = sbn(2, [128, 512], F32, "szg")
        qkT_ = sbn(2, [128, 8, 128], BF16, "qkT")
        geT_ = sbn(2, [128, 6, 128], BF16, "geT")
        q_e_ = sbn(2, [128, 256], BF16, "q_e")
        k_e_ = sbn(2, [128, 256], BF16, "k_e")
        e_sb, b_e = sb([128, 256], F32, "e_sb")
        l_sb_ = sbn(2, [128, 256], F32, "l_sb")
        E1_ = sbn(2, [128, 256], F32, "E1")
        E2_ = sbn(2, [128, 256], F32, "E2")
        a_fm_ = sbn(2, [128, 2], F32, "a_fm")
        glrT_ = sbn(2, [32, 128], BF16, "glrT")
        wg_aug, b_wg = sb([32, 256], BF16, "wg_aug")
        ST_r_ = sbn(2, [128, 512], BF16, "ST_r")
        ST_g_ = sbn(2, [128, 512], BF16, "ST_g")
        R, b_R = sb([128, 4, 128], F32, "R")
        R_bf, b_Rbf = sb([128, 4, 128], BF16, "R_bf")
        Sg, b_Sg = sb([128, 2, 128], F32, "Sg")
        Sg_bf, b_Sgbf = sb([128, 2, 128], BF16, "Sg_bf")
        mix_ = sbn(2, [128, D], BF16, "mix")
        mixT_ = sbn(2, [128, 8, 128], BF16, "mixT")
        res_ = sbn(2, [128, D], F32, "res")
        gate_bc, b_gbc = sb([128, D], F32, "gate_bc")
        fg_bc, b_fgbc = sb([128, D], F32, "fg_bc")
        ada_tm, b_adatm = sb([NB, 3 * D], F32, "ada_tm")
        bg_bc, b_bgbc = sb([128, D], F32, "bg_bc")
        sel, b_sel = sb([NB, NB, 128], F32, "sel")
        stats_ = [st.enter_context(nc.sbuf_tensor(f"S_stats{i}", [128, 16], F32)) for i in range(2)]
        msq_ = [st.enter_context(nc.sbuf_tensor(f"S_msq{i}", [128, 16], F32)) for i in range(2)]
        rstd_ = [st.enter_context(nc.sbuf_tensor(f"S_rstd{i}", [128, 16], F32)) for i in range(2)]
        b_stats = [{k: Buf(f"stats{i}{k}") for k in "xyf"} for i in range(2)]
        b_msq = [{k: Buf(f"msq{i}{k}") for k in "xyf"} for i in range(2)]
        b_rstd = [{k: Buf(f"rstd{i}{k}") for k in "xyf"} for i in range(2)]
        eps_t, b_eps = sb([128, 1], F32, "eps_t")
        m16, b_m16 = sb([128, 2], F32, "m16")
        cT, b_cT = sb([128, 8, NB], F32, "cT")
        ng_fm, b_ngfm = sb([128, 8], F32, "ng_fm")
        bada_fm, b_badafm = sb([128, 24], F32, "bada_fm")
        gng_fm, b_gngfm = sb([128, 1], F32, "gng_fm")
        ada_fm, b_ada = sb([128, 24, NB], F32, "ada_fm")
        A_fm, b_Afm = sb([128, 8, NB], F32, "A_fm")

        headb = [ps([128, 512], F32, f"phead{i}") for i in range(2)]
        tailb = [ps([128, 512], F32, f"ptail{i}") for i in range(3)]
        pT_ = [ps([128, 8, 128], BF16, f"pT{i}") for i in range(2)]
        pM, b_pM = ps([128, 512], F32, "pM")
        _rr = {"h": 0, "t": 0}

        def bank(kind="t"):
            pool_ = headb if kind == "h" else tailb
            t = pool_[_rr[kind] % len(pool_)]
            _rr[kind] += 1
            return t

        for s in ["ld_x0", "ld_x1", "ld_x2", "ld_t0", "ld_t1", "ld_s0", "ld_s1", "ld_s2", "ld_s3", "st0", "st1"]:
            kb.new_sem(s)

        kb.dma("sp", None, [(cst[:], cst_d[:, :])], writes=[b_cst])
        kb.dma("sp", None, [(cT[:, :, b], c_d[b:b + 1, :].rearrange("o (k p) -> p (o k)", p=128)) for b in range(NB)],
               writes=[b_cT], allow_slow_non_contiguous=True)
        kb.dma("sp", None, [(ng_fm[:], ng_d.rearrange("o (k p) -> p (o k)", p=128))], writes=[b_ngfm],
               allow_slow_non_contiguous=True)
        kb.dma("sp", None, [(bada_fm[:, j * 8:(j + 1) * 8], bada_d[:, j * 1024:(j + 1) * 1024].rearrange("o (k p) -> p (o k)", p=128))
                            for j in range(3)], writes=[b_badafm], allow_slow_non_contiguous=True)
        kb.dma("sp", None, [(gng_fm[:], gng_d.rearrange("o (p one) -> (o p) one", one=1))], writes=[b_gngfm],
               allow_slow_non_contiguous=True)
        kb.dma("sp", None, [(fg_bc[:], fg_d[0:1, :].to_broadcast([128, D]))], writes=[b_fgbc])
        kb.dma("pool", None, [(w_in_bf[:, kc, :], win_d[kc * 128:(kc + 1) * 128, :]) for kc in range(8)],
               writes=[b_win], cost=90.0, max_dma_last_dim=4096)
        kb.dma("pool", None, [(w_out_bf[:, kc, :], wout_d[kc * 128:(kc + 1) * 128, :]) for kc in range(4)],
               writes=[b_wout], cost=20.0, max_dma_last_dim=4096)
        kb.dma("pool", None, [(wg_aug[0:16, :], wg_d[:, :]), (wg_aug[16:17, :], bg_d[:, :])], writes=[b_wg])

        dve(lambda: V.memset(eps_t[:], EPS), 1, [], [b_eps])
        dve(lambda: V.memset(m16[:], -1.0 / 16.0), 2, [], [b_m16])
        for i in range(2):
            dve(lambda i=i: V.memset(glrT_[i][0][:], 1.0), 128, [], [glrT_[i][1]])
            dve(lambda i=i: V.memset(geT_[i][0][:], 0.0), 768, [], [geT_[i][1]])
        dve(lambda: V.tensor_copy(out=ident_bf[:], in_=identf), 128, [b_cst], [b_idbf])

        for kc in range(4, 8):
            stg, b_stg = outs[kc % 2]
            kb.dma("sp", f"ld_s{kc % 2}", [(stg[:], wout_d[kc * 128:(kc + 1) * 128, :])], writes=[b_stg])
            dve(lambda kc=kc, stg=stg: V.tensor_scalar(out=w_out_bf[:, kc, :], in0=stg[:], scalar1=gng_fm[:, 0:1], scalar2=None, op0=ALU.mult),
                1024, [b_stg, b_gngfm], [b_wout])

        act(lambda: S_.activation(out=cT[:], in_=cT[:], func=AF.Silu), 8 * NB, [b_cT], [b_cT])
        kb.dma("sp", None, [(bg_bc[:], bada_d[0:1, 2048:3072].to_broadcast([128, D]))], writes=[b_bgbc])
        for bb in range(NB):
            dve(lambda bb=bb: V.tensor_copy(out=sel[:, bb, :], in_=identf[0:NB, bb:bb + 1].to_broadcast([NB, 128])), 128, [b_cst], [b_sel])
        accb = [headb[0], headb[1], tailb[0], tailb[1], tailb[2], (pM, b_pM)]
        stg_list = [outs[0], outs[1], res_[0], res_[1]]
        si = 0
        for kc in range(8):
            for th in range(3):
                stg, b_stg = stg_list[si % 4]
                kb.dma("sp", f"ld_s{si % 4}", [(stg[:], wada_d[kc * 128:(kc + 1) * 128, th * 1024:(th + 1) * 1024])], writes=[b_stg])
                (pa, b_pa), (pb2, b_pb2) = accb[2 * th], accb[2 * th + 1]

                def mm_ada(kc=kc, stg=stg, pa=pa, pb2=pb2):
                    T_.matmul(pa[0:NB, :], lhsT=cT[:, kc, :], rhs=stg[:, 0:512], start=(kc == 0), stop=(kc == 7))
                    return T_.matmul(pb2[0:NB, :], lhsT=cT[:, kc, :], rhs=stg[:, 512:1024], start=(kc == 0), stop=(kc == 7))
                pe(mm_ada, 2 * 4 * 512, [b_stg, b_cT], [b_pa, b_pb2])
                si += 1
        for blk in range(6):
            pa, b_pa = accb[blk]
            act(lambda blk=blk, pa=pa: S_.copy(out=ada_tm[:, blk * 512:(blk + 1) * 512], in_=pa[0:NB, :]), 512, [b_pa], [b_adatm])

        def tr_ada():
            for j in range(16):
                i_ = T_.transpose(out=pM[:, j * NB:(j + 1) * NB], in_=ada_tm[0:NB, j * 128:(j + 1) * 128], identity=identf[0:NB, 0:NB])
            return i_
        pe(tr_ada, 16 * 64 * 4, [b_adatm, b_cst], [b_pM])
        dve(lambda: V.tensor_tensor(out=ada_fm[:, 0:16, :], in0=pM[:, 0:16 * NB].rearrange("p (j b) -> p j b", b=NB),
                                    in1=bada_fm[:, 0:16].unsqueeze(2).to_broadcast([128, 16, NB]), op=ALU.add), 16 * NB, [b_pM, b_badafm], [b_ada])
        dve(lambda: V.tensor_scalar(out=A_fm[:], in0=ada_fm[:, 8:16, :], scalar1=1.0, scalar2=None, op0=ALU.add), 8 * NB, [b_ada], [b_Afm])
        dve(lambda: V.tensor_tensor(out=A_fm[:], in0=A_fm[:], in1=ng_fm[:].unsqueeze(2).to_broadcast([128, 8, NB]), op=ALU.mult),
            8 * NB, [b_Afm, b_ngfm], [b_Afm])

        cd = [G[h] ** 128.0 for h in range(4)]
        LN_EIGHTH = math.log(0.125)

        def rsqrt_cols(par, grp, c0, c1, inv_n):
            stt, msq, rstd = stats_[par], msq_[par], rstd_[par]

            act(lambda: S_.activation(out=msq[:, c0:c1], in_=stt[:, c0:c1], func=AF.Ln, scale=inv_n, bias=eps_t[:, 0:1]),
                c1 - c0, [b_stats[par][grp], b_eps], [b_msq[par][grp]])
            act(lambda: S_.activation(out=rstd[:, c0:c1], in_=msq[:, c0:c1], func=AF.Exp, scale=-0.5),
                c1 - c0, [b_msq[par][grp]], [b_rstd[par][grp]])

        def load_tile(g):
            t = g % NT
            xt, b_x = xs[g % 3]
            tt, b_t = tabs[g % 2]
            kb.dma("sp", f"ld_x{g % 3}", [(xt[:], x_d[g * 128:(g + 1) * 128, :])], writes=[b_x])
            kb.dma("sp", f"ld_t{g % 2}", [(tt[:], tab_d[t, :, :])], writes=[b_t])

        def seq_start(b):
            dve(lambda: V.memset(R[:], 0.0), 512, [], [b_R])
            dve(lambda: V.memset(R_bf[:], 0.0), 512, [], [b_Rbf], mode=2.0)
            dve(lambda: V.memset(Sg[:], 0.0), 256, [], [b_Sg])
            dve(lambda: V.memset(Sg_bf[:], 0.0), 256, [], [b_Sgbf], mode=2.0)
            for n in range(2):
                pb, b_pb = tailb[1 + n]
                pe(lambda n=n, pb=pb: T_.matmul(pb[:, :], lhsT=sel[:, b, :], rhs=ada_tm[0:NB, 2048 + n * 512:2048 + (n + 1) * 512], start=True, stop=True),
                   4 * 512, [b_sel, b_adatm], [b_pb])
                dve(lambda n=n, pb=pb: V.tensor_tensor(out=gate_bc[:, n * 512:(n + 1) * 512], in0=pb[:], in1=bg_bc[:, n * 512:(n + 1) * 512], op=ALU.add),
                    512, [b_pb, b_bgbc], [b_gbc])

        def rotary(pb, b_pb, tt, b_t, toff, dst, b_dst):
            src4 = pb[:].rearrange("p (h two f) -> p h two f", h=4, two=2)
            cs4 = tt[:, toff:toff + 256].rearrange("p (h f) -> p h f", h=4).unsqueeze(2).to_broadcast([128, 4, 2, 64])
            sn4 = tt[:, toff + 256:toff + 512].rearrange("p (h f) -> p h f", h=4).unsqueeze(2).to_broadcast([128, 4, 2, 64])
            A4 = rA[:].rearrange("p (h two f) -> p h two f", h=4, two=2)
            B4 = rB[:].rearrange("p (h two f) -> p h two f", h=4, two=2)
            d4 = dst[:].rearrange("p (h two f) -> p h two f", h=4, two=2)
            dve(lambda: V.tensor_tensor(out=A4, in0=src4, in1=cs4, op=ALU.mult), 512, [b_pb, b_t], [b_rA])
            dve(lambda: V.tensor_tensor(out=B4, in0=src4, in1=sn4, op=ALU.mult), 512, [b_pb, b_t], [b_rB])
            dve(lambda: V.tensor_tensor(out=d4[:, :, 0, :], in0=A4[:, :, 0, :], in1=B4[:, :, 1, :], op=ALU.subtract), 256, [b_rA, b_rB], [b_dst])
            dve(lambda: V.tensor_tensor(out=d4[:, :, 1, :], in0=B4[:, :, 0, :], in1=A4[:, :, 1, :], op=ALU.add), 256, [b_rA, b_rB], [b_dst])

        def inproj_group(hTt, b_hT, c0, ncols):
            pb, b_pb = bank("h")

            def mm():
                for kc in range(8):
                    i = T_.matmul(pb[:, 0:ncols], lhsT=hTt[:, kc, :], rhs=w_in_bf[:, kc, c0:c0 + ncols], start=(kc == 0), stop=(kc == 7))
                return i
            pe(mm, 8 * ncols, list(b_hT) + [b_win], [b_pb])
            return pb, b_pb

        def tile_body(g, phase):
            b = g // NT
            t = g % NT
            par = g % 2
            xt, b_x = xs[g % 3]
            tt, b_t = tabs[par]
            hTt, b_hT = hT[par]
            ot, b_o = outs[par]
            xn, b_xn = xn_[par]
            q_r, b_qr = q_r_[par]
            k_r, b_kr = k_r_[par]
            v_bf, b_v = v_bf_[par]
            gv_bf, b_gv = gv_bf_[par]
            szr, b_szr = szr_[par]
            szg, b_szg = szg_[par]
            qkT, b_qkT = qkT_[par]
            geT, b_geT = geT_[par]
            q_e, b_qe = q_e_[par]
            k_e, b_ke = k_e_[par]
            l_sb, b_l = l_sb_[par]
            E1, b_E1 = E1_[par]
            E2, b_E2 = E2_[par]
            a_fm, b_afm = a_fm_[par]
            glrT, b_glrT = glrT_[par]
            ST_r, b_STr = ST_r_[par]
            ST_g, b_STg = ST_g_[par]
            mix, b_mix = mix_[par]
            mixT, b_mixT = mixT_[par]
            res, b_res = res_[par]
            stats, rstd = stats_[par], rstd_[par]
            bs, br = b_stats[par], b_rstd[par]
            pT, b_pT = pT_[par]
            if phase == 1:
                if t == 0:
                    seq_start(b)
                return tile_main(locals())
            act(lambda: S_.activation(out=xn[:], in_=xt[:], func=AF.Square, accum_out=stats[:, 0:1]), 1024, [b_x], [bs["x"], b_xn])
            rsqrt_cols(par, "x", 0, 1, 1.0 / D)
            act(lambda: S_.activation(out=xn[:], in_=xt[:], func=AF.Copy, scale=rstd[:, 0:1]), 1024, [b_x, br["x"]], [b_xn])
            def tr_x():
                for kc in range(8):
                    i = T_.transpose(out=pT[:, kc, :], in_=xn[:, kc * 128:(kc + 1) * 128], identity=ident_bf[:])
                return i
            pe(tr_x, 1024, [b_xn, b_idbf], [b_pT])

            b_hT2 = b_hTb[par]

            def ev_h():
                for kc in range(4):
                    i = S_.activation(out=hTt[:, kc, :], in_=pT[:, kc, :], func=AF.Identity,
                                      scale=A_fm[:, kc, b:b + 1], bias=ada_fm[:, kc, b:b + 1])
                return i
            kb.op("act", ev_h, [b_pT, b_Afm, b_ada], [b_hT], cost=4 * 0.38)

            def ev_h2():
                for kc in range(4, 8):
                    i = V.tensor_scalar(out=hTt[:, kc, :], in0=pT[:, kc, :], scalar1=A_fm[:, kc, b:b + 1], scalar2=ada_fm[:, kc, b:b + 1],
                                        op0=ALU.mult, op1=ALU.add)
                return i
            def ev_h2a():
                for kc in range(4, 8):
                    i = S_.activation(out=hTt[:, kc, :], in_=pT[:, kc, :], func=AF.Identity,
                                      scale=A_fm[:, kc, b:b + 1], bias=ada_fm[:, kc, b:b + 1])
                return i
            if EVH2:
                kb.op("dve", ev_h2, [b_pT, b_Afm, b_ada], [b_hT2], cost=4 * 0.25)
            else:
                kb.op("act", ev_h2a, [b_pT, b_Afm, b_ada], [b_hT2], cost=4 * 0.38)
            def mm_glr():
                for kc in range(8):
                    i = T_.matmul(pM[0:16, 256:384], lhsT=w_in_bf[:, kc, 3584:3600], rhs=hTt[:, kc, :], start=(kc == 0), stop=(kc == 7))
                return i
            pe(mm_glr, 1024, [b_hT, b_hT2, b_win], [b_pM])
            act(lambda: S_.copy(out=glrT[0:16, :], in_=pM[0:16, 256:384]), 128, [b_pM], [b_glrT])
            pe(lambda: T_.matmul(pM[:, 0:256], lhsT=glrT[0:17, :], rhs=wg_aug[0:17, :], start=True, stop=True), 256, [b_glrT, b_wg], [b_pM])
            act(lambda: S_.activation(out=e_sb[:], in_=pM[:, 0:256], func=AF.Exp, scale=-1.0), 256, [b_pM], [b_e])
            act(lambda: S_.activation(out=l_sb[:], in_=e_sb[:], func=AF.Ln, bias=1.0), 256, [b_e], [b_l])

            def mm_bl():
                T_.matmul(pM[:, 384:385], lhsT=l_sb[:, 0:128], rhs=m16[:, 0:1], start=True, stop=True)
                i = T_.matmul(pM[:, 385:386], lhsT=l_sb[:, 128:256], rhs=m16[:, 0:1], start=True, stop=True)
                return i
            if BSPLIT:
                pe(mm_bl, 512, [b_l, b_m16], [b_pM])
                pe(lambda: T_.matmul(pM[:, 0:256], lhsT=Lp, rhs=l_sb[:], start=True, stop=True), 1024, [b_l, b_cst], [b_pM])
            else:
                def mm_b():
                    mm_bl()
                    return T_.matmul(pM[:, 0:256], lhsT=Lp, rhs=l_sb[:], start=True, stop=True)
                pe(mm_b, 512 + 1024, [b_l, b_m16, b_cst], [b_pM])
            act(lambda: S_.activation(out=E1[:], in_=pM[:, 0:256], func=AF.Exp, bias=LN_EIGHTH), 256, [b_pM], [b_E1])
            act(lambda: S_.activation(out=E2[:], in_=pM[:, 0:256], func=AF.Exp, scale=-1.0), 256, [b_pM], [b_E2])
            act(lambda: S_.activation(out=a_fm[:], in_=pM[:, 384:386], func=AF.Exp), 2, [b_pM], [b_afm])
            return None

        def tile_main(L):
            g, b, t, par = L["g"], L["b"], L["t"], L["par"]
            xt, b_x, tt, b_t, hTt, b_hT, ot, b_o = L["xt"], L["b_x"], L["tt"], L["b_t"], L["hTt"], L["b_hT"], L["ot"], L["b_o"]
            b_hT2 = b_hTb[par]
            q_r, b_qr, k_r, b_kr, v_bf, b_v, gv_bf, b_gv = L["q_r"], L["b_qr"], L["k_r"], L["b_kr"], L["v_bf"], L["b_v"], L["gv_bf"], L["b_gv"]
            szr, b_szr, szg, b_szg, qkT, b_qkT, geT, b_geT = L["szr"], L["b_szr"], L["szg"], L["b_szg"], L["qkT"], L["b_qkT"], L["geT"], L["b_geT"]
            q_e, b_qe, k_e, b_ke = L["q_e"], L["b_qe"], L["k_e"], L["b_ke"]
            E1, b_E1, E2, b_E2, a_fm, b_afm = L["E1"], L["b_E1"], L["E2"], L["b_E2"], L["a_fm"], L["b_afm"]
            ST_r, b_STr, ST_g, b_STg, mix, b_mix, mixT, b_mixT, res, b_res = (L["ST_r"], L["b_STr"], L["ST_g"], L["b_STg"], L["mix"], L["b_mix"],
                                                                                L["mixT"], L["b_mixT"], L["res"], L["b_res"])
            stats, rstd, bs, br, pT, b_pT = L["stats"], L["rstd"], L["bs"], L["br"], L["pT"], L["b_pT"]
            pq, b_pq = inproj_group(hTt, (b_hT, b_hT2), 0, 512)
            rotary(pq, b_pq, tt, b_t, 0, q_r, b_qr)
            pk, b_pk = inproj_group(hTt, (b_hT, b_hT2), 512, 512)
            rotary(pk, b_pk, tt, b_t, 512, k_r, b_kr)
            pg, b_pg = inproj_group(hTt, (b_hT, b_hT2), 2048, 512)
            dve(lambda: V.tensor_tensor(out=q_e[:], in0=pg[:, 0:256], in1=E1[:], op=ALU.mult), 256, [b_pg, b_E1], [b_qe])
            dve(lambda: V.tensor_tensor(out=k_e[:], in0=pg[:, 256:512], in1=E2[:], op=ALU.mult), 256, [b_pg, b_E2], [b_ke])
            pv, b_pv = inproj_group(hTt, (b_hT, b_hT2), 1024, 512)
            act(lambda: S_.copy(out=v_bf[:], in_=pv[:]), 512, [b_pv], [b_v])
            pgv, b_pgv = inproj_group(hTt, (b_hT, b_hT2), 2560, 512)
            act(lambda: S_.copy(out=gv_bf[:], in_=pgv[:]), 512, [b_pgv], [b_gv])
            pz, b_pz = inproj_group(hTt, (b_hT, b_hT2), 1536, 512)
            pgz, b_pgz = inproj_group(hTt, (b_hT, b_hT2), 3072, 512)

            def silus():
                S_.activation(out=szr[:], in_=pz[:], func=AF.Silu)
                return S_.activation(out=szg[:], in_=pgz[:], func=AF.Silu)
            kb.op("act", silus, [b_pz, b_pgz], [b_szr, b_szg], cost=2 * 0.64)
            def tr_qk():
                for h in range(4):
                    T_.transpose(out=pT[:, h, :], in_=q_r[:, h * 128:(h + 1) * 128], identity=ident_bf[:])
                for h in range(4):
                    i = T_.transpose(out=pT[:, 4 + h, :], in_=k_r[:, h * 128:(h + 1) * 128], identity=ident_bf[:])
                return i
            pe(tr_qk, 1024, [b_qr, b_kr, b_idbf], [b_pT])
            dve(lambda: V.tensor_copy(out=qkT[:], in_=pT[:]), 1024, [b_pT], [b_qkT], mode=2.0)
            def tr_ge():
                for u in range(2):
                    T_.transpose(out=pT[:, u, :], in_=q_e[:, u * 128:(u + 1) * 128], identity=ident_bf[:])
                for u in range(2):
                    i = T_.transpose(out=pT[:, 2 + u, :], in_=k_e[:, u * 128:(u + 1) * 128], identity=ident_bf[:])
                return i
            pe(tr_ge, 512, [b_qe, b_ke, b_idbf], [b_pT])

            def ev_ge():
                for h in range(4):
                    r0 = (h % 2) * 64
                    V.tensor_copy(out=geT[r0:r0 + 64, h, :], in_=pT[r0:r0 + 64, h // 2, :])
                return V.tensor_copy(out=geT[:, 4:6, :], in_=pT[:, 2:4, :])
            kb.op("dve", ev_ge, [b_pT], [b_geT], cost=5 * 0.25)
            (T0, bT0), (T1, bT1), (T2, bT2) = tailb
            psr, b_psr = T0, bT0
            psg, b_psg = T1, bT1
            pyr, b_pyr = T2, bT2
            pkr, b_pkr = T0, bT0
            pyg, b_pyg = T1, bT1
            pkg, b_pkg = T0, bT0

            def mm_sr():
                for h in range(4):
                    i = T_.matmul(psr[:, h * 128:(h + 1) * 128], lhsT=qkT[:, 4 + h, :], rhs=qkT[:, h, :], start=True, stop=True)
                return i
            pe(mm_sr, 512, [b_qkT], [b_psr])
            dve(lambda: V.tensor_tensor(out=ST_r[:], in0=psr[:], in1=mask_r, op=ALU.mult), 512, [b_psr, b_cst], [b_STr])

            def mm_sg():
                for h in range(4):
                    i = T_.matmul(psg[:, h * 128:(h + 1) * 128], lhsT=geT[:, 4 + h // 2, :], rhs=geT[:, h, :], start=True, stop=True)
                return i
            pe(mm_sg, 512, [b_geT], [b_psg])
            dve(lambda: V.tensor_tensor(out=ST_g[:].rearrange("p (h i) -> p h i", h=4), in0=psg[:].rearrange("p (h i) -> p h i", h=4),
                                        in1=mask_g.unsqueeze(1).to_broadcast([128, 4, 128]), op=ALU.mult), 512, [b_psg, b_cst], [b_STg])

            def mm_yr():
                for h in range(4):
                    T_.matmul(pyr[:, h * 128:(h + 1) * 128], lhsT=ST_r[:, h * 128:(h + 1) * 128], rhs=v_bf[:, h * 128:(h + 1) * 128],
                              start=True, stop=False)
                    i = T_.matmul(pyr[:, h * 128:(h + 1) * 128], lhsT=qkT[:, h, :], rhs=R_bf[:, h, :], start=False, stop=True)
                return i
            pe(mm_yr, 1024, [b_STr, b_v, b_qkT, b_Rbf], [b_pyr])

            def mm_kr():
                for h in range(4):
                    i = T_.matmul(pkr[:, h * 128:(h + 1) * 128], lhsT=k_r[:, h * 128:(h + 1) * 128], rhs=v_bf[:, h * 128:(h + 1) * 128],
                                  start=True, stop=True)
                return i
            pe(mm_kr, 512, [b_kr, b_v], [b_pkr])

            def up_R():
                for h in range(4):
                    i = V.scalar_tensor_tensor(out=R[:, h, :], in0=R[:, h, :], scalar=cd[h], in1=pkr[:, h * 128:(h + 1) * 128],
                                               op0=ALU.mult, op1=ALU.add)
                return i
            kb.op("dve", up_R, [b_pkr], [b_R], cost=4 * 0.3)
            act(lambda: S_.copy(out=R_bf[:], in_=R[:]), 512, [b_R], [b_Rbf])

            def mm_yg():
                for h in range(4):
                    T_.matmul(pyg[:, h * 128:(h + 1) * 128], lhsT=ST_g[:, h * 128:(h + 1) * 128], rhs=gv_bf[:, h * 128:(h + 1) * 128],
                              start=True, stop=False)
                    i = T_.matmul(pyg[:, h * 128:(h + 1) * 128], lhsT=geT[:, h, :], rhs=Sg_bf[:, h // 2, :], start=False, stop=True)
                return i
            pe(mm_yg, 1024, [b_STg, b_gv, b_geT, b_Sgbf], [b_pyg])

            def mm_kg():
                for h in range(4):
                    r0 = (h % 2) * 64
                    u = h // 2
                    i = T_.matmul(pkg[r0:r0 + 64, u * 128:(u + 1) * 128], lhsT=k_e[:, h * 64:(h + 1) * 64], rhs=gv_bf[:, h * 128:(h + 1) * 128],
                                  start=True, stop=True)
                return i
            pe(mm_kg, 512, [b_ke, b_gv], [b_pkg])
            Sg2 = Sg[:].rearrange("p u e -> p (u e)")
            dve(lambda: V.tensor_tensor(out=Sg2, in0=Sg2, in1=pkg[:, 0:256], op=ALU.add), 256, [b_pkg], [b_Sg])
            dve(lambda: V.tensor_tensor(out=Sg[:], in0=Sg[:], in1=a_fm[:].unsqueeze(2).to_broadcast([128, 2, 128]), op=ALU.mult), 256, [b_afm], [b_Sg])
            act(lambda: S_.copy(out=Sg_bf[:], in_=Sg[:]), 256, [b_Sg], [b_Sgbf])
            def sq_y():
                for h in range(4):
                    S_.activation(out=mix[:, h * 128:(h + 1) * 128], in_=pyr[:, h * 128:(h + 1) * 128], func=AF.Square, accum_out=stats[:, 1 + h:2 + h])
                for h in range(4):
                    i = S_.activation(out=mix[:, 512 + h * 128:512 + (h + 1) * 128], in_=pyg[:, h * 128:(h + 1) * 128], func=AF.Square, accum_out=stats[:, 5 + h:6 + h])
                return i
            kb.op("act", sq_y, [b_pyr, b_pyg], [bs["y"], b_mix], cost=8 * 0.42)
            rsqrt_cols(par, "y", 1, 9, 1.0 / 128.0)

            def mk_mix():
                for h in range(4):
                    V.scalar_tensor_tensor(out=mix[:, h * 128:(h + 1) * 128], in0=pyr[:, h * 128:(h + 1) * 128], scalar=rstd[:, 1 + h:2 + h],
                                           in1=szr[:, h * 128:(h + 1) * 128], op0=ALU.mult, op1=ALU.mult)
                for h in range(4):
                    i = V.scalar_tensor_tensor(out=mix[:, 512 + h * 128:512 + (h + 1) * 128], in0=pyg[:, h * 128:(h + 1) * 128],
                                               scalar=rstd[:, 5 + h:6 + h], in1=szg[:, h * 128:(h + 1) * 128], op0=ALU.mult, op1=ALU.mult)
                return i
            kb.op("dve", mk_mix, [b_pyr, b_pyg, br["y"], b_szr, b_szg], [b_mix], cost=8 * 0.3)
            def tr_m():
                for kc in range(8):
                    i = T_.transpose(out=pT[:, kc, :], in_=mix[:, kc * 128:(kc + 1) * 128], identity=ident_bf[:])
                return i
            pe(tr_m, 1024, [b_mix, b_idbf], [b_pT])
            dve(lambda: V.tensor_copy(out=mixT[:], in_=pT[:]), 1024, [b_pT], [b_mixT], mode=2.0)
            for n in range(2):
                po, b_po = tailb[0] if n == 0 else tailb[2]

                def mm_o(n=n, po=po):
                    for kc in range(8):
                        i = T_.matmul(po[:], lhsT=mixT[:, kc, :], rhs=w_out_bf[:, kc, n * 512:(n + 1) * 512], start=(kc == 0), stop=(kc == 7))
                    return i
                pe(mm_o, 8 * 512, [b_mixT, b_wout], [b_po])
                sl = slice(n * 512, (n + 1) * 512)
                dve(lambda po=po, sl=sl: V.tensor_tensor(out=res[:, sl], in0=po[:], in1=gate_bc[:, sl], op=ALU.mult), 512, [b_po, b_gbc], [b_res])
            dve(lambda: V.tensor_tensor(out=res[:], in0=res[:], in1=xt[:], op=ALU.add), 1024, [b_res, b_x], [b_res])
            act(lambda: S_.activation(out=ot[:], in_=res[:], func=AF.Square, accum_out=stats[:, 9:10]), 1024, [b_res], [bs["f"], b_o])
            rsqrt_cols(par, "f", 9, 10, 1.0 / D)
            dve(lambda: V.scalar_tensor_tensor(out=ot[:], in0=res[:], scalar=rstd[:, 9:10], in1=fg_bc[:], op0=ALU.mult, op1=ALU.mult),
                1024, [b_res, br["f"], b_fgbc], [b_o])
            kb.dma("sp", f"st{par}", [(out_d[g * 128:(g + 1) * 128, :], ot[:])], reads=[b_o])

        NG = NB * NT
        kb.tag = 0
        load_tile(0)
        tile_body(0, 0)
        for g in range(NG):
            if g + 1 < NG:
                kb.tag = g + 1
                load_tile(g + 1)
                tile_body(g + 1, 0)
            kb.tag = g
            tile_body(g, 1)
        kb.run()
        build_nc.last_est_us = kb.est_us
        build_nc.last_kb = kb
    return nc


_CACHE = {}


def kernel(x, c, norm_gain, w_ada, b_ada, w_in, w_gate_up, b_gate_up, gla_norm_gain, w_out, final_gain):
    NB, NT = 4, 16
    f = lambda a: np.ascontiguousarray(np.asarray(a, dtype=np.float32))
    x = f(x)
    c = f(c)
    if "nc" not in _CACHE:
        _CACHE["nc"] = build_nc(NB, NT)
        _CACHE["consts"] = make_consts()
    nc = _CACHE["nc"]
    tab, cst = _CACHE["consts"]
    shared = {
        "norm_gain": f(norm_gain).reshape(1, D), "w_ada": f(w_ada).reshape(D, 3 * D), "b_ada": f(b_ada).reshape(1, 3 * D),
        "w_in": f(w_in).reshape(D, DIN), "w_gate_up": f(w_gate_up).reshape(16, 256), "b_gate_up": f(b_gate_up).reshape(1, 256),
        "gla_norm_gain": f(gla_norm_gain).reshape(1, 128), "w_out": f(w_out).reshape(D, D), "final_gain": f(final_gain).reshape(1, D),
        "tab": tab, "cst": cst,
    }
    in_maps = []
    for i in range(NCORES):
        m = dict(shared)
        m["x"] = x[i * NB:(i + 1) * NB].reshape(NB * SEQ, D)
        m["c"] = c[i * NB:(i + 1) * NB]
        in_maps.append(m)
    res = run_bass_kernel_spmd(nc, in_maps, core_ids=list(range(NCORES)))
    out = np.concatenate([r["out"].reshape(NB, SEQ, D) for r in res.results], axis=0)
    return out.astype(np.float32)
```

```python
import math
from contextlib import ExitStack

import numpy as np
import concourse.bass as bass
import concourse.mybir as mybir
from concourse.bass_utils import run_bass_kernel_spmd

F32 = mybir.dt.float32
BF16 = mybir.dt.bfloat16
AF = mybir.ActivationFunctionType
ALU = mybir.AluOpType

D = 1024
SEQ = 2048
DIN = 3600
NCORES = 8
EPS = 1e-6
G = [1.0 - 2.0 ** (-5.0 - h) for h in range(4)]
SKIP_PE_SELF = False
SEQ_ORDER = False
EVH2 = False
BSPLIT = True
LAT_PE = 0.7
LAT_SAME = 0.1
LAT_X = 0.3


class Buf:
    __slots__ = ("name",)

    def __init__(self, name):
        self.name = name


class Rec:
    __slots__ = ("kind", "e", "fn", "reads", "writes", "cost", "sem", "pairs", "kw", "deps", "raw", "start", "tag")


class KB:
    ENG = ("pe", "act", "dve", "pool", "sp")

    def __init__(self, nc, stack):
        self.nc = nc
        self.engs = {"pe": nc.tensor, "act": nc.scalar, "dve": nc.vector, "pool": nc.gpsimd, "sp": nc.sync}
        self.sems = {}
        self.cnt = {}
        self.stack = stack
        for e in self.engs:
            self.sems[e] = stack.enter_context(nc.semaphore("s_" + e))
            self.cnt[e] = 0
        self.seen = {e: {} for e in self.engs}
        self.prog = []
        self.tag = -1
        self.nwaits = 0

    def new_sem(self, name):
        self.sems[name] = self.stack.enter_context(self.nc.semaphore("s_" + name))
        self.cnt[name] = 0
        return name

    def op(self, e, fn, reads=(), writes=(), cost=0.3):
        r = Rec()
        r.kind, r.e, r.fn, r.reads, r.writes, r.cost = "op", e, fn, tuple(reads), tuple(writes), cost
        r.tag = self.tag
        self.prog.append(r)

    def dma(self, e, sem, pairs, reads=(), writes=(), cost=6.0, **kw):
        if sem is None:
            sem = self.new_sem(f"d{len(self.sems)}")
        r = Rec()
        r.kind, r.e, r.reads, r.writes, r.cost = "dma", e, tuple(reads), tuple(writes), cost
        r.sem, r.pairs, r.kw = sem, list(pairs), kw
        r.tag = self.tag
        self.prog.append(r)

    def _wait(self, e, toks):
        best = {}
        for (s, v) in toks:
            if v > best.get(s, 0):
                best[s] = v
        for s, v in best.items():
            if self.seen[e].get(s, 0) < v:
                self.engs[e].wait_ge(self.sems[s], v)
                self.seen[e][s] = v
                self.nwaits += 1

    def run(self):
        prog = self.prog
        N = len(prog)
        lastw, readers = {}, {}
        succ = [[] for _ in range(N)]
        indeg = [0] * N
        for i, r in enumerate(prog):
            raw, other = set(), set()
            for b in r.reads:
                if b in lastw:
                    raw.add(lastw[b])
            for b in r.writes:
                if b in lastw:
                    other.add(lastw[b])
                other.update(readers.get(b, ()))
            deps = (raw | other)
            deps.discard(i)
            r.deps, r.raw = deps, raw
            for d in deps:
                succ[d].append(i)
            indeg[i] = len(deps)
            for b in r.writes:
                lastw[b] = i
                readers[b] = []
            for b in r.reads:
                if b not in r.writes:
                    readers.setdefault(b, []).append(i)
        finish = [0.0] * N
        ready_t = [0.0] * N
        avail = {e: [] for e in self.ENG}
        free = {e: 0.0 for e in self.ENG}
        for i in range(N):
            if indeg[i] == 0:
                avail[prog[i].e].append(i)
        order = []
        while len(order) < N:
            best = None
            for e in self.ENG:
                fe = free[e]
                for i in avail[e]:
                    key = (max(ready_t[i], fe), i)
                    if best is None or key < best[0]:
                        best = (key, e, i)
            (st, _), e, i = best
            avail[e].remove(i)
            r = prog[i]
            r.start = st
            if r.kind == "dma":
                free[e] = st + 0.15 * len(r.pairs)
            else:
                free[e] = st + r.cost
            finish[i] = st + r.cost
            order.append(i)
            for s_ in succ[i]:
                lat = LAT_SAME if prog[s_].e == e and r.kind == "op" else (LAT_PE if prog[s_].e == "pe" else LAT_X)
                if finish[i] + lat > ready_t[s_]:
                    ready_t[s_] = finish[i] + lat
                indeg[s_] -= 1
                if indeg[s_] == 0:
                    avail[prog[s_].e].append(s_)
        self.est_us = max(finish) if N else 0.0
        self.finish_t = finish
        self.order = order
        if SEQ_ORDER:
            order = list(range(N))
        tok = [None] * N
        dma_last = {}
        for i in order:
            r = prog[i]
            waits = []
            for d in r.deps:
                pd = prog[d]
                if pd.kind == "op" and pd.e == r.e == "pe" and SKIP_PE_SELF:
                    continue
                waits.append(tok[d])
            self._wait(r.e, waits)
            if r.kind == "op":
                inst = r.fn()
                self.cnt[r.e] += 1
                inst.then_inc(self.sems[r.e], 1)
                tok[i] = (r.e, self.cnt[r.e])
            else:
                for (o, a) in r.pairs:
                    inst = self.engs[r.e].dma_start(out=o, in_=a, **r.kw)
                    self.cnt[r.sem] += 16
                    inst.then_inc(self.sems[r.sem], 16)
                tok[i] = (r.sem, self.cnt[r.sem])
                dma_last[r.sem] = tok[i]
        for e in ("sp", "pool"):
            self._wait(e, list(dma_last.values()))


def make_consts():
    C = 128
    pos = np.arange(SEQ, dtype=np.float64)
    invf = 10000.0 ** (-np.arange(0, 128, 2, dtype=np.float64) / 128.0)
    ang = pos[:, None] * invf[None, :]
    cos = np.cos(ang).reshape(16, C, 1, 64)
    sin = np.sin(ang).reshape(16, C, 1, 64)
    p = np.arange(C, dtype=np.float64)
    g = np.array(G, dtype=np.float64)
    sq = g[None, :] ** (p[:, None] + 1.0)
    sk = g[None, :] ** (C - 1.0 - p[:, None]) * (128.0 ** -0.5)
    tab = np.concatenate(
        [cos * sq[None, :, :, None], sin * sq[None, :, :, None], cos * sk[None, :, :, None], sin * sk[None, :, :, None]],
        axis=2,
    )
    tab = tab.reshape(16, C, 1024).astype(np.float32)
    j = np.arange(C)[:, None]
    i = np.arange(C)[None, :]
    caus = (i >= j).astype(np.float64)
    mask_r = np.stack([caus * g[h] ** (-float(C)) for h in range(4)], axis=1).reshape(C, 512)
    Lp = (j <= i).astype(np.float64) * (-1.0 / 16.0)
    Up = (j > i).astype(np.float64) * (-1.0 / 16.0)
    ident = np.eye(C)
    cst = np.concatenate([mask_r, caus, Lp, Up, ident], axis=1).astype(np.float32)
    return tab, cst


def build_nc(NB=4, NT=16):
    NTOK = NB * NT * 128
    nc = bass.Bass("TRN2", target_bir_lowering=False)
    dr = lambda name, shape, kind="ExternalInput": nc.dram_tensor(name, shape, F32, kind=kind).ap()
    x_d = dr("x", [NTOK, D])
    c_d = dr("c", [NB, D])
    ng_d = dr("norm_gain", [1, D])
    wada_d = dr("w_ada", [D, 3 * D])
    bada_d = dr("b_ada", [1, 3 * D])
    win_d = dr("w_in", [D, DIN])
    wg_d = dr("w_gate_up", [16, 256])
    bg_d = dr("b_gate_up", [1, 256])
    gng_d = dr("gla_norm_gain", [1, 128])
    wout_d = dr("w_out", [D, D])
    fg_d = dr("final_gain", [1, D])
    tab_d = dr("tab", [16, 128, 1024])
    cst_d = dr("cst", [128, 1024])
    out_d = dr("out", [NTOK, D], kind="ExternalOutput")

    with ExitStack() as st:
        kb = KB(nc, st)
        _n = [0]

        def sb(shape, dt, name):
            t = st.enter_context(nc.sbuf_tensor("S_" + name, shape, dt))
            return t, Buf(name)

        def sbn(n, shape, dt, name):
            return [sb(shape, dt, f"{name}{i}") for i in range(n)]

        def ps(shape, dt, name):
            t = st.enter_context(nc.psum_tensor(name, shape, dt))
            return t, Buf(name)

        V, S_, P_, T_ = nc.vector, nc.scalar, nc.gpsimd, nc.tensor

        def dve(fn, n, reads, writes, mode=1.0):
            kb.op("dve", fn, reads, writes, cost=(n / mode + 150.0) / 960.0)

        def act(fn, n, reads, writes):
            kb.op("act", fn, reads, writes, cost=(n + 260.0) / 1200.0)

        def pe(fn, cols, reads, writes):
            kb.op("pe", fn, reads, writes, cost=cols / 2000.0 + 0.06)

        def pool(fn, cost, reads, writes):
            kb.op("pool", fn, reads, writes, cost=cost)

        w_in_bf, b_win = sb([128, 8, DIN], BF16, "w_in_bf")
        w_out_bf, b_wout = sb([128, 8, D], BF16, "w_out_bf")
        cst, b_cst = sb([128, 1024], F32, "cst")
        ident_bf, b_idbf = sb([128, 128], BF16, "ident_bf")
        mask_r = cst[:, 0:512]
        mask_g = cst[:, 512:640]
        Lp = cst[:, 640:768]
        Up = cst[:, 768:896]
        identf = cst[:, 896:1024]
        xs = sbn(3, [128, D], F32, "xs")
        tabs = sbn(2, [128, 1024], F32, "tabs")
        outs = sbn(2, [128, D], F32, "outs")
        xn_ = sbn(2, [128, D], BF16, "xn")
        hT = sbn(2, [128, 8, 128], BF16, "hT")
        b_hTb = [Buf("hTb0"), Buf("hTb1")]
        rA, b_rA = sb([128, 512], F32, "rA")
        rB, b_rB = sb([128, 512], F32, "rB")
        q_r_ = sbn(2, [128, 512], BF16, "q_r")
        k_r_ = sbn(2, [128, 512], BF16, "k_r")
        v_bf_ = sbn(2, [128, 512], BF16, "v_bf")
        gv_bf_ = sbn(2, [128, 512], BF16, "gv_bf")
        szr_ = sbn(2, [128, 512], F32, "szr")
        szg_ = sbn(2, [128, 512], F32, "szg")
        qkT_ = sbn(2, [128, 8, 128], BF16, "qkT")
        geT_ = sbn(2, [128, 6, 128], BF16, "geT")
        q_e_ = sbn(2, [128, 256], BF16, "q_e")
        k_e_ = sbn(2, [128, 256], BF16, "k_e")
        e_sb, b_e = sb([128, 256], F32, "e_sb")
        l_sb_ = sbn(2, [128, 256], F32, "l_sb")
        E1_ = sbn(2, [128, 256], F32, "E1")
        E2_ = sbn(2, [128, 256], F32, "E2")
        a_fm_ = sbn(2, [128, 2], F32, "a_fm")
        glrT_ = sbn(2, [32, 128], BF16, "glrT")
        wg_aug, b_wg = sb([32, 256], BF16, "wg_aug")
        ST_r_ = sbn(2, [128, 512], BF16, "ST_r")
        ST_g_ = sbn(2, [128, 512], BF16, "ST_g")
        R, b_R = sb([128, 4, 128], F32, "R")
        R_bf, b_Rbf = sb([128, 4, 128], BF16, "R_bf")
        Sg, b_Sg = sb([128, 2, 128], F32, "Sg")
        Sg_bf, b_Sgbf = sb([128, 2, 128], BF16, "Sg_bf")
        mix_ = sbn(2, [128, D], BF16, "mix")
        mixT_ = sbn(2, [128, 8, 128], BF16, "mixT")
        res_ = sbn(2, [128, D], F32, "res")
        gate_bc, b_gbc = sb([128, D], F32, "gate_bc")
        fg_bc, b_fgbc = sb([128, D], F32, "fg_bc")
        ada_tm, b_adatm = sb([NB, 3 * D], F32, "ada_tm")
        bg_bc, b_bgbc = sb([128, D], F32, "bg_bc")
        sel, b_sel = sb([NB, NB, 128], F32, "sel")
        stats_ = [st.enter_context(nc.sbuf_tensor(f"S_stats{i}", [128, 16], F32)) for i in range(2)]
        msq_ = [st.enter_context(nc.sbuf_tensor(f"S_msq{i}", [128, 16], F32)) for i in range(2)]
        rstd_ = [st.enter_context(nc.sbuf_tensor(f"S_rstd{i}", [128, 16], F32)) for i in range(2)]
        b_stats = [{k: Buf(f"stats{i}{k}") for k in "xyf"} for i in range(2)]
        b_msq = [{k: Buf(f"msq{i}{k}") for k in "xyf"} for i in range(2)]
        b_rstd = [{k: Buf(f"rstd{i}{k}") for k in "xyf"} for i in range(2)]
        eps_t, b_eps = sb([128, 1], F32, "eps_t")
        m16, b_m16 = sb([128, 2], F32, "m16")
        cT, b_cT = sb([128, 8, NB], F32, "cT")
        ng_fm, b_ngfm = sb([128, 8], F32, "ng_fm")
        bada_fm, b_badafm = sb([128, 24], F32, "bada_fm")
        gng_fm, b_gngfm = sb([128, 1], F32, "gng_fm")
        ada_fm, b_ada = sb([128, 24, NB], F32, "ada_fm")
        A_fm, b_Afm = sb([128, 8, NB], F32, "A_fm")

        headb = [ps([128, 512], F32, f"phead{i}") for i in range(2)]
        tailb = [ps([128, 512], F32, f"ptail{i}") for i in range(3)]
        pT_ = [ps([128, 8, 128], BF16, f"pT{i}") for i in range(2)]
        pM, b_pM = ps([128, 512], F32, "pM")
        _rr = {"h": 0, "t": 0}

        def bank(kind="t"):
            pool_ = headb if kind == "h" else tailb
            t = pool_[_rr[kind] % len(pool_)]
            _rr[kind] += 1
            return t

        for s in ["ld_x0", "ld_x1", "ld_x2", "ld_t0", "ld_t1", "ld_s0", "ld_s1", "ld_s2", "ld_s3", "st0", "st1"]:
            kb.new_sem(s)

        kb.dma("sp", None, [(cst[:], cst_d[:, :])], writes=[b_cst])
        kb.dma("sp", None, [(cT[:, :, b], c_d[b:b + 1, :].rearrange("o (k p) -> p (o k)", p=128)) for b in range(NB)],
               writes=[b_cT], allow_slow_non_contiguous=True)
        kb.dma("sp", None, [(ng_fm[:], ng_d.rearrange("o (k p) -> p (o k)", p=128))], writes=[b_ngfm],
               allow_slow_non_contiguous=True)
        kb.dma("sp", None, [(bada_fm[:, j * 8:(j + 1) * 8], bada_d[:, j * 1024:(j + 1) * 1024].rearrange("o (k p) -> p (o k)", p=128))
                            for j in range(3)], writes=[b_badafm], allow_slow_non_contiguous=True)
        kb.dma("sp", None, [(gng_fm[:], gng_d.rearrange("o (p one) -> (o p) one", one=1))], writes=[b_gngfm],
               allow_slow_non_contiguous=True)
        kb.dma("sp", None, [(fg_bc[:], fg_d[0:1, :].to_broadcast([128, D]))], writes=[b_fgbc])
        kb.dma("pool", None, [(w_in_bf[:, kc, :], win_d[kc * 128:(kc + 1) * 128, :]) for kc in range(8)],
               writes=[b_win], cost=90.0, max_dma_last_dim=4096)
        kb.dma("pool", None, [(w_out_bf[:, kc, :], wout_d[kc * 128:(kc + 1) * 128, :]) for kc in range(4)],
               writes=[b_wout], cost=20.0, max_dma_last_dim=4096)
        kb.dma("pool", None, [(wg_aug[0:16, :], wg_d[:, :]), (wg_aug[16:17, :], bg_d[:, :])], writes=[b_wg])

        dve(lambda: V.memset(eps_t[:], EPS), 1, [], [b_eps])
        dve(lambda: V.memset(m16[:], -1.0 / 16.0), 2, [], [b_m16])
        for i in range(2):
            dve(lambda i=i: V.memset(glrT_[i][0][:], 1.0), 128, [], [glrT_[i][1]])
            dve(lambda i=i: V.memset(geT_[i][0][:], 0.0), 768, [], [geT_[i][1]])
        dve(lambda: V.tensor_copy(out=ident_bf[:], in_=identf), 128, [b_cst], [b_idbf])

        for kc in range(4, 8):
            stg, b_stg = outs[kc % 2]
            kb.dma("sp", f"ld_s{kc % 2}", [(stg[:], wout_d[kc * 128:(kc + 1) * 128, :])], writes=[b_stg])
            dve(lambda kc=kc, stg=stg: V.tensor_scalar(out=w_out_bf[:, kc, :], in0=stg[:], scalar1=gng_fm[:, 0:1], scalar2=None, op0=ALU.mult),
                1024, [b_stg, b_gngfm], [b_wout])

        act(lambda: S_.activation(out=cT[:], in_=cT[:], func=AF.Silu), 8 * NB, [b_cT], [b_cT])
        kb.dma("sp", None, [(bg_bc[:], bada_d[0:1, 2048:3072].to_broadcast([128, D]))], writes=[b_bgbc])
        for bb in range(NB):
            dve(lambda bb=bb: V.tensor_copy(out=sel[:, bb, :], in_=identf[0:NB, bb:bb + 1].to_broadcast([NB, 128])), 128, [b_cst], [b_sel])
        accb = [headb[0], headb[1], tailb[0], tailb[1], tailb[2], (pM, b_pM)]
        stg_list = [outs[0], outs[1], res_[0], res_[1]]
        si = 0
        for kc in range(8):
            for th in range(3):
                stg, b_stg = stg_list[si % 4]
                kb.dma("sp", f"ld_s{si % 4}", [(stg[:], wada_d[kc * 128:(kc + 1) * 128, th * 1024:(th + 1) * 1024])], writes=[b_stg])
                (pa, b_pa), (pb2, b_pb2) = accb[2 * th], accb[2 * th + 1]

                def mm_ada(kc=kc, stg=stg, pa=pa, pb2=pb2):
                    T_.matmul(pa[0:NB, :], lhsT=cT[:, kc, :], rhs=stg[:, 0:512], start=(kc == 0), stop=(kc == 7))
                    return T_.matmul(pb2[0:NB, :], lhsT=cT[:, kc, :], rhs=stg[:, 512:1024], start=(kc == 0), stop=(kc == 7))
                pe(mm_ada, 2 * 4 * 512, [b_stg, b_cT], [b_pa, b_pb2])
                si += 1
        for blk in range(6):
            pa, b_pa = accb[blk]
            act(lambda blk=blk, pa=pa: S_.copy(out=ada_tm[:, blk * 512:(blk + 1) * 512], in_=pa[0:NB, :]), 512, [b_pa], [b_adatm])

        def tr_ada():
            for j in range(16):
                i_ = T_.transpose(out=pM[:, j * NB:(j + 1) * NB], in_=ada_tm[0:NB, j * 128:(j + 1) * 128], identity=identf[0:NB, 0:NB])
            return i_
        pe(tr_ada, 16 * 64 * 4, [b_adatm, b_cst], [b_pM])
        dve(lambda: V.tensor_tensor(out=ada_fm[:, 0:16, :], in0=pM[:, 0:16 * NB].rearrange("p (j b) -> p j b", b=NB),
                                    in1=bada_fm[:, 0:16].unsqueeze(2).to_broadcast([128, 16, NB]), op=ALU.add), 16 * NB, [b_pM, b_badafm], [b_ada])
        dve(lambda: V.tensor_scalar(out=A_fm[:], in0=ada_fm[:, 8:16, :], scalar1=1.0, scalar2=None, op0=ALU.add), 8 * NB, [b_ada], [b_Afm])
        dve(lambda: V.tensor_tensor(out=A_fm[:], in0=A_fm[:], in1=ng_fm[:].unsqueeze(2).to_broadcast([128, 8, NB]), op=ALU.mult),
            8 * NB, [b_Afm, b_ngfm], [b_Afm])

        cd = [G[h] ** 128.0 for h in range(4)]
        LN_EIGHTH = math.log(0.125)

        def rsqrt_cols(par, grp, c0, c1, inv_n):
            stt, msq, rstd = stats_[par], msq_[par], rstd_[par]

            act(lambda: S_.activation(out=msq[:, c0:c1], in_=stt[:, c0:c1], func=AF.Ln, scale=inv_n, bias=eps_t[:, 0:1]),
                c1 - c0, [b_stats[par][grp], b_eps], [b_msq[par][grp]])
            act(lambda: S_.activation(out=rstd[:, c0:c1], in_=msq[:, c0:c1], func=AF.Exp, scale=-0.5),
                c1 - c0, [b_msq[par][grp]], [b_rstd[par][grp]])

        def load_tile(g):
            t = g % NT
            xt, b_x = xs[g % 3]
            tt, b_t = tabs[g % 2]
            kb.dma("sp", f"ld_x{g % 3}", [(xt[:], x_d[g * 128:(g + 1) * 128, :])], writes=[b_x])
            kb.dma("sp", f"ld_t{g % 2}", [(tt[:], tab_d[t, :, :])], writes=[b_t])

        def seq_start(b):
            dve(lambda: V.memset(R[:], 0.0), 512, [], [b_R])
            dve(lambda: V.memset(R_bf[:], 0.0), 512, [], [b_Rbf], mode=2.0)
            dve(lambda: V.memset(Sg[:], 0.0), 256, [], [b_Sg])
            dve(lambda: V.memset(Sg_bf[:], 0.0), 256, [], [b_Sgbf], mode=2.0)
            for n in range(2):
                pb, b_pb = tailb[1 + n]
                pe(lambda n=n, pb=pb: T_.matmul(pb[:, :], lhsT=sel[:, b, :], rhs=ada_tm[0:NB, 2048 + n * 512:2048 + (n + 1) * 512], start=True, stop=True),
                   4 * 512, [b_sel, b_adatm], [b_pb])
                dve(lambda n=n, pb=pb: V.tensor_tensor(out=gate_bc[:, n * 512:(n + 1) * 512], in0=pb[:], in1=bg_bc[:, n * 512:(n + 1) * 512], op=ALU.add),
                    512, [b_pb, b_bgbc], [b_gbc])

        def rotary(pb, b_pb, tt, b_t, toff, dst, b_dst):
            src4 = pb[:].rearrange("p (h two f) -> p h two f", h=4, two=2)
            cs4 = tt[:, toff:toff + 256].rearrange("p (h f) -> p h f", h=4).unsqueeze(2).to_broadcast([128, 4, 2, 64])
            sn4 = tt[:, toff + 256:toff + 512].rearrange("p (h f) -> p h f", h=4).unsqueeze(2).to_broadcast([128, 4, 2, 64])
            A4 = rA[:].rearrange("p (h two f) -> p h two f", h=4, two=2)
            B4 = rB[:].rearrange("p (h two f) -> p h two f", h=4, two=2)
            d4 = dst[:].rearrange("p (h two f) -> p h two f", h=4, two=2)
            dve(lambda: V.tensor_tensor(out=A4, in0=src4, in1=cs4, op=ALU.mult), 512, [b_pb, b_t], [b_rA])
            dve(lambda: V.tensor_tensor(out=B4, in0=src4, in1=sn4, op=ALU.mult), 512, [b_pb, b_t], [b_rB])
            dve(lambda: V.tensor_tensor(out=d4[:, :, 0, :], in0=A4[:, :, 0, :], in1=B4[:, :, 1, :], op=ALU.subtract), 256, [b_rA, b_rB], [b_dst])
            dve(lambda: V.tensor_tensor(out=d4[:, :, 1, :], in0=B4[:, :, 0, :], in1=A4[:, :, 1, :], op=ALU.add), 256, [b_rA, b_rB], [b_dst])

        def inproj_group(hTt, b_hT, c0, ncols):
            pb, b_pb = bank("h")

            def mm():
                for kc in range(8):
                    i = T_.matmul(pb[:, 0:ncols], lhsT=hTt[:, kc, :], rhs=w_in_bf[:, kc, c0:c0 + ncols], start=(kc == 0), stop=(kc == 7))
                return i
            pe(mm, 8 * ncols, list(b_hT) + [b_win], [b_pb])
            return pb, b_pb

        def tile_body(g, phase):
            b = g // NT
            t = g % NT
            par = g % 2
            xt, b_x = xs[g % 3]
            tt, b_t = tabs[par]
            hTt, b_hT = hT[par]
            ot, b_o = outs[par]
            xn, b_xn = xn_[par]
            q_r, b_qr = q_r_[par]
            k_r, b_kr = k_r_[par]
            v_bf, b_v = v_bf_[par]
            gv_bf, b_gv = gv_bf_[par]
            szr, b_szr = szr_[par]
            szg, b_szg = szg_[par]
            qkT, b_qkT = qkT_[par]
            geT, b_geT = geT_[par]
            q_e, b_qe = q_e_[par]
            k_e, b_ke = k_e_[par]
            l_sb, b_l = l_sb_[par]
            E1, b_E1 = E1_[par]
            E2, b_E2 = E2_[par]
            a_fm, b_afm = a_fm_[par]
            glrT, b_glrT = glrT_[par]
            ST_r, b_STr = ST_r_[par]
            ST_g, b_STg = ST_g_[par]
            mix, b_mix = mix_[par]
            mixT, b_mixT = mixT_[par]
            res, b_res = res_[par]
            stats, rstd = stats_[par], rstd_[par]
            bs, br = b_stats[par], b_rstd[par]
            pT, b_pT = pT_[par]
            if phase == 1:
                if t == 0:
                    seq_start(b)
                return tile_main(locals())
            act(lambda: S_.activation(out=xn[:], in_=xt[:], func=AF.Square, accum_out=stats[:, 0:1]), 1024, [b_x], [bs["x"], b_xn])
            rsqrt_cols(par, "x", 0, 1, 1.0 / D)
            act(lambda: S_.activation(out=xn[:], in_=xt[:], func=AF.Copy, scale=rstd[:, 0:1]), 1024, [b_x, br["x"]], [b_xn])
            def tr_x():
                for kc in range(8):
                    i = T_.transpose(out=pT[:, kc, :], in_=xn[:, kc * 128:(kc + 1) * 128], identity=ident_bf[:])
                return i
            pe(tr_x, 1024, [b_xn, b_idbf], [b_pT])

            b_hT2 = b_hTb[par]

            def ev_h():
                for kc in range(4):
                    i = S_.activation(out=hTt[:, kc, :], in_=pT[:, kc, :], func=AF.Identity,
                                      scale=A_fm[:, kc, b:b + 1], bias=ada_fm[:, kc, b:b + 1])
                return i
            kb.op("act", ev_h, [b_pT, b_Afm, b_ada], [b_hT], cost=4 * 0.38)

            def ev_h2():
                for kc in range(4, 8):
                    i = V.tensor_scalar(out=hTt[:, kc, :], in0=pT[:, kc, :], scalar1=A_fm[:, kc, b:b + 1], scalar2=ada_fm[:, kc, b:b + 1],
                                        op0=ALU.mult, op1=ALU.add)
                return i
            def ev_h2a():
                for kc in range(4, 8):
                    i = S_.activation(out=hTt[:, kc, :], in_=pT[:, kc, :], func=AF.Identity,
                                      scale=A_fm[:, kc, b:b + 1], bias=ada_fm[:, kc, b:b + 1])
                return i
            if EVH2:
                kb.op("dve", ev_h2, [b_pT, b_Afm, b_ada], [b_hT2], cost=4 * 0.25)
            else:
                kb.op("act", ev_h2a, [b_pT, b_Afm, b_ada], [b_hT2], cost=4 * 0.38)
            def mm_glr():
                for kc in range(8):
                    i = T_.matmul(pM[0:16, 256:384], lhsT=w_in_bf[:, kc, 3584:3600], rhs=hTt[:, kc, :], start=(kc == 0), stop=(kc == 7))
                return i
            pe(mm_glr, 1024, [b_hT, b_hT2, b_win], [b_pM])
            act(lambda: S_.copy(out=glrT[0:16, :], in_=pM[0:16, 256:384]), 128, [b_pM], [b_glrT])
            pe(lambda: T_.matmul(pM[:, 0:256], lhsT=glrT[0:17, :], rhs=wg_aug[0:17, :], start=True, stop=True), 256, [b_glrT, b_wg], [b_pM])
            act(lambda: S_.activation(out=e_sb[:], in_=pM[:, 0:256], func=AF.Exp, scale=-1.0), 256, [b_pM], [b_e])
            act(lambda: S_.activation(out=l_sb[:], in_=e_sb[:], func=AF.Ln, bias=1.0), 256, [b_e], [b_l])

            def mm_bl():
                T_.matmul(pM[:, 384:385], lhsT=l_sb[:, 0:128], rhs=m16[:, 0:1], start=True, stop=True)
                i = T_.matmul(pM[:, 385:386], lhsT=l_sb[:, 128:256], rhs=m16[:, 0:1], start=True, stop=True)
                return i
            if BSPLIT:
                pe(mm_bl, 512, [b_l, b_m16], [b_pM])
                pe(lambda: T_.matmul(pM[:, 0:256], lhsT=Lp, rhs=l_sb[:], start=True, stop=True), 1024, [b_l, b_cst], [b_pM])
            else:
                def mm_b():
                    mm_bl()
                    return T_.matmul(pM[:, 0:256], lhsT=Lp, rhs=l_sb[:], start=True, stop=True)
                pe(mm_b, 512 + 1024, [b_l, b_m16, b_cst], [b_pM])
            act(lambda: S_.activation(out=E1[:], in_=pM[:, 0:256], func=AF.Exp, bias=LN_EIGHTH), 256, [b_pM], [b_E1])
            act(lambda: S_.activation(out=E2[:], in_=pM[:, 0:256], func=AF.Exp, scale=-1.0), 256, [b_pM], [b_E2])
            act(lambda: S_.activation(out=a_fm[:], in_=pM[:, 384:386], func=AF.Exp), 2, [b_pM], [b_afm])
            return None

        def tile_main(L):
            g, b, t, par = L["g"], L["b"], L["t"], L["par"]
            xt, b_x, tt, b_t, hTt, b_hT, ot, b_o = L["xt"], L["b_x"], L["tt"], L["b_t"], L["hTt"], L["b_hT"], L["ot"], L["b_o"]
            b_hT2 = b_hTb[par]
            q_r, b_qr, k_r, b_kr, v_bf, b_v, gv_bf, b_gv = L["q_r"], L["b_qr"], L["k_r"], L["b_kr"], L["v_bf"], L["b_v"], L["gv_bf"], L["b_gv"]
            szr, b_szr, szg, b_szg, qkT, b_qkT, geT, b_geT = L["szr"], L["b_szr"], L["szg"], L["b_szg"], L["qkT"], L["b_qkT"], L["geT"], L["b_geT"]
            q_e, b_qe, k_e, b_ke = L["q_e"], L["b_qe"], L["k_e"], L["b_ke"]
            E1, b_E1, E2, b_E2, a_fm, b_afm = L["E1"], L["b_E1"], L["E2"], L["b_E2"], L["a_fm"], L["b_afm"]
            ST_r, b_STr, ST_g, b_STg, mix, b_mix, mixT, b_mixT, res, b_res = (L["ST_r"], L["b_STr"], L["ST_g"], L["b_STg"], L["mix"], L["b_mix"],
                                                                                L["mixT"], L["b_mixT"], L["res"], L["b_res"])
            stats, rstd, bs, br, pT, b_pT = L["stats"], L["rstd"], L["bs"], L["br"], L["pT"], L["b_pT"]
            pq, b_pq = inproj_group(hTt, (b_hT, b_hT2), 0, 512)
            rotary(pq, b_pq, tt, b_t, 0, q_r, b_qr)
            pk, b_pk = inproj_group(hTt, (b_hT, b_hT2), 512, 512)
            rotary(pk, b_pk, tt, b_t, 512, k_r, b_kr)
            pg, b_pg = inproj_group(hTt, (b_hT, b_hT2), 2048, 512)
            dve(lambda: V.tensor_tensor(out=q_e[:], in0=pg[:, 0:256], in1=E1[:], op=ALU.mult), 256, [b_pg, b_E1], [b_qe])
            dve(lambda: V.tensor_tensor(out=k_e[:], in0=pg[:, 256:512], in1=E2[:], op=ALU.mult), 256, [b_pg, b_E2], [b_ke])
            pv, b_pv = inproj_group(hTt, (b_hT, b_hT2), 1024, 512)
            act(lambda: S_.copy(out=v_bf[:], in_=pv[:]), 512, [b_pv], [b_v])
            pgv, b_pgv = inproj_group(hTt, (b_hT, b_hT2), 2560, 512)
            act(lambda: S_.copy(out=gv_bf[:], in_=pgv[:]), 512, [b_pgv], [b_gv])
            pz, b_pz = inproj_group(hTt, (b_hT, b_hT2), 1536, 512)
            pgz, b_pgz = inproj_group(hTt, (b_hT, b_hT2), 3072, 512)

            def silus():
                S_.activation(out=szr[:], in_=pz[:], func=AF.Silu)
                return S_.activation(out=szg[:], in_=pgz[:], func=AF.Silu)
            kb.op("act", silus, [b_pz, b_pgz], [b_szr, b_szg], cost=2 * 0.64)
            def tr_qk():
                for h in range(4):
                    T_.transpose(out=pT[:, h, :], in_=q_r[:, h * 128:(h + 1) * 128], identity=ident_bf[:])
                for h in range(4):
                    i = T_.transpose(out=pT[:, 4 + h, :], in_=k_r[:, h * 128:(h + 1) * 128], identity=ident_bf[:])
                return i
            pe(tr_qk, 1024, [b_qr, b_kr, b_idbf], [b_pT])
            dve(lambda: V.tensor_copy(out=qkT[:], in_=pT[:]), 1024, [b_pT], [b_qkT], mode=2.0)
            def tr_ge():
                for u in range(2):
                    T_.transpose(out=pT[:, u, :], in_=q_e[:, u * 128:(u + 1) * 128], identity=ident_bf[:])
                for u in range(2):
                    i = T_.transpose(out=pT[:, 2 + u, :], in_=k_e[:, u * 128:(u + 1) * 128], identity=ident_bf[:])
                return i
            pe(tr_ge, 512, [b_qe, b_ke, b_idbf], [b_pT])

            def ev_ge():
                for h in range(4):
                    r0 = (h % 2) * 64
                    V.tensor_copy(out=geT[r0:r0 + 64, h, :], in_=pT[r0:r0 + 64, h // 2, :])
                return V.tensor_copy(out=geT[:, 4:6, :], in_=pT[:, 2:4, :])
            kb.op("dve", ev_ge, [b_pT], [b_geT], cost=5 * 0.25)
            (T0, bT0), (T1, bT1), (T2, bT2) = tailb
            psr, b_psr = T0, bT0
            psg, b_psg = T1, bT1
            pyr, b_pyr = T2, bT2
            pkr, b_pkr = T0, bT0
            pyg, b_pyg = T1, bT1
            pkg, b_pkg = T0, bT0

            def mm_sr():
                for h in range(4):
                    i = T_.matmul(psr[:, h * 128:(h + 1) * 128], lhsT=qkT[:, 4 + h, :], rhs=qkT[:, h, :], start=True, stop=True)
                return i
            pe(mm_sr, 512, [b_qkT], [b_psr])
            dve(lambda: V.tensor_tensor(out=ST_r[:], in0=psr[:], in1=mask_r, op=ALU.mult), 512, [b_psr, b_cst], [b_STr])

            def mm_sg():
                for h in range(4):
                    i = T_.matmul(psg[:, h * 128:(h + 1) * 128], lhsT=geT[:, 4 + h // 2, :], rhs=geT[:, h, :], start=True, stop=True)
                return i
            pe(mm_sg, 512, [b_geT], [b_psg])
            dve(lambda: V.tensor_tensor(out=ST_g[:].rearrange("p (h i) -> p h i", h=4), in0=psg[:].rearrange("p (h i) -> p h i", h=4),
                                        in1=mask_g.unsqueeze(1).to_broadcast([128, 4, 128]), op=ALU.mult), 512, [b_psg, b_cst], [b_STg])

            def mm_yr():
                for h in range(4):
                    T_.matmul(pyr[:, h * 128:(h + 1) * 128], lhsT=ST_r[:, h * 128:(h + 1) * 128], rhs=v_bf[:, h * 128:(h + 1) * 128],
                              start=True, stop=False)
                    i = T_.matmul(pyr[:, h * 128:(h + 1) * 128], lhsT=qkT[:, h, :], rhs=R_bf[:, h, :], start=False, stop=True)
                return i
            pe(mm_yr, 1024, [b_STr, b_v, b_qkT, b_Rbf], [b_pyr])

            def mm_kr():
                for h in range(4):
                    i = T_.matmul(pkr[:, h * 128:(h + 1) * 128], lhsT=k_r[:, h * 128:(h + 1) * 128], rhs=v_bf[:, h * 128:(h + 1) * 128],
                                  start=True, stop=True)
                return i
            pe(mm_kr, 512, [b_kr, b_v], [b_pkr])

            def up_R():
                for h in range(4):
                    i = V.scalar_tensor_tensor(out=R[:, h, :], in0=R[:, h, :], scalar=cd[h], in1=pkr[:, h * 128:(h + 1) * 128],
                                               op0=ALU.mult, op1=ALU.add)
                return i
            kb.op("dve", up_R, [b_pkr], [b_R], cost=4 * 0.3)
            act(lambda: S_.copy(out=R_bf[:], in_=R[:]), 512, [b_R], [b_Rbf])

            def mm_yg():
                for h in range(4):
                    T_.matmul(pyg[:, h * 128:(h + 1) * 128], lhsT=ST_g[:, h * 128:(h + 1) * 128], rhs=gv_bf[:, h * 128:(h + 1) * 128],
                              start=True, stop=False)
                    i = T_.matmul(pyg[:, h * 128:(h + 1) * 128], lhsT=geT[:, h, :], rhs=Sg_bf[:, h // 2, :], start=False, stop=True)
                return i
            pe(mm_yg, 1024, [b_STg, b_gv, b_geT, b_Sgbf], [b_pyg])

            def mm_kg():
                for h in range(4):
                    r0 = (h % 2) * 64
                    u = h // 2
                    i = T_.matmul(pkg[r0:r0 + 64, u * 128:(u + 1) * 128], lhsT=k_e[:, h * 64:(h + 1) * 64], rhs=gv_bf[:, h * 128:(h + 1) * 128],
                                  start=True, stop=True)
                return i
            pe(mm_kg, 512, [b_ke, b_gv], [b_pkg])
            Sg2 = Sg[:].rearrange("p u e -> p (u e)")
            dve(lambda: V.tensor_tensor(out=Sg2, in0=Sg2, in1=pkg[:, 0:256], op=ALU.add), 256, [b_pkg], [b_Sg])
            dve(lambda: V.tensor_tensor(out=Sg[:], in0=Sg[:], in1=a_fm[:].unsqueeze(2).to_broadcast([128, 2, 128]), op=ALU.mult), 256, [b_afm], [b_Sg])
            act(lambda: S_.copy(out=Sg_bf[:], in_=Sg[:]), 256, [b_Sg], [b_Sgbf])
            def sq_y():
                for h in range(4):
                    S_.activation(out=mix[:, h * 128:(h + 1) * 128], in_=pyr[:, h * 128:(h + 1) * 128], func=AF.Square, accum_out=stats[:, 1 + h:2 + h])
                for h in range(4):
                    i = S_.activation(out=mix[:, 512 + h * 128:512 + (h + 1) * 128], in_=pyg[:, h * 128:(h + 1) * 128], func=AF.Square, accum_out=stats[:, 5 + h:6 + h])
                return i
            kb.op("act", sq_y, [b_pyr, b_pyg], [bs["y"], b_mix], cost=8 * 0.42)
            rsqrt_cols(par, "y", 1, 9, 1.0 / 128.0)

            def mk_mix():
                for h in range(4):
                    V.scalar_tensor_tensor(out=mix[:, h * 128:(h + 1) * 128], in0=pyr[:, h * 128:(h + 1) * 128], scalar=rstd[:, 1 + h:2 + h],
                                           in1=szr[:, h * 128:(h + 1) * 128], op0=ALU.mult, op1=ALU.mult)
                for h in range(4):
                    i = V.scalar_tensor_tensor(out=mix[:, 512 + h * 128:512 + (h + 1) * 128], in0=pyg[:, h * 128:(h + 1) * 128],
                                               scalar=rstd[:, 5 + h:6 + h], in1=szg[:, h * 128:(h + 1) * 128], op0=ALU.mult, op1=ALU.mult)
                return i
            kb.op("dve", mk_mix, [b_pyr, b_pyg, br["y"], b_szr, b_szg], [b_mix], cost=8 * 0.3)
            def tr_m():
                for kc in range(8):
                    i = T_.transpose(out=pT[:, kc, :], in_=mix[:, kc * 128:(kc + 1) * 128], identity=ident_bf[:])
                return i
            pe(tr_m, 1024, [b_mix, b_idbf], [b_pT])
            dve(lambda: V.tensor_copy(out=mixT[:], in_=pT[:]), 1024, [b_pT], [b_mixT], mode=2.0)
            for n in range(2):
                po, b_po = tailb[0] if n == 0 else tailb[2]

                def mm_o(n=n, po=po):
                    for kc in range(8):
                        i = T_.matmul(po[:], lhsT=mixT[:, kc, :], rhs=w_out_bf[:, kc, n * 512:(n + 1) * 512], start=(kc == 0), stop=(kc == 7))
                    return i
                pe(mm_o, 8 * 512, [b_mixT, b_wout], [b_po])
                sl = slice(n * 512, (n + 1) * 512)
                dve(lambda po=po, sl=sl: V.tensor_tensor(out=res[:, sl], in0=po[:], in1=gate_bc[:, sl], op=ALU.mult), 512, [b_po, b_gbc], [b_res])
            dve(lambda: V.tensor_tensor(out=res[:], in0=res[:], in1=xt[:], op=ALU.add), 1024, [b_res, b_x], [b_res])
            act(lambda: S_.activation(out=ot[:], in_=res[:], func=AF.Square, accum_out=stats[:, 9:10]), 1024, [b_res], [bs["f"], b_o])
            rsqrt_cols(par, "f", 9, 10, 1.0 / D)
            dve(lambda: V.scalar_tensor_tensor(out=ot[:], in0=res[:], scalar=rstd[:, 9:10], in1=fg_bc[:], op0=ALU.mult, op1=ALU.mult),
                1024, [b_res, br["f"], b_fgbc], [b_o])
            kb.dma("sp", f"st{par}", [(out_d[g * 128:(g + 1) * 128, :], ot[:])], reads=[b_o])

        NG = NB * NT
        kb.tag = 0
        load_tile(0)
        tile_body(0, 0)
        for g in range(NG):
            if g + 1 < NG:
                kb.tag = g + 1
                load_tile(g + 1)
                tile_body(g + 1, 0)
            kb.tag = g
            tile_body(g, 1)
        kb.run()
        build_nc.last_est_us = kb.est_us
        build_nc.last_kb = kb
    return nc


_CACHE = {}


def kernel(x, c, norm_gain, w_ada, b_ada, w_in, w_gate_up, b_gate_up, gla_norm_gain, w_out, final_gain):
    NB, NT = 4, 16
    f = lambda a: np.ascontiguousarray(np.asarray(a, dtype=np.float32))
    x = f(x)
    c = f(c)
    if "nc" not in _CACHE:
        _CACHE["nc"] = build_nc(NB, NT)
        _CACHE["consts"] = make_consts()
    nc = _CACHE["nc"]
    tab, cst = _CACHE["consts"]
    shared = {
        "norm_gain": f(norm_gain).reshape(1, D), "w_ada": f(w_ada).reshape(D, 3 * D), "b_ada": f(b_ada).reshape(1, 3 * D),
        "w_in": f(w_in).reshape(D, DIN), "w_gate_up": f(w_gate_up).reshape(16, 256), "b_gate_up": f(b_gate_up).reshape(1, 256),
        "gla_norm_gain": f(gla_norm_gain).reshape(1, 128), "w_out": f(w_out).reshape(D, D), "final_gain": f(final_gain).reshape(1, D),
        "tab": tab, "cst": cst,
    }
    in_maps = []
    for i in range(NCORES):
        m = dict(shared)
        m["x"] = x[i * NB:(i + 1) * NB].reshape(NB * SEQ, D)
        m["c"] = c[i * NB:(i + 1) * NB]
        in_maps.append(m)
    res = run_bass_kernel_spmd(nc, in_maps, core_ids=list(range(NCORES)))
    out = np.concatenate([r["out"].reshape(NB, SEQ, D) for r in res.results], axis=0)
    return out.astype(np.float32)
```
